# Optimizing a Trainium2 kernel written in Bass

```python
import math
import jax
import jax.numpy as jnp
from jax import lax
import numpy as np

D_MODEL = 2048
BATCH = 16
SEQ = 2048
DEPTH = 4

GRID_W = 64
CTX_LEN = 256
EPS = 1e-6
S5_WIDTH = D_MODEL // 2
S5_GROUP_W = 16
S5_GROUPS = S5_WIDTH // S5_GROUP_W
S5_STATE = 64
HG_WIDTH = D_MODEL // 2
HG_HEAD_DIM = 128
HG_HEADS = HG_WIDTH // HG_HEAD_DIM
HG_CHUNK = 32
HG_START = S5_WIDTH
HG_END = HG_START + 5 * HG_WIDTH
PROJ_WIDTH = HG_END + 2 * D_MODEL
PEER_HEADS = 8
PEER_D_KEY = 256
PEER_HALF = PEER_D_KEY // 2
PEER_N_KEYS = 128
PEER_N_EXPERTS = PEER_N_KEYS * PEER_N_KEYS
PEER_TOPK = 16
PEER_BLOCK = 128

kernel_name = "hybrid_s5_hgrn2_peer_dit"


def rms_norm(x, g):
    x32 = x.astype(jnp.float32)
    y = x32 * lax.rsqrt(jnp.mean(x32 * x32, axis=-1, keepdims=True) + EPS)
    return (y * g.astype(jnp.float32)).astype(x.dtype)


def modulate(h, shift, scale):
    return h * (1.0 + scale) + shift


def grid_transpose(t, rows, cols):
    b, l, ch = t.shape
    return t.reshape(b, rows, cols, ch).transpose(0, 2, 1, 3).reshape(b, l, ch)


def _linear_combine(left, right):
    a_l, b_l = left
    a_r, b_r = right
    return a_r * a_l, a_r * b_l + b_r


def s5_mixer(u, a_re, a_im, b_re, b_im, c_re, c_im, log_dt, d_skip, w_glu, b_glu, h0, with_output):
    bsz, l, _ = u.shape
    f32 = jnp.float32
    ug = u.astype(f32).reshape(bsz, l, S5_GROUPS, S5_GROUP_W)
    y, finals = None, []
    for d in range(2):
        lam = lax.complex(a_re[d].astype(f32), a_im[d].astype(f32))
        dt = jnp.exp(log_dt[d].astype(f32))[:, None]
        a_bar = jnp.exp(lam * dt)
        b_bar = ((a_bar - 1.0) / lam)[..., None] * lax.complex(b_re[d].astype(f32), b_im[d].astype(f32))
        seq = ug if d == 0 else ug[:, ::-1]
        bu = jnp.einsum('gpc,blgc->blgp', b_bar, seq.astype(jnp.complex64))
        if h0[d] is not None:
            bu = bu.at[:, 0].add(a_bar * h0[d])
        _, states = lax.associative_scan(_linear_combine, (jnp.broadcast_to(a_bar, bu.shape), bu), axis=1)
        finals.append(states[:, -1])
        if with_output:
            c_mat = lax.complex(c_re[d].astype(f32), c_im[d].astype(f32))
            yd = jnp.real(jnp.einsum('gcp,blgp->blgc', c_mat, states))
            yd = yd if d == 0 else yd[:, ::-1]
            y = yd if y is None else y + yd
    if not with_output:
        return None, finals
    y = y.reshape(bsz, l, S5_WIDTH) + d_skip.astype(f32) * u.astype(f32)
    z = jax.nn.gelu(y)
    out = z * jax.nn.sigmoid(z @ w_glu.astype(f32) + b_glu.astype(f32))
    return out.astype(u.dtype), finals


def hgrn_chunk_scan(q, k, v, log_f, s0, with_output):
    bsz, l, h, dk = k.shape
    dv = v.shape[-1]
    n = l // HG_CHUNK
    blocks = lambda t: t.reshape(bsz, n, HG_CHUNK, h, t.shape[-1])
    k, v, log_f = blocks(k), blocks(v), blocks(log_f)
    b = jnp.cumsum(log_f, axis=2)
    b_last = b[:, :, -1:]
    chunk_kv = jnp.einsum('bnshk,bnshv->bnhkv', k * jnp.exp(b_last - b), v)
    chunk_decay = jnp.exp(b_last[:, :, 0])
    s_init = jnp.zeros((bsz, h, dk, dv), jnp.float32) if s0 is None else s0

    def step(s, inp):
        dec, kv = inp
        return dec[..., None] * s + kv, s

    s_final, s_in = lax.scan(step, s_init, (jnp.moveaxis(chunk_decay, 1, 0), jnp.moveaxis(chunk_kv, 1, 0)))
    if not with_output:
        return None, s_final
    s_in = jnp.moveaxis(s_in, 0, 1)
    q = blocks(q)
    b_ref = b[:, :, HG_CHUNK // 2 - 1:HG_CHUNK // 2]
    scores = jnp.einsum('bnthk,bnshk->bnhts', q * jnp.exp(b - b_ref), k * jnp.exp(b_ref - b))
    mask = jnp.tril(jnp.ones((HG_CHUNK, HG_CHUNK), dtype=bool))
    scores = jnp.where(mask, scores, 0.0)
    o = jnp.einsum('bnhts,bnshv->bnthv', scores, v)
    o = o + jnp.einsum('bnthk,bnhkv->bnthv', q * jnp.exp(b), s_in)
    return o.reshape(bsz, l, h, dv), s_final


def hgrn_mixer(p_hg, lb, norm_g, s0, with_output):
    bsz, l, _ = p_hg.shape
    q_raw, i_raw, f_fw, f_bw, og = jnp.split(p_hg.astype(jnp.float32), 5, axis=-1)
    heads = lambda t: t.reshape(bsz, l, HG_HEADS, HG_HEAD_DIM)
    q = heads(jax.nn.silu(q_raw)) if with_output else None
    v = heads(i_raw)
    o, finals = None, []
    for d, f_raw in enumerate((f_fw, f_bw)):
        f = lb[d] + (1.0 - lb[d]) * jax.nn.sigmoid(f_raw)
        seqs = (q, heads(1.0 - f), v, heads(jnp.log(f)))
        if d == 1:
            seqs = tuple(None if t is None else t[:, ::-1] for t in seqs)
        od, s_fin = hgrn_chunk_scan(seqs[0], seqs[1], seqs[2], seqs[3], s0[d], with_output)
        finals.append(s_fin)
        if with_output:
            od = od if d == 0 else od[:, ::-1]
            o = od if o is None else o + od
    if not with_output:
        return None, finals
    o = heads(o.reshape(bsz, l, HG_WIDTH) * jax.nn.silu(og))
    o = o * lax.rsqrt(jnp.mean(o * o, axis=-1, keepdims=True) + EPS)
    return (o.reshape(bsz, l, HG_WIDTH) * norm_g.astype(jnp.float32)).astype(p_hg.dtype), finals


def merge_branches(gate_logits, y_s5, y_hg, w_bs, w_bh, w_o):
    g_s5, g_hg = jnp.split(jax.nn.sigmoid(gate_logits), 2, axis=-1)
    return (g_s5 * (y_s5 @ w_bs) + g_hg * (y_hg @ w_bh)) @ w_o


def peer_ffn(h, w_q, k1, k2, u_tab, v_tab):
    shp = h.shape
    t = h.reshape(-1, shp[-1])
    n_tok = t.shape[0]
    q = (t @ w_q).reshape(n_tok, PEER_HEADS, PEER_D_KEY)
    s1 = jnp.einsum('thd,hnd->thn', q[..., :PEER_HALF], k1).astype(jnp.float32)
    s2 = jnp.einsum('thd,hnd->thn', q[..., PEER_HALF:], k2).astype(jnp.float32)
    v1, i1 = lax.top_k(s1, PEER_TOPK)
    v2, i2 = lax.top_k(s2, PEER_TOPK)
    cand = (v1[..., :, None] + v2[..., None, :]).reshape(n_tok, PEER_HEADS, PEER_TOPK * PEER_TOPK)
    sc, ic = lax.top_k(cand, PEER_TOPK)
    experts = (jnp.take_along_axis(i1, ic // PEER_TOPK, axis=-1) * PEER_N_KEYS
               + jnp.take_along_axis(i2, ic % PEER_TOPK, axis=-1))
    g = jax.nn.softmax(sc, axis=-1).astype(h.dtype)
    nb = n_tok // PEER_BLOCK

    def block(args):
        tb, eb, gb = args
        act = jax.nn.gelu(jnp.einsum('thkd,td->thk', jnp.take(u_tab, eb, axis=0), tb))
        return jnp.einsum('thk,thkd->td', gb * act, jnp.take(v_tab, eb, axis=0))

    out = lax.map(block, (t.reshape(nb, PEER_BLOCK, shp[-1]),
                          experts.reshape(nb, PEER_BLOCK, PEER_HEADS, PEER_TOPK),
                          g.reshape(nb, PEER_BLOCK, PEER_HEADS, PEER_TOPK)))
    return out.reshape(shp)


def setup_inputs(seed: int = 0) -> dict:
    key = jax.random.key(seed)
    ks = iter(jax.random.split(key, 32))
    f32 = jnp.float32
    D = D_MODEL
    nrm = lambda shape, scale: scale * jax.random.normal(next(ks), shape, f32)
    gain = lambda shape: 1.0 + nrm(shape, 0.01)
    inp = {}
    inp['x'] = nrm((BATCH, SEQ, D), 1.0)
    inp['c'] = nrm((BATCH, D), 1.0)
    inp['ctx'] = nrm((BATCH, CTX_LEN, D), 1.0)
    inp['c_ctx'] = nrm((D,), 1.0)
    inp['w_ada'] = nrm((DEPTH, D, 6 * D), 0.5 * D ** -0.5)
    inp['b_ada'] = nrm((DEPTH, 6 * D), 0.01)
    inp['norm_mix_g'] = gain((DEPTH, D))
    inp['norm_ffn_g'] = gain((DEPTH, D))
    inp['w_in'] = nrm((DEPTH, D, PROJ_WIDTH), D ** -0.5)
    inp['s5_a_re'] = -0.5 + nrm((DEPTH, 2, S5_GROUPS, S5_STATE), 0.01)
    inp['s5_a_im'] = math.pi * jnp.arange(S5_STATE, dtype=f32) + nrm((DEPTH, 2, S5_GROUPS, S5_STATE), 0.01)
    inp['s5_b_re'] = nrm((DEPTH, 2, S5_GROUPS, S5_STATE, S5_GROUP_W), (2 * S5_GROUP_W) ** -0.5)
    inp['s5_b_im'] = nrm((DEPTH, 2, S5_GROUPS, S5_STATE, S5_GROUP_W), (2 * S5_GROUP_W) ** -0.5)
    inp['s5_c_re'] = nrm((DEPTH, 2, S5_GROUPS, S5_GROUP_W, S5_STATE), S5_STATE ** -0.5)
    inp['s5_c_im'] = nrm((DEPTH, 2, S5_GROUPS, S5_GROUP_W, S5_STATE), S5_STATE ** -0.5)
    inp['s5_log_dt'] = jax.random.uniform(next(ks), (DEPTH, 2, S5_GROUPS), f32,
                                          minval=math.log(1e-3), maxval=math.log(1e-1))
    inp['s5_d'] = nrm((DEPTH, S5_WIDTH), 1.0)
    inp['s5_w_glu'] = nrm((DEPTH, S5_WIDTH, S5_WIDTH), S5_WIDTH ** -0.5)
    inp['s5_b_glu'] = nrm((DEPTH, S5_WIDTH), 0.01)
    inp['hg_lb_gamma'] = nrm((DEPTH, 2, HG_WIDTH), 0.5)
    inp['hg_norm_g'] = gain((DEPTH, HG_WIDTH))
    inp['w_branch_s5'] = nrm((DEPTH, S5_WIDTH, D), S5_WIDTH ** -0.5)
    inp['w_branch_hg'] = nrm((DEPTH, HG_WIDTH, D), HG_WIDTH ** -0.5)
    inp['w_out'] = nrm((DEPTH, D, D), D ** -0.5)
    inp['peer_w_q'] = nrm((DEPTH, D, PEER_HEADS * PEER_D_KEY), D ** -0.5)
    inp['peer_k1'] = nrm((DEPTH, PEER_HEADS, PEER_N_KEYS, PEER_HALF), PEER_HALF ** -0.5)
    inp['peer_k2'] = nrm((DEPTH, PEER_HEADS, PEER_N_KEYS, PEER_HALF), PEER_HALF ** -0.5)
    inp['peer_u'] = nrm((DEPTH, PEER_N_EXPERTS, D), D ** -0.5)
    inp['peer_v'] = nrm((DEPTH, PEER_N_EXPERTS, D), PEER_HEADS ** -0.5)
    inp['final_norm_g'] = gain((D,))
    return inp


def reference(x, c, ctx, c_ctx, w_ada, b_ada, norm_mix_g, norm_ffn_g, w_in, s5_a_re, s5_a_im, s5_b_re, s5_b_im,
              s5_c_re, s5_c_im, s5_log_dt, s5_d, s5_w_glu, s5_b_glu, hg_lb_gamma, hg_norm_g, w_branch_s5,
              w_branch_hg, w_out, peer_w_q, peer_k1, peer_k2, peer_u, peer_v, final_norm_g):
    seq_len = x.shape[1]
    rows = seq_len // GRID_W
    p_lb = jax.nn.softmax(hg_lb_gamma.astype(jnp.float32), axis=0)
    lower_bounds = jnp.cumsum(p_lb, axis=0) - p_lb[:1]
    silu_c = jax.nn.silu(c)
    silu_cc = jax.nn.silu(c_ctx)
    for l in range(DEPTH):
        ctx_out = l < DEPTH - 1
        mod = silu_c @ w_ada[l] + b_ada[l]
        mod_c = silu_cc @ w_ada[l] + b_ada[l]
        sh_m, sc_m, gt_m, sh_f, sc_f, gt_f = jnp.split(mod[:, None, :], 6, axis=-1)
        csh_m, csc_m, cgt_m, csh_f, csc_f, cgt_f = jnp.split(mod_c, 6, axis=-1)
        s5_args = (s5_a_re[l], s5_a_im[l], s5_b_re[l], s5_b_im[l], s5_c_re[l], s5_c_im[l], s5_log_dt[l],
                   s5_d[l], s5_w_glu[l], s5_b_glu[l])
        pr_ctx = modulate(rms_norm(ctx, norm_mix_g[l]), csh_m, csc_m) @ w_in[l]
        pr_lat = modulate(rms_norm(x, norm_mix_g[l]), sh_m, sc_m) @ w_in[l]
        y5_ctx, s5_states = s5_mixer(pr_ctx[..., :S5_WIDTH], *s5_args, (None, None), ctx_out)
        yh_ctx, hg_states = hgrn_mixer(pr_ctx[..., HG_START:HG_END], lower_bounds[l], hg_norm_g[l],
                                       (None, None), ctx_out)
        y5_lat, _ = s5_mixer(pr_lat[..., :S5_WIDTH], *s5_args, s5_states, True)
        hg_in = grid_transpose(pr_lat[..., HG_START:HG_END], rows, GRID_W)
        yh_lat, _ = hgrn_mixer(hg_in, lower_bounds[l], hg_norm_g[l], hg_states, True)
        yh_lat = grid_transpose(yh_lat, GRID_W, rows)
        x = x + gt_m * merge_branches(pr_lat[..., HG_END:], y5_lat, yh_lat, w_branch_s5[l], w_branch_hg[l], w_out[l])
        if ctx_out:
            ctx = ctx + cgt_m * merge_branches(pr_ctx[..., HG_END:], y5_ctx, yh_ctx, w_branch_s5[l],
                                               w_branch_hg[l], w_out[l])
            ctx = ctx + cgt_f * peer_ffn(modulate(rms_norm(ctx, norm_ffn_g[l]), csh_f, csc_f),
                                         peer_w_q[l], peer_k1[l], peer_k2[l], peer_u[l], peer_v[l])
        x = x + gt_f * peer_ffn(modulate(rms_norm(x, norm_ffn_g[l]), sh_f, sc_f),
                                peer_w_q[l], peer_k1[l], peer_k2[l], peer_u[l], peer_v[l])
    return rms_norm(x, final_norm_g)
```

```python
import contextlib
import numpy as np
import ml_dtypes
import concourse.bass as bass
import concourse.mybir as mybir
from concourse.bass_utils import run_bass_kernel_spmd

F32 = mybir.dt.float32
BF16 = mybir.dt.bfloat16
U32 = mybir.dt.uint32
I32 = mybir.dt.int32
ALU = mybir.AluOpType
AF = mybir.ActivationFunctionType
AX = mybir.AxisListType

D = 2048
NB = 4
N_CORES = 16 // NB
CTX = 256
SEQ = 2048
TOK = CTX + SEQ
NT = NB * TOK
DEPTH = 4
EPS = 1e-6
PW = 10240
NKEY = 128
NEXP = 16384
TWO_PI = 6.283185307179586
PI = 3.141592653589793


class Buf:
    __slots__ = ("name", "w", "r")

    def __init__(self, name=""):
        self.name = name
        self.w = {}
        self.r = {}


class EngW:
    def __init__(self, nc, eng, name, es):
        self.eng = eng
        self.name = name
        self.sem = es.enter_context(nc.semaphore("s_" + name))
        self.key = "E" + name
        self.count = 0
        self.waited = {}


class DSem:
    def __init__(self, nc, name, es):
        self.sem = es.enter_context(nc.semaphore(name))
        self.key = "D" + name
        self.count = 0


class Tile:
    def __init__(self, h, buf, dsem=None):
        self.h = h
        self.buf = buf
        self.dsem = dsem

    def __getitem__(self, idx):
        return self.h[idx]


class K:
    def __init__(self, nc, es):
        self.nc = nc
        self.es = es
        self.pe = EngW(nc, nc.tensor, "pe", es)
        self.dve = EngW(nc, nc.vector, "dve", es)
        self.act = EngW(nc, nc.scalar, "act", es)
        self.pool = EngW(nc, nc.gpsimd, "pool", es)
        self.sp = EngW(nc, nc.sync, "sp", es)
        self.engs = [self.pe, self.dve, self.act, self.pool, self.sp]
        self.dsems = [DSem(nc, "d%d" % i, es) for i in range(48)]
        self.dfree = list(self.dsems)
        self.uid = 0
        self.ninst = 0

    def sb(self, st, shape, dtype, name=None, dma=False):
        self.uid += 1
        name = (name or "t") + "_%d" % self.uid
        h = st.enter_context(self.nc.sbuf_tensor(name, list(shape), dtype))
        ds = None
        if dma:
            ds = self.dfree.pop(0)
            st.callback(lambda d=ds: self.dfree.append(d))
        return Tile(h, Buf(name), ds)

    def ps(self, st, shape, dtype=F32, name=None):
        self.uid += 1
        name = (name or "p") + "_%d" % self.uid
        h = st.enter_context(self.nc.psum_tensor(name, list(shape), dtype))
        return Tile(h, Buf(name))

    def _need(self, reads, writes):
        need = {}
        for t in reads:
            b = t.buf if isinstance(t, Tile) else t
            for k, (s, v) in b.w.items():
                if k not in need or need[k][1] < v:
                    need[k] = (s, v)
        for t in writes:
            b = t.buf if isinstance(t, Tile) else t
            for dd in (b.w, b.r):
                for k, (s, v) in dd.items():
                    if k not in need or need[k][1] < v:
                        need[k] = (s, v)
        return need

    def _wait(self, E, need):
        for k, (s, v) in need.items():
            if k == E.key and E.name in ("pe", "sp"):
                continue
            if E.waited.get(k, 0) >= v:
                continue
            E.eng.wait_ge(s, v)
            E.waited[k] = v

    def _mark(self, reads, writes, key, sem, val, accumulate=False):
        for t in reads:
            b = t.buf if isinstance(t, Tile) else t
            b.r[key] = (sem, val)
        for t in writes:
            b = t.buf if isinstance(t, Tile) else t
            if accumulate:
                b.w[key] = (sem, val)
            else:
                b.w = {key: (sem, val)}
                b.r = {}

    def op(self, E, fn, reads=(), writes=()):
        self._wait(E, self._need(reads, writes))
        ins = fn(E.eng)
        E.count += 1
        ins.then_inc(E.sem, 1)
        self._mark(reads, writes, E.key, E.sem, E.count)
        self.ninst += 1
        return ins

    def dma(self, Q, pairs, sem_tile, reads=(), writes=(), accumulate=False, **kw):
        ds = sem_tile.dsem if isinstance(sem_tile, Tile) else sem_tile
        self._wait(Q, self._need(reads, writes))
        for (o, i) in pairs:
            Q.eng.dma_start(out=o, in_=i, **kw).then_inc(ds.sem, 16)
            ds.count += 16
            self.ninst += 1
        self._mark(reads, writes, ds.key, ds.sem, ds.count, accumulate=accumulate)

    @contextlib.contextmanager
    def scope(self):
        with contextlib.ExitStack() as st:
            yield st
            self.barrier()

    def tt(self, E, o, a, b, op, R, W):
        return self.op(E, lambda e: e.tensor_tensor(out=o, in0=a, in1=b, op=op), R, W)

    def ts(self, E, o, a, s1, s2, op0, op1, R, W):
        if s2 is None:
            return self.op(E, lambda e: e.tensor_scalar(out=o, in0=a, scalar1=s1, scalar2=None, op0=op0), R, W)
        return self.op(E, lambda e: e.tensor_scalar(out=o, in0=a, scalar1=s1, scalar2=s2, op0=op0, op1=op1), R, W)

    def stt(self, o, a, s, b, op0, op1, R, W):
        return self.op(self.dve, lambda e: e.scalar_tensor_tensor(out=o, in0=a, scalar=s, in1=b, op0=op0, op1=op1), R, W)

    def actf(self, o, a, func, R, W, bias=None, scale=None):
        kw = {}
        if bias is not None:
            kw["bias"] = bias
        if scale is not None:
            kw["scale"] = scale
        return self.op(self.act, lambda e: e.activation(out=o, in_=a, func=func, **kw), R, W)

    def cp(self, E, o, a, R, W):
        if E is self.act:
            return self.op(E, lambda e: e.copy(out=o, in_=a), R, W)
        return self.op(E, lambda e: e.tensor_copy(out=o, in_=a), R, W)

    def mm(self, o, lhsT, rhs, start, stop, R, W):
        return self.op(self.pe, lambda e: e.matmul(o, lhsT=lhsT, rhs=rhs, start=start, stop=stop), R, W)

    def barrier(self):
        for E in self.engs:
            for F in self.engs:
                if F is E or F.count == 0:
                    continue
                if E.waited.get(F.key, 0) >= F.count:
                    continue
                E.eng.wait_ge(F.sem, F.count)
                E.waited[F.key] = F.count
            for d in self.dsems:
                if d.count == 0 or E.waited.get(d.key, 0) >= d.count:
                    continue
                E.eng.wait_ge(d.sem, d.count)
                E.waited[d.key] = d.count


class Prog:
    def __init__(self, dbg=None):
        self.dbg = dbg or {}
        self.nc = bass.Bass("TRN2", target_bir_lowering=False)
        self.es = contextlib.ExitStack()
        self.k = K(self.nc, self.es)
        self.dram = {}
        self.dbuf = {}

    def din(self, name, shape, dtype=F32):
        t = self.nc.dram_tensor(name, list(shape), dtype, kind="ExternalInput").ap()
        self.dram[name] = t
        self.dbuf[name] = Buf(name)
        return t

    def dout(self, name, shape, dtype=F32):
        t = self.nc.dram_tensor(name, list(shape), dtype, kind="ExternalOutput").ap()
        self.dram[name] = t
        self.dbuf[name] = Buf(name)
        return t

    def dscr(self, name, shape, dtype=F32):
        kind = "ExternalOutput" if name in self.dbg.get("dump", ()) else "Internal"
        t = self.nc.dram_tensor(name, list(shape), dtype, kind=kind).ap()
        self.dram[name] = t
        self.dbuf[name] = Buf(name)
        return t

    def dump(self, name, tile, ap, shape, dtype=F32):
        if name not in self.dbg.get("dumps", ()) or ("dbg_" + name) in self.dbg.get("dumped", []):
            return
        k = self.k
        t = self.nc.dram_tensor("dbg_" + name, list(shape), dtype, kind="ExternalOutput").ap()
        if not hasattr(self, "dbg_ds"):
            self.dbg_ds = k.dfree.pop()
            self.dbg_buf = Buf("dbg")
        k.dma(k.sp, [(t, ap)], self.dbg_ds, reads=[tile], writes=[self.dbg_buf], accumulate=True)
        self.dbg.setdefault("dumped", []).append("dbg_" + name)

    def consts(self, st):
        k = self.k
        c = self.dram["cst"]
        self.ident_f = k.sb(st, [128, 128], F32, "identf", dma=True)
        self.ident_b = k.sb(st, [128, 128], BF16, "identb")
        self.ones_f = k.sb(st, [1, 512], F32, "onesf")
        self.ones_b = k.sb(st, [1, 512], BF16, "onesb")
        k.dma(k.sp, [(self.ident_f[:], c[0])], self.ident_f, reads=[self.dbuf["cst"]], writes=[self.ident_f])
        k.op(k.dve, lambda e: e.tensor_copy(out=self.ident_b[:], in_=self.ident_f[:]), [self.ident_f], [self.ident_b])
        k.op(k.dve, lambda e: e.memset(self.ones_f[:], 1.0), [], [self.ones_f])
        k.op(k.dve, lambda e: e.memset(self.ones_b[:], 1.0), [], [self.ones_b])

    def phase_init_x(self):
        k = self.k
        X = self.dram["X"]
        with self.k.scope() as st:
            ds = k.dfree[0]
            pairs = []
            for b in range(NB):
                pairs.append((X[b * TOK:b * TOK + CTX, :], self.dram["ctx"][b]))
                for q in range(4):
                    pairs.append((X[b * TOK + CTX + q * 512:b * TOK + CTX + (q + 1) * 512, :],
                                  self.dram["x"][b, q * 512:(q + 1) * 512, :]))
            k.dma(k.sp, pairs, ds, reads=[self.dbuf["x"], self.dbuf["ctx"]], writes=[self.dbuf["X"]], accumulate=True)
        k.barrier()

    def phase_silu_c(self, st):
        k = self.k
        self.LB = [k.sb(st, [128, 16, 128], BF16, "LB") for _ in range(NB + 1)]
        with self.k.scope() as s2:
            rows = []
            for b in range(NB + 1):
                r = k.sb(s2, [1, D], F32, "crow", dma=True)
                k.dma(k.sp, [(r[:], self.dram["cvec"][b:b + 1, :])], r, reads=[self.dbuf["cvec"]], writes=[r])
                rb = k.sb(s2, [1, D], BF16, "crowb")
                k.op(k.act, lambda e, r=r, rb=rb: e.activation(out=rb[:], in_=r[:], func=AF.Silu), [r], [rb])
                rows.append(rb)
            pss = [k.ps(s2, [128, 512]) for _ in range(2)]
            n = 0
            for b in range(NB + 1):
                lb = self.LB[b]
                for q in range(4):
                    p = pss[n % 2]
                    n += 1
                    for j in range(4):
                        kc = q * 4 + j
                        k.op(k.pe, lambda e, p=p, j=j, kc=kc, b=b: e.matmul(
                            p[:, j * 128:(j + 1) * 128], lhsT=rows[b][0:1, kc * 128:(kc + 1) * 128],
                            rhs=self.ones_b[0:1, 0:128], start=True, stop=True), [rows[b], self.ones_b], [p])
                    k.op(k.dve, lambda e, p=p, lb=lb, q=q: e.tensor_copy(
                        out=lb[:, q * 4:(q + 1) * 4, :], in_=p[:].rearrange("p (a b) -> p a b", a=4)), [p], [lb])
            k.barrier()

    def phase_mods(self, l):
        k = self.k
        wada = self.dram["w_ada"][l].rearrange("(kc p) n -> p kc n", p=128)
        MODS = self.dram["MODS"]
        with self.k.scope() as st:
            self.phase_silu_c(st)
            brow = k.sb(st, [1, 6 * D], F32, "brow", dma=True)
            k.dma(k.sp, [(brow[:], self.dram["b_ada"][l:l + 1, :])], brow, reads=[self.dbuf["b_ada"]], writes=[brow])
            grow = k.sb(st, [1, 2 * D], F32, "grow", dma=True)
            k.dma(k.sp, [(grow[:, 0:D], self.dram["norm_mix_g"][l:l + 1, :]),
                         (grow[:, D:2 * D], self.dram["norm_ffn_g"][l:l + 1, :])], grow,
                  reads=[self.dbuf["norm_mix_g"]], writes=[grow])
            G = k.sb(st, [128, 2 * D], F32, "G")
            pss = [k.ps(st, [128, 512]) for _ in range(4)]
            for q in range(8):
                p = pss[q % 4]
                k.op(k.pe, lambda e, p=p, q=q: e.matmul(p[:], lhsT=self.ones_f[0:1, 0:128],
                                                       rhs=grow[0:1, q * 512:(q + 1) * 512], start=True, stop=True),
                     [grow, self.ones_f], [p])
                k.op(k.act, lambda e, p=p, q=q: e.copy(out=G[:, q * 512:(q + 1) * 512], in_=p[:]), [p], [G])
            was = [k.sb(st, [128, 16, 512], BF16, "WA", dma=True) for _ in range(2)]
            stg = [k.sb(st, [128, 512], F32, "stg", dma=True) for _ in range(4)]
            n = 0
            for nci in range(24):
                wa = was[nci % 2]
                k.dma(k.pool, [(wa[:, 0:8, :], wada[:, 0:8, nci * 512:(nci + 1) * 512]),
                               (wa[:, 8:16, :], wada[:, 8:16, nci * 512:(nci + 1) * 512])], wa,
                      reads=[self.dbuf["w_ada"]], writes=[wa])
                seg = nci // 4
                col = (nci % 4) * 512
                for b in range(NB + 1):
                    p = pss[n % 4]
                    sg = stg[n % 4]
                    n += 1
                    for kc in range(16):
                        k.op(k.pe, lambda e, p=p, kc=kc, b=b, wa=wa: e.matmul(
                            p[:], lhsT=self.LB[b][:, kc, :], rhs=wa[:, kc, :], start=(kc == 0), stop=False),
                            [self.LB[b], wa], [p])
                    k.op(k.pe, lambda e, p=p, nci=nci: e.matmul(
                        p[:], lhsT=self.ones_f[0:1, 0:128], rhs=brow[0:1, nci * 512:(nci + 1) * 512],
                        start=False, stop=True), [brow, self.ones_f], [p])
                    if seg in (1, 4):
                        g0 = (0 if seg == 1 else D) + col
                        k.op(k.dve, lambda e, p=p, sg=sg, g0=g0: e.scalar_tensor_tensor(
                            out=sg[:], in0=p[:], scalar=1.0, in1=G[:, g0:g0 + 512], op0=ALU.add, op1=ALU.mult),
                            [p, G], [sg])
                    else:
                        k.op(k.act, lambda e, p=p, sg=sg: e.copy(out=sg[:], in_=p[:]), [p], [sg])
                    k.dma(k.sp, [(MODS[b, :, nci * 512:(nci + 1) * 512], sg[:])], sg,
                          reads=[sg], writes=[self.dbuf["MODS"]], accumulate=True)
        k.barrier()

    def norm_mod_tile(self, st_tiles, xin, A, SH, xn_out):
        k = self.k
        junk, ss, rs, tmp = st_tiles
        k.op(k.act, lambda e: e.activation(out=junk[:], in_=xin[:], func=AF.Square, accum_out=ss[:]), [xin], [junk, ss])
        k.op(k.dve, lambda e: e.tensor_scalar(out=rs[:], in0=ss[:], scalar1=1.0 / D, scalar2=EPS, op0=ALU.mult, op1=ALU.add),
             [ss], [rs])
        k.op(k.act, lambda e: e.activation(out=rs[:], in_=rs[:], func=AF.Sqrt), [rs], [rs])
        k.op(k.dve, lambda e: e.reciprocal(out=rs[:], in_=rs[:]), [rs], [rs])
        k.op(k.dve, lambda e: e.scalar_tensor_tensor(out=tmp[:], in0=xin[:], scalar=rs[:, 0:1], in1=A[:],
                                                     op0=ALU.mult, op1=ALU.mult), [xin, rs, A], [tmp])
        k.op(k.pool, lambda e: e.tensor_tensor(out=xn_out[:], in0=tmp[:], in1=SH[:], op=ALU.add), [tmp, SH], [xn_out])

    def phase_win(self, l):
        k = self.k
        X = self.dram["X"]
        MODS = self.dram["MODS"]
        win = self.dram["w_in"][l].rearrange("(kc p) n -> p kc n", p=128)
        UT, QFT, VOG, GT = self.dram["UT"], self.dram["QFT"], self.dram["VOG"], self.dram["GT"]
        for b in range(NB):
            with self.k.scope() as st:
                XT = k.sb(st, [128, 16, TOK], BF16, "XT")
                pss = [k.ps(st, [128, 512]) for _ in range(4)]
                with self.k.scope() as s1:
                    AM = [k.sb(s1, [128, D], F32, "AM", dma=True) for _ in range(2)]
                    SM = [k.sb(s1, [128, D], F32, "SM", dma=True) for _ in range(2)]
                    for j, bb in enumerate((NB, b)):
                        k.dma(k.sp, [(AM[j][:], MODS[bb, :, D:2 * D])], AM[j], reads=[self.dbuf["MODS"]], writes=[AM[j]])
                        k.dma(k.sp, [(SM[j][:], MODS[bb, :, 0:D])], SM[j], reads=[self.dbuf["MODS"]], writes=[SM[j]])
                    xins = [k.sb(s1, [128, D], F32, "xin", dma=True) for _ in range(2)]
                    junk = k.sb(s1, [128, D], BF16, "junk")
                    tmp = k.sb(s1, [128, D], F32, "tmp")
                    xns = [k.sb(s1, [128, D], BF16, "xn") for _ in range(2)]
                    sss = [k.sb(s1, [128, 1], F32, "ss") for _ in range(2)]
                    rss = [k.sb(s1, [128, 1], F32, "rs") for _ in range(2)]
                    for i in range(TOK // 128):
                        xin = xins[i % 2]
                        r0 = b * TOK + i * 128
                        k.dma(k.sp, [(xin[:], X[r0:r0 + 128, :])], xin, reads=[self.dbuf["X"]], writes=[xin])
                        j = 0 if i < CTX // 128 else 1
                        xn = xns[i % 2]
                        self.norm_mod_tile((junk, sss[i % 2], rss[i % 2], tmp), xin, AM[j], SM[j], xn)
                        for q in range(4):
                            p = pss[q]
                            for jj in range(4):
                                kc = q * 4 + jj
                                k.op(k.pe, lambda e, p=p, jj=jj, kc=kc, xn=xn: e.matmul(
                                    p[:, jj * 128:(jj + 1) * 128], lhsT=xn[:, kc * 128:(kc + 1) * 128],
                                    rhs=self.ident_b[:], start=True, stop=True), [xn, self.ident_b], [p])
                            E = k.act if q % 2 == 0 else k.dve
                            if E is k.act:
                                k.op(E, lambda e, p=p, q=q, i=i: e.copy(
                                    out=XT[:, q * 4:(q + 1) * 4, i * 128:(i + 1) * 128],
                                    in_=p[:].rearrange("p (a b) -> p a b", a=4)), [p], [XT])
                            else:
                                k.op(E, lambda e, p=p, q=q, i=i: e.tensor_copy(
                                    out=XT[:, q * 4:(q + 1) * 4, i * 128:(i + 1) * 128],
                                    in_=p[:].rearrange("p (a b) -> p a b", a=4)), [p], [XT])
                ws = [k.sb(st, [128, 16, 512], BF16, "W", dma=True) for _ in range(2)]
                stg = [k.sb(st, [128, 512], F32, "stg", dma=True) for _ in range(4)]
                n = 0
                blocks = [(0, 256)] + [(256 + q * 512, 512) for q in range(4)]
                for ci in range(20):
                    w = ws[ci % 2]
                    c0 = ci * 512
                    k.dma(k.pool, [(w[:, 0:8, :], win[:, 0:8, c0:c0 + 512]), (w[:, 8:16, :], win[:, 8:16, c0:c0 + 512])],
                          w, reads=[self.dbuf["w_in"]], writes=[w])
                    if ci in (4, 5):
                        dcol = c0 - 2048
                        for i in range(TOK // 128):
                            p = pss[n % 4]
                            sg = stg[n % 4]
                            n += 1
                            for kc in range(16):
                                k.op(k.pe, lambda e, p=p, kc=kc, i=i, w=w: e.matmul(
                                    p[:], lhsT=XT[:, kc, i * 128:(i + 1) * 128], rhs=w[:, kc, :],
                                    start=(kc == 0), stop=(kc == 15)), [XT, w], [p])
                            if n % 2 == 0:
                                k.op(k.act, lambda e, p=p, sg=sg: e.copy(out=sg[:], in_=p[:]), [p], [sg])
                            else:
                                k.op(k.dve, lambda e, p=p, sg=sg: e.tensor_copy(out=sg[:], in_=p[:]), [p], [sg])
                            r0 = b * TOK + i * 128
                            k.dma(k.sp, [(VOG[r0:r0 + 128, dcol:dcol + 512], sg[:])], sg, reads=[sg],
                                  writes=[self.dbuf["VOG"]], accumulate=True)
                    else:
                        if ci < 2:
                            dst, drow, sig = UT, c0, False
                        elif ci < 4:
                            dst, drow, sig = QFT, c0 - 1024, False
                        elif ci < 8:
                            dst, drow, sig = QFT, 1024 + c0 - 3072, False
                        elif ci < 10:
                            dst, drow, sig = QFT, 2048 + c0 - 4096, False
                        elif ci < 12:
                            dst, drow, sig = QFT, 3072 + c0 - 5120, False
                        else:
                            dst, drow, sig = GT, c0 - 6144, True
                        dname = {id(UT): "UT", id(QFT): "QFT", id(GT): "GT"}[id(dst)]
                        for sub in range(4):
                            for (t0, tn) in blocks:
                                p = pss[n % 4]
                                sg = stg[n % 4]
                                n += 1
                                for kc in range(16):
                                    k.op(k.pe, lambda e, p=p, kc=kc, w=w, sub=sub, t0=t0, tn=tn: e.matmul(
                                        p[:, 0:tn], lhsT=w[:, kc, sub * 128:(sub + 1) * 128], rhs=XT[:, kc, t0:t0 + tn],
                                        start=(kc == 0), stop=(kc == 15)), [XT, w], [p])
                                if sig:
                                    k.op(k.act, lambda e, p=p, sg=sg, tn=tn: e.activation(
                                        out=sg[:, 0:tn], in_=p[:, 0:tn], func=AF.Sigmoid), [p], [sg])
                                elif n % 2 == 0:
                                    k.op(k.act, lambda e, p=p, sg=sg, tn=tn: e.copy(out=sg[:, 0:tn], in_=p[:, 0:tn]), [p], [sg])
                                else:
                                    k.op(k.dve, lambda e, p=p, sg=sg, tn=tn: e.tensor_copy(out=sg[:, 0:tn], in_=p[:, 0:tn]),
                                         [p], [sg])
                                rr = drow + sub * 128
                                k.dma(k.sp, [(dst[rr:rr + 128, b * TOK + t0:b * TOK + t0 + tn], sg[:, 0:tn])], sg,
                                      reads=[sg], writes=[self.dbuf[dname]], accumulate=True)
            k.barrier()


    def ang_reduce(self, xT, xap, kiT, kiap, kfT, kfap):
        k = self.k
        k.ts(k.dve, kiap, xap, 1.0 / TWO_PI, None, ALU.mult, None, [xT], [kiT])
        k.cp(k.dve, kfap, kiap, [kiT], [kfT])
        k.stt(xap, kfap, -TWO_PI, xap, ALU.mult, ALU.add, [kfT, xT], [xT])
        k.ts(k.dve, xap, xap, -3.14159, 3.14159, ALU.max, ALU.min, [xT], [xT])

    def s5_params(self, l, st):
        k = self.k
        prm = []
        for d in range(2):
            prm.append(dict(BT=k.sb(st, [128, 2, 8, 128], BF16, "BTb"), BT3=k.sb(st, [128, 2, 8, 128], BF16, "BT3"),
                            CT=k.sb(st, [128, 2, 32, 64], BF16, "CTb"),
                            R=k.sb(st, [128, 32], F32, "RSL"), TH=k.sb(st, [128, 32], F32, "THR")))
        msk = k.sb(st, [128, 4], F32, "msk", dma=True)
        k.dma(k.sp, [(msk[:], self.dram["cst3"][:, :])], msk, reads=[self.dbuf["cst3"]], writes=[msk])
        with self.k.scope() as s2:
            for d in range(2):
                P = prm[d]
                sl = k.sb(s2, [128, 3, 32], F32, "sl", dma=True)
                bl = k.sb(s2, [128, 3, 512], F32, "bl", dma=True)
                bt = k.sb(s2, [128, 2, 8, 128], F32, "bt", dma=True)
                ct = k.sb(s2, [128, 2, 32, 32], F32, "ct", dma=True)
                k.dma(k.sp, [(sl[:], self.dram["s5sl"][l, d])], sl, reads=[self.dbuf["s5sl"]], writes=[sl])
                k.dma(k.sp, [(bl[:], self.dram["s5bl"][l, d])], bl, reads=[self.dbuf["s5bl"]], writes=[bl])
                k.dma(k.sp, [(bt[:], self.dram["s5bt"][l, d])], bt, reads=[self.dbuf["s5bt"]], writes=[bt])
                k.dma(k.sp, [(ct[:], self.dram["s5ct"][l, d])], ct, reads=[self.dbuf["s5ct"]], writes=[ct])
                k.op(k.pool, lambda e, P=P: e.memset(P["CT"][:], 0.0), [], [P["CT"]])
                k.cp(k.pool, P["CT"][:, :, :, 32:64], ct[:], [ct], [P["CT"]])
                w1 = k.sb(s2, [128, 32], F32, "w1")
                wi = k.sb(s2, [128, 32], I32, "wi")
                wf = k.sb(s2, [128, 32], F32, "wf")
                k.actf(w1[:], sl[:, 2, :], AF.Exp, [sl], [w1])
                k.tt(k.dve, P["TH"][:], sl[:, 1, :], w1[:], ALU.mult, [sl, w1], [P["TH"]])
                k.tt(k.dve, w1[:], sl[:, 0, :], w1[:], ALU.mult, [sl, w1], [w1])
                k.actf(P["R"][:], w1[:], AF.Exp, [w1], [P["R"]])
                self.ang_reduce(P["TH"], P["TH"][:], wi, wi[:], wf, wf[:])
                N = 512
                dt = k.sb(s2, [128, N], F32, "dt")
                r = k.sb(s2, [128, N], F32, "r")
                th = k.sb(s2, [128, N], F32, "th")
                th2 = k.sb(s2, [128, N], F32, "th2")
                ki = k.sb(s2, [128, N], I32, "ki")
                kf = k.sb(s2, [128, N], F32, "kf")
                sn = k.sb(s2, [128, N], F32, "sn")
                cs = k.sb(s2, [128, N], F32, "cs")
                t1 = k.sb(s2, [128, N], F32, "t1")
                t2 = k.sb(s2, [128, N], F32, "t2")
                cR = k.sb(s2, [128, N], F32, "cR")
                cI = k.sb(s2, [128, N], F32, "cI")
                are, aim = bl[:, 0, :], bl[:, 1, :]
                k.actf(dt[:], bl[:, 2, :], AF.Exp, [bl], [dt])
                k.tt(k.dve, th[:], aim, dt[:], ALU.mult, [bl, dt], [th])
                k.tt(k.dve, dt[:], are, dt[:], ALU.mult, [bl, dt], [dt])
                k.actf(r[:], dt[:], AF.Exp, [dt], [r])
                self.ang_reduce(th, th[:], ki, ki[:], kf, kf[:])
                k.ts(k.dve, th2[:], th[:], PI / 2, None, ALU.add, None, [th], [th2])
                self.ang_reduce(th2, th2[:], ki, ki[:], kf, kf[:])
                k.actf(sn[:], th[:], AF.Sin, [th], [sn])
                k.actf(cs[:], th2[:], AF.Sin, [th2], [cs])
                k.tt(k.dve, cs[:], r[:], cs[:], ALU.mult, [r, cs], [cs])
                k.ts(k.dve, cs[:], cs[:], -1.0, None, ALU.add, None, [cs], [cs])
                k.tt(k.dve, sn[:], r[:], sn[:], ALU.mult, [r, sn], [sn])
                k.tt(k.dve, t1[:], are, are, ALU.mult, [bl], [t1])
                k.tt(k.dve, t2[:], aim, aim, ALU.mult, [bl], [t2])
                k.tt(k.dve, t1[:], t1[:], t2[:], ALU.add, [t1, t2], [t1])
                k.op(k.dve, lambda e: e.reciprocal(out=t1[:], in_=t1[:]), [t1], [t1])
                k.tt(k.dve, cR[:], cs[:], are, ALU.mult, [cs, bl], [cR])
                k.tt(k.dve, t2[:], sn[:], aim, ALU.mult, [sn, bl], [t2])
                k.tt(k.dve, cR[:], cR[:], t2[:], ALU.add, [cR, t2], [cR])
                k.tt(k.dve, cR[:], cR[:], t1[:], ALU.mult, [cR, t1], [cR])
                k.tt(k.dve, cI[:], sn[:], are, ALU.mult, [sn, bl], [cI])
                k.tt(k.dve, t2[:], cs[:], aim, ALU.mult, [cs, bl], [t2])
                k.tt(k.dve, cI[:], cI[:], t2[:], ALU.subtract, [cI, t2], [cI])
                k.tt(k.dve, cI[:], cI[:], t1[:], ALU.mult, [cI, t1], [cI])
                bc = lambda t: t[:].rearrange("p (k q) -> p k q", k=8).unsqueeze(2).broadcast_to([128, 8, 2, 64])
                v4 = lambda ap: ap.rearrange("p k (s q) -> p k s q", s=2)
                u1 = k.sb(s2, [128, 8, 128], F32, "u1")
                u2 = k.sb(s2, [128, 8, 128], F32, "u2")
                k.tt(k.dve, v4(u1[:]), v4(bt[:, 0]), bc(cR), ALU.mult, [bt, cR], [u1])
                k.tt(k.dve, v4(u2[:]), v4(bt[:, 1]), bc(cI), ALU.mult, [bt, cI], [u2])
                k.tt(k.dve, P["BT"][:, 0], u1[:], u2[:], ALU.subtract, [u1, u2], [P["BT"]])
                k.tt(k.dve, v4(u1[:]), v4(bt[:, 1]), bc(cR), ALU.mult, [bt, cR], [u1])
                k.tt(k.dve, v4(u2[:]), v4(bt[:, 0]), bc(cI), ALU.mult, [bt, cI], [u2])
                k.tt(k.dve, P["BT"][:, 1], u1[:], u2[:], ALU.add, [u1, u2], [P["BT"]])
                k.ts(k.dve, P["BT3"][:], P["BT"][:], msk[:, 0:1], None, ALU.mult, None, [P["BT"], msk], [P["BT3"]])
                if d == 0:
                    self.dump("R", P["R"], P["R"][:], [128, 32])
                    self.dump("TH", P["TH"], P["TH"][:], [128, 32])
                    self.dump("cR", cR, cR[:], [128, 512])
                    self.dump("cI", cI, cI[:], [128, 512])
                    self.dump("BT", P["BT"], P["BT"][:], [128, 2, 8, 128], BF16)
                    self.dump("CT", P["CT"], P["CT"][:], [128, 2, 32, 64], BF16)
        return prm

    def phase_s5(self, l):
        k = self.k
        UT, Y5T = self.dram["UT"], self.dram["Y5T"]
        with self.k.scope() as st:
            prm = self.s5_params(l, st)
            iot = k.sb(st, [128, 513], F32, "iota", dma=True)
            k.dma(k.sp, [(iot[:], self.dram["cst2"][:, :])], iot, reads=[self.dbuf["cst2"]], writes=[iot])
            dsk = k.sb(st, [128, 8], F32, "dsk", dma=True)
            bgl = k.sb(st, [128, 8], F32, "bgl", dma=True)
            k.dma(k.sp, [(dsk[:], self.dram["s5d_l"][l])], dsk, reads=[self.dbuf["s5d_l"]], writes=[dsk])
            k.dma(k.sp, [(bgl[:], self.dram["bglu_l"][l])], bgl, reads=[self.dbuf["bglu_l"]], writes=[bgl])
            WG = None
            for b in range(NB):
                with self.k.scope() as sb_:
                    Y = k.sb(sb_, [128, 8, TOK], F32, "Y")
                    uTb = k.sb(sb_, [128, 8, TOK], BF16, "uTb", dma=True)
                    k.dma(k.pool, [(uTb[:, kk, :], UT[kk * 128:(kk + 1) * 128, b * TOK:(b + 1) * TOK]) for kk in range(8)],
                          uTb, reads=[self.dbuf["UT"]], writes=[uTb], max_dma_last_dim=4096)
                    with self.k.scope() as sc:
                        self.s5_scan(sc, prm, iot, Y, uTb)
                    if b == 1:
                        self.dump("Y", Y, Y[:], [128, 8, TOK])
                    self.s5_glu(sb_, l, b, Y, uTb, dsk, bgl, WG)
        k.barrier()

    def s5_scan(self, sc, prm, iot, Y, uTb):
        k = self.k
        SIN = [k.sb(sc, [128, 513], F32, "SIN") for _ in range(2)]
        COS = [k.sb(sc, [128, 513], F32, "COS") for _ in range(2)]
        ang = k.sb(sc, [128, 513], F32, "ang")
        ki = k.sb(sc, [128, 513], I32, "aki")
        kf = k.sb(sc, [128, 513], F32, "akf")
        XR = [k.ps(sc, [128, 512]) for _ in range(2)]
        XI = [k.ps(sc, [128, 512]) for _ in range(2)]
        YP = [k.ps(sc, [128, 512]) for _ in range(2)]
        t1 = k.sb(sc, [128, 512], F32, "m1")
        t2 = k.sb(sc, [128, 512], F32, "m2")
        t3 = k.sb(sc, [128, 512], F32, "m3")
        t4 = k.sb(sc, [128, 512], F32, "m4")
        xr = [k.sb(sc, [128, 512], F32, "xr") for _ in range(2)]
        xi = [k.sb(sc, [128, 512], F32, "xi") for _ in range(2)]
        gR = [k.sb(sc, [128, 512], F32, "gR") for _ in range(2)]
        gI = [k.sb(sc, [128, 512], F32, "gI") for _ in range(2)]
        hR = [k.sb(sc, [128, 512], BF16, "hR") for _ in range(2)]
        hI = [k.sb(sc, [128, 512], BF16, "hI") for _ in range(2)]
        ini = [k.sb(sc, [128, 2], F32, "ini") for _ in range(2)]
        us = k.sb(sc, [128, 2], F32, "us")
        blocks_f = [(0, 256)] + [(256 + q * 512, 512) for q in range(4)]
        blocks_b = [(0, 256)] + [(256 + q * 512, 512) for q in (3, 2, 1, 0)]
        n_unit = 0
        n_tab = 0
        for d in range(2):
            P = prm[d]
            for j in [4 * a + b_ for a in range(8) for b_ in (3, 2, 1, 0)]:
                kk, jl = j // 4, j % 4
                rows = slice(32 * jl, 32 * jl + 32) if jl < 3 else slice(64, 128)
                BTt = P["BT"] if jl < 3 else P["BT3"]
                ccols = slice(32, 64) if jl < 3 else slice(0, 64)
                S, C = SIN[n_tab % 2], COS[n_tab % 2]
                n_tab += 1
                k.ts(k.dve, ang[:], iot[:], P["TH"][:, j:j + 1], None, ALU.mult, None, [iot, P["TH"]], [ang])
                self.ang_reduce(ang, ang[:], ki, ki[:], kf, kf[:])
                k.actf(S[:], ang[:], AF.Sin, [ang], [S])
                k.ts(k.dve, ang[:], ang[:], PI / 2, None, ALU.add, None, [ang], [ang])
                self.ang_reduce(ang, ang[:], ki, ki[:], kf, kf[:])
                k.actf(C[:], ang[:], AF.Sin, [ang], [C])
                if d == 0 and j == 3:
                    self.dump("SIN", S, S[:], [128, 513])
                    self.dump("COS", C, C[:], [128, 513])
                first = True
                for (t0, n) in (blocks_f if d == 0 else blocks_b):
                    u = n_unit % 2
                    n_unit += 1
                    if d == 0:
                        cols = slice(t0, t0 + n)
                    else:
                        cols = slice(t0 + n - 1, (t0 - 1) if t0 > 0 else None, -1)
                    k.mm(XR[u][:, 0:n], BTt[rows, 0, kk, :], uTb[rows, kk, cols], True, True, [BTt, uTb], [XR[u]])
                    k.mm(XI[u][:, 0:n], BTt[rows, 1, kk, :], uTb[rows, kk, cols], True, True, [BTt, uTb], [XI[u]])
                    k.tt(k.dve, t1[:, 0:n], XR[u][:, 0:n], C[:, 0:n], ALU.mult, [XR[u], C], [t1])
                    k.tt(k.dve, t2[:, 0:n], XI[u][:, 0:n], S[:, 0:n], ALU.mult, [XI[u], S], [t2])
                    k.tt(k.dve, t3[:, 0:n], XI[u][:, 0:n], C[:, 0:n], ALU.mult, [XI[u], C], [t3])
                    k.tt(k.dve, t4[:, 0:n], XR[u][:, 0:n], S[:, 0:n], ALU.mult, [XR[u], S], [t4])
                    k.tt(k.pool, xr[u][:, 0:n], t1[:, 0:n], t2[:, 0:n], ALU.add, [t1, t2], [xr[u]])
                    k.tt(k.pool, xi[u][:, 0:n], t3[:, 0:n], t4[:, 0:n], ALU.subtract, [t3, t4], [xi[u]])
                    rb = P["R"][:, j:j + 1].broadcast_to([128, n])
                    iv = ini[0]
                    i0 = 0.0 if first else iv[:, 0:1]
                    i1 = 0.0 if first else iv[:, 1:2]
                    rd = [P["R"], xr[u]] + ([] if first else [iv])
                    k.op(k.dve, lambda e, u=u, n=n, rb=rb, i0=i0: e.tensor_tensor_scan(
                        out=gR[u][:, 0:n], data0=rb, data1=xr[u][:, 0:n], initial=i0, op0=ALU.mult, op1=ALU.add),
                        rd, [gR[u]])
                    rd = [P["R"], xi[u]] + ([] if first else [iv])
                    k.op(k.dve, lambda e, u=u, n=n, rb=rb, i1=i1: e.tensor_tensor_scan(
                        out=gI[u][:, 0:n], data0=rb, data1=xi[u][:, 0:n], initial=i1, op0=ALU.mult, op1=ALU.add),
                        rd, [gI[u]])
                    first = False
                    cT, sT = C[:, n:n + 1], S[:, n:n + 1]
                    k.ts(k.dve, us[:, 0:1], gI[u][:, n - 1:n], sT, None, ALU.mult, None, [gI[u], S], [us])
                    k.ts(k.dve, us[:, 1:2], gI[u][:, n - 1:n], cT, None, ALU.mult, None, [gI[u], C], [us])
                    k.stt(iv[:, 0:1], gR[u][:, n - 1:n], cT, us[:, 0:1], ALU.mult, ALU.subtract, [gR[u], C, us], [iv])
                    k.stt(iv[:, 1:2], gR[u][:, n - 1:n], sT, us[:, 1:2], ALU.mult, ALU.add, [gR[u], S, us], [iv])
                    k.tt(k.dve, t1[:, 0:n], gR[u][:, 0:n], C[:, 0:n], ALU.mult, [gR[u], C], [t1])
                    k.tt(k.pool, t2[:, 0:n], gI[u][:, 0:n], S[:, 0:n], ALU.mult, [gI[u], S], [t2])
                    k.tt(k.pool, hR[u][:, 0:n], t1[:, 0:n], t2[:, 0:n], ALU.subtract, [t1, t2], [hR[u]])
                    k.tt(k.pool, t3[:, 0:n], gR[u][:, 0:n], S[:, 0:n], ALU.mult, [gR[u], S], [t3])
                    k.tt(k.dve, t4[:, 0:n], gI[u][:, 0:n], C[:, 0:n], ALU.mult, [gI[u], C], [t4])
                    k.stt(hI[u][:, 0:n], t3[:, 0:n], -1.0, t4[:, 0:n], ALU.mult, ALU.subtract, [t3, t4], [hI[u]])
                    if d == 0 and j == 3 and t0 == 256:
                        self.dump("xr", xr[u], xr[u][:], [128, 512])
                        self.dump("gR", gR[u], gR[u][:], [128, 512])
                        self.dump("hR", hR[u], hR[u][:], [128, 512], BF16)
                    k.mm(YP[u][rows, 0:n], P["CT"][:, 0, j, ccols], hR[u][:, 0:n], True, False, [P["CT"], hR[u]], [YP[u]])
                    k.mm(YP[u][rows, 0:n], P["CT"][:, 1, j, ccols], hI[u][:, 0:n], False, True, [P["CT"], hI[u]], [YP[u]])
                    if d == 0:
                        k.cp(k.act, Y[rows, kk, t0:t0 + n], YP[u][rows, 0:n], [YP[u]], [Y])
                    else:
                        k.tt(k.dve, Y[rows, kk, t0:t0 + n], YP[u][rows, n - 1::-1], Y[rows, kk, t0:t0 + n], ALU.add,
                             [YP[u], Y], [Y])

    def s5_glu(self, sb_, l, b, Y, uTb, dsk, bgl, WG):
        k = self.k
        UT, Y5T = self.dram["UT"], self.dram["Y5T"]
        WG = k.sb(sb_, [128, 8, 1024], BF16, "WG", dma=True)
        k.dma(k.pool, [(WG[:], self.dram["s5_w_glu"][l].rearrange("(kc p) n -> p kc n", p=128))], WG,
              reads=[self.dbuf["s5_w_glu"]], writes=[WG])
        uf = [k.sb(sb_, [128, TOK], F32, "uf", dma=True) for _ in range(2)]
        z = uTb
        for kk in range(8):
            f = uf[kk % 2]
            k.dma(k.sp, [(f[:], UT[kk * 128:(kk + 1) * 128, b * TOK:(b + 1) * TOK])], f, reads=[self.dbuf["UT"]], writes=[f])
            k.stt(Y[:, kk, :], f[:], dsk[:, kk:kk + 1], Y[:, kk, :], ALU.mult, ALU.add, [f, dsk, Y], [Y])
            k.actf(z[:, kk, :], Y[:, kk, :], AF.Gelu_apprx_tanh, [Y], [z])
        pss = [k.ps(sb_, [128, 512]) for _ in range(2)]
        sg = [k.sb(sb_, [128, 512], F32, "sg") for _ in range(2)]
        ob = [k.sb(sb_, [128, 512], BF16, "ob", dma=True) for _ in range(2)]
        blocks = [(0, 256)] + [(256 + q * 512, 512) for q in range(4)]
        n_ = 0
        for oc in range(8):
            for (t0, n) in blocks:
                p = pss[n_ % 2]
                s_ = sg[n_ % 2]
                o = ob[n_ % 2]
                n_ += 1
                for kc in range(8):
                    k.mm(p[:, 0:n], WG[:, kc, oc * 128:(oc + 1) * 128], z[:, kc, t0:t0 + n], kc == 0, kc == 7, [WG, z], [p])
                k.actf(s_[:, 0:n], p[:, 0:n], AF.Sigmoid, [p, bgl], [s_], bias=bgl[:, oc:oc + 1])
                k.tt(k.dve, o[:, 0:n], s_[:, 0:n], z[:, oc, t0:t0 + n], ALU.mult, [s_, z], [o])
                k.dma(k.sp, [(Y5T[oc * 128:(oc + 1) * 128, b * TOK + t0:b * TOK + t0 + n], o[:, 0:n])], o,
                      reads=[o], writes=[self.dbuf["Y5T"]], accumulate=True)


    def phase_hg_lb(self, st):
        k = self.k
        self.LBT = k.sb(st, [128, DEPTH, 16], F32, "LBT")
        self.OML = k.sb(st, [128, DEPTH, 16], F32, "OML")
        with self.k.scope() as s2:
            gam = k.sb(s2, [128, DEPTH, 16], F32, "gam", dma=True)
            k.dma(k.sp, [(gam[:], self.dram["gam_l"][:, :, :])], gam, reads=[self.dbuf["gam_l"]], writes=[gam])
            e = k.sb(s2, [128, DEPTH, 16], F32, "ge")
            sm = k.sb(s2, [128, 16], F32, "gs")
            k.actf(e[:], gam[:], AF.Exp, [gam], [e])
            k.tt(k.dve, sm[:], e[:, 0, :], e[:, 1, :], ALU.add, [e], [sm])
            for i in range(2, DEPTH):
                k.tt(k.dve, sm[:], sm[:], e[:, i, :], ALU.add, [e, sm], [sm])
            k.op(k.dve, lambda en: en.reciprocal(out=sm[:], in_=sm[:]), [sm], [sm])
            k.op(k.dve, lambda en: en.memset(self.LBT[:], 0.0), [], [self.LBT])
            for i in range(1, DEPTH):
                k.tt(k.dve, e[:, i, :], e[:, i, :], sm[:], ALU.mult, [e, sm], [e])
                k.tt(k.dve, self.LBT[:, i, :], self.LBT[:, i - 1, :], e[:, i, :], ALU.add, [e, self.LBT], [self.LBT])
            k.ts(k.dve, self.OML[:], self.LBT[:], -1.0, 1.0, ALU.mult, ALU.add, [self.LBT], [self.OML])

    def phase_hgrn(self, l):
        k = self.k
        with self.k.scope() as st:
            ghg = k.sb(st, [128, 8], F32, "ghg", dma=True)
            k.dma(k.sp, [(ghg[:], self.dram["ghg_l"][l])], ghg, reads=[self.dbuf["ghg_l"]], writes=[ghg])
            pat = k.sb(st, [128, 32], F32, "pat")
            k.op(k.dve, lambda e: e.memset(pat[:], 1.0), [], [pat])
            k.op(k.dve, lambda e: e.memset(pat[:, 0:1], 0.0), [], [pat])
            mask = k.sb(st, [128, TOK + 32], F32, "mask")
            k.cp(k.dve, mask[:].rearrange("p (n s) -> p n s", s=32), pat[:].unsqueeze(1).broadcast_to([128, TOK // 32 + 1, 32]),
                 [pat], [mask])
            mk = k.sb(st, [64, 2, 64], F32, "mk", dma=True)
            k.dma(k.sp, [(mk[:, 0, :], self.dram["cst"][1, 0:64, 0:64]), (mk[:, 1, :], self.dram["cst"][2, 0:64, 0:64])], mk,
                  reads=[self.dbuf["cst"]], writes=[mk])
            onesq = k.sb(st, [128, 128], F32, "onesq")
            k.op(k.dve, lambda e: e.memset(onesq[:], 1.0), [], [onesq])
            for b in range(NB):
                for h in range(8):
                    if "hg_heads" in self.dbg and (b, h) not in self.dbg["hg_heads"]:
                        continue
                    with self.k.scope() as sh:
                        self.hg_head(sh, l, b, h, ghg, mask, mk, onesq)

    def hg_head(self, sh, l, b, h, ghg, mask, mk, onesq):
        k = self.k
        QFT, VOG, YHT = self.dram["QFT"], self.dram["VOG"], self.dram["YHT"]
        c0 = b * TOK
        NCH = TOK // 32
        NG = TOK // 64
        qraw = k.sb(sh, [128, TOK], F32, "qraw", dma=True)
        fraw = k.sb(sh, [128, TOK], F32, "fraw", dma=True)
        qP = k.sb(sh, [128, TOK], F32, "qP")
        sogP = k.sb(sh, [128, TOK], F32, "sogP")
        T1 = k.sb(sh, [128, TOK], F32, "T1")
        T2 = k.sb(sh, [128, TOK], F32, "T2")
        T3 = k.sb(sh, [128, TOK], F32, "T3")
        T4 = k.sb(sh, [128, TOK], F32, "T4")
        T5 = k.sb(sh, [128, TOK], F32, "T5")
        A = k.sb(sh, [128, TOK], BF16, "A")
        Bt = k.sb(sh, [128, TOK], BF16, "B")
        KD = k.sb(sh, [64, NG, 128], BF16, "KD")
        VT = k.sb(sh, [64, NG, 128], BF16, "VT", dma=True)
        QB = [k.sb(sh, [128, TOK], BF16, "QB") for _ in range(2)]
        SIN_ = [k.sb(sh, [128, NCH, 128], BF16, "Sin") for _ in range(2)]
        SC = [k.sb(sh, [64, NG, 64], BF16, "SC") for _ in range(2)]
        dec = k.sb(sh, [128, NCH], F32, "dec")
        S = k.sb(sh, [128, 128], F32, "S")
        YR = k.sb(sh, [128, TOK], BF16, "YR", dma=True)
        PT = [k.ps(sh, [128, 512]) for _ in range(2)]
        KV = [k.ps(sh, [128, 512]) for _ in range(4)]
        PO = [k.ps(sh, [128, 512]) for _ in range(2)]
        rows = slice(h * 128, (h + 1) * 128)
        k.dma(k.sp, [(qraw[:], QFT[rows, c0:c0 + TOK])], qraw, reads=[self.dbuf["QFT"]], writes=[qraw])
        k.dma(k.sp, [(fraw[:], QFT[3072 + h * 128:3072 + (h + 1) * 128, c0:c0 + TOK])], fraw, reads=[self.dbuf["QFT"]], writes=[fraw])
        vsrc = VOG[c0 + CTX:c0 + TOK, rows].rearrange("(r g c) d -> c r g d", g=32, c=2)
        k.dma(k.pool, [(VT[0:64, 0:4, :], VOG[c0:c0 + CTX, rows].rearrange("(g s) d -> s g d", s=64)),
                       (VT[0:32, 4:NG, :], vsrc[0]), (VT[32:64, 4:NG, :], vsrc[1])], VT,
              reads=[self.dbuf["VOG"]], writes=[VT])

        def toP(func, o, i):
            k.actf(o[:, 0:CTX], i[:, 0:CTX], func, [i], [o])
            k.actf(o[:, CTX:].rearrange("p (c r) -> p c r", r=32), i[:, CTX:].rearrange("p (r c) -> p c r", c=64), func, [i], [o])

        toP(AF.Silu, qP, qraw)
        toP(AF.Silu, sogP, fraw)
        segs = [(0, CTX), (CTX, TOK)]
        n_pt = 0
        n_kv = 0
        for d in range(2):
            col = d * 8 + h
            r0 = 1024 * (1 + d) + h * 128
            k.dma(k.sp, [(fraw[:], QFT[r0:r0 + 128, c0:c0 + TOK])], fraw, reads=[self.dbuf["QFT"]], writes=[fraw])
            toP(AF.Sigmoid, T1, fraw)
            k.ts(k.dve, T1[:], T1[:], self.OML[:, l, col:col + 1], self.LBT[:, l, col:col + 1], ALU.mult, ALU.add,
                 [T1, self.OML, self.LBT], [T1])
            k.actf(T2[:], T1[:], AF.Ln, [T1], [T2])
            k.ts(k.pool, T1[:], T1[:], -1.0, 1.0, ALU.mult, ALU.add, [T1], [T1])
            for (a, e_) in segs:
                sl = slice(a, e_) if d == 0 else slice(e_ - 1, (a - 1) if a > 0 else None, -1)
                slm = slice(a, e_) if d == 0 else slice(e_, a, -1)
                k.op(k.dve, lambda en, sl=sl, slm=slm: en.tensor_tensor_scan(out=T3[:, sl], data0=mask[:, slm], data1=T2[:, sl],
                                                                 initial=0.0, op0=ALU.mult, op1=ALU.add), [mask, T2], [T3])
            b3 = T3[:].rearrange("p (n s) -> p n s", s=32)
            iL, iR = (31, 15) if d == 0 else (0, 16)
            BL3 = b3[:, :, iL:iL + 1].broadcast_to([128, NCH, 32])
            BR3 = b3[:, :, iR:iR + 1].broadcast_to([128, NCH, 32])
            v3 = lambda t: t[:].rearrange("p (n s) -> p n s", s=32)
            k.tt(k.dve, v3(T4), BL3, b3, ALU.subtract, [T3], [T4])
            k.actf(T4[:], T4[:], AF.Exp, [T4], [T4])
            k.tt(k.pool, A[:], T1[:], T4[:], ALU.mult, [T1, T4], [A])
            k.actf(dec[:], b3[:, :, iL], AF.Exp, [T3], [dec])
            if d == self.dbg.get("hg_dd", 0):
                self.dump("hg_f", T1, T1[:], [128, TOK])
                self.dump("hg_b", T3, T3[:], [128, TOK])
                self.dump("hg_dec", dec, dec[:], [128, NCH])
                self.dump("hg_kdec", A, A[:], [128, TOK], BF16)
                self.dump("hg_qP", qP, qP[:], [128, TOK])
                self.dump("hg_VT", VT, VT[:], [64, NG, 128], BF16)
            if self.dbg.get("hg_stop", 9) <= 1:
                continue
            for q in range(NG // 4):
                p = PT[n_pt % 2]
                n_pt += 1
                for jj in range(4):
                    g = q * 4 + jj
                    k.mm(p[0:64, jj * 128:(jj + 1) * 128], A[:, 64 * g:64 * g + 64], self.ident_b[:], True, True,
                         [A, self.ident_b], [p])
                k.cp(k.act, KD[:, q * 4:(q + 1) * 4, :], p[0:64, :].rearrange("p (a b) -> p a b", a=4), [p], [KD])
            if self.dbg.get("hg_sub", 9) <= 1:
                continue
            order = list(range(NCH)) if d == 0 else (list(range(7, -1, -1)) + list(range(NCH - 1, 7, -1)))
            k.op(k.dve, lambda en: en.memset(S[:], 0.0), [], [S])
            for i8 in range(0, NCH, 8):
                pc = [KV[(n_kv % 2) * 2 + 0], KV[(n_kv % 2) * 2 + 1]]
                n_kv += 1
                for jj in range(8):
                    n = order[i8 + jj]
                    g, c = n // 2, n % 2
                    sl_ = (jj // 2) * 128
                    k.mm(pc[c][:, sl_:sl_ + 128], KD[32 * c:32 * c + 32, g, :], VT[32 * c:32 * c + 32, g, :], True, True,
                         [KD, VT], [pc[c]])
                if self.dbg.get("hg_sub", 9) <= 2:
                    continue
                for jj in range(8):
                    n = order[i8 + jj]
                    c = n % 2
                    sl_ = (jj // 2) * 128
                    k.cp(k.act, SIN_[d][:, n, :], S[:], [S], [SIN_[d]])
                    k.stt(S[:], S[:], dec[:, n:n + 1], pc[c][:, sl_:sl_ + 128], ALU.mult, ALU.add, [S, dec, pc[c]], [S])
            if self.dbg.get("hg_stop", 9) <= 2:
                continue
            k.tt(k.dve, v3(T4), b3, BR3, ALU.subtract, [T3], [T4])
            k.actf(T5[:], T4[:], AF.Exp, [T4], [T5])
            k.actf(T4[:], T4[:], AF.Exp, [T4], [T4], scale=-1.0)
            k.tt(k.pool, A[:], qP[:], T5[:], ALU.mult, [qP, T5], [A])
            k.tt(k.dve, Bt[:], T1[:], T4[:], ALU.mult, [T1, T4], [Bt])
            for q in range((NG + 7) // 8):
                p = PT[n_pt % 2]
                n_pt += 1
                ng = min(8, NG - q * 8)
                for jj in range(ng):
                    g = q * 8 + jj
                    k.mm(p[0:64, jj * 64:(jj + 1) * 64], Bt[:, 64 * g:64 * g + 64], A[:, 64 * g:64 * g + 64], True, True,
                         [A, Bt], [p])
                k.tt(k.dve, SC[d][:, q * 8:q * 8 + ng, :], p[0:64, 0:ng * 64].rearrange("p (a b) -> p a b", b=64),
                     mk[:, d, :].unsqueeze(1).broadcast_to([64, ng, 64]), ALU.mult, [p, mk], [SC[d]])
            k.actf(T5[:], T3[:], AF.Exp, [T3], [T5])
            k.tt(k.pool, QB[d][:], qP[:], T5[:], ALU.mult, [qP, T5], [QB[d]])
            if d == self.dbg.get("hg_dd", 0):
                self.dump("hg_KD", KD, KD[:], [64, NG, 128], BF16)
                self.dump("hg_sin", SIN_[d], SIN_[d][:], [128, NCH, 128], BF16)
                self.dump("hg_sc", SC[d], SC[d][:], [64, NG, 64], BF16)
                self.dump("hg_qb", QB[d], QB[d][:], [128, TOK], BF16)
        if self.dbg.get("hg_stop", 9) <= 3:
            return
        n_po = 0
        for q in range((NG + 7) // 8):
            p = PO[n_po % 2]
            n_po += 1
            ng = min(8, NG - q * 8)
            for jj in range(ng):
                g = q * 8 + jj
                o = lambda a_, b_: p[:, jj * 64 + a_:jj * 64 + b_]
                k.mm(o(0, 64), VT[0:64, g, :], SC[0][:, g, :], True, False, [VT, SC[0]], [p])
                k.mm(o(0, 64), VT[0:64, g, :], SC[1][:, g, :], False, False, [VT, SC[1]], [p])
                for d in range(2):
                    for c in range(2):
                        n = 2 * g + c
                        k.mm(o(32 * c, 32 * c + 32), SIN_[d][:, n, :], QB[d][:, 32 * n:32 * n + 32], False, (d == 1 and c == 1),
                             [SIN_[d], QB[d]], [p])
            k.tt(k.dve, T1[:, q * 512:q * 512 + ng * 64], p[:, 0:ng * 64], sogP[:, q * 512:q * 512 + ng * 64], ALU.mult,
                 [p, sogP], [T1])
        self.dump("hg_y", T1, T1[:], [128, TOK])
        if self.dbg.get("hg_stop", 9) <= 4:
            return
        k.actf(T2[:], T1[:], AF.Square, [T1], [T2])
        blocks = [(q * 512, 512) for q in range(4)] + [(2048, 256)]
        for (t0, n) in blocks:
            p = PT[n_pt % 2]
            n_pt += 1
            k.mm(p[:, 0:n], onesq[:], T2[:, t0:t0 + n], True, True, [onesq, T2], [p])
            k.ts(k.dve, T3[:, t0:t0 + n], p[:, 0:n], 1.0 / 128, EPS, ALU.mult, ALU.add, [p], [T3])
        k.actf(T3[:], T3[:], AF.Sqrt, [T3], [T3])
        k.op(k.dve, lambda en: en.reciprocal(out=T3[:], in_=T3[:]), [T3], [T3])
        k.stt(T2[:], T1[:], ghg[:, h:h + 1], T3[:], ALU.mult, ALU.mult, [T1, ghg, T3], [T2])
        k.cp(k.act, YR[:, 0:CTX], T2[:, 0:CTX], [T2], [YR])
        k.cp(k.pool, YR[:, CTX:].rearrange("p (r c) -> p c r", c=64), T2[:, CTX:].rearrange("p (c r) -> p c r", r=32), [T2], [YR])
        k.dma(k.sp, [(YHT[rows, c0:c0 + TOK], YR[:])], YR, reads=[YR], writes=[self.dbuf["YHT"]], accumulate=True)


    def phase_merge(self, l):
        k = self.k
        X, MODS, GT, Y5T, YHT = (self.dram[n] for n in ("X", "MODS", "GT", "Y5T", "YHT"))
        blocks = [(0, 256)] + [(256 + q * 512, 512) for q in range(4)]
        wv = lambda name: self.dram[name][l].rearrange("(kc p) n -> p kc n", p=128)
        for b in range(NB):
            c0 = b * TOK
            with self.k.scope() as st:
                mT = k.sb(st, [128, 16, TOK], BF16, "mT")
                with self.k.scope() as s1:
                    WBS = k.sb(s1, [128, 8, D], BF16, "WBS", dma=True)
                    WBH = k.sb(s1, [128, 8, D], BF16, "WBH", dma=True)
                    for (W, nm) in ((WBS, "w_branch_s5"), (WBH, "w_branch_hg")):
                        k.dma(k.pool, [(W[:, 2 * i:2 * i + 2, :], wv(nm)[:, 2 * i:2 * i + 2, :]) for i in range(4)], W,
                              reads=[self.dbuf[nm]], writes=[W])
                    y5 = [k.sb(s1, [128, 8, 512], BF16, "y5", dma=True) for _ in range(2)]
                    yh = [k.sb(s1, [128, 8, 512], BF16, "yh", dma=True) for _ in range(2)]
                    g5 = [k.sb(s1, [128, 512], F32, "g5", dma=True) for _ in range(2)]
                    gh = [k.sb(s1, [128, 512], F32, "gh", dma=True) for _ in range(2)]
                    t1 = [k.sb(s1, [128, 512], F32, "t1") for _ in range(2)]
                    t2 = [k.sb(s1, [128, 512], F32, "t2") for _ in range(2)]
                    pa = [k.ps(s1, [128, 512]) for _ in range(2)]
                    pb = [k.ps(s1, [128, 512]) for _ in range(2)]
                    n_ = 0
                    for bi, (t0, n) in enumerate(blocks):
                        a5, ah = y5[bi % 2], yh[bi % 2]
                        k.dma(k.sp, [(a5[:, :, 0:n], Y5T[:, c0 + t0:c0 + t0 + n].rearrange("(kc p) t -> p kc t", p=128))], a5,
                              reads=[self.dbuf["Y5T"]], writes=[a5])
                        k.dma(k.sp, [(ah[:, :, 0:n], YHT[:, c0 + t0:c0 + t0 + n].rearrange("(kc p) t -> p kc t", p=128))], ah,
                              reads=[self.dbuf["YHT"]], writes=[ah])
                        for oc in range(16):
                            u = n_ % 2
                            n_ += 1
                            k.dma(k.sp, [(g5[u][:, 0:n], GT[oc * 128:(oc + 1) * 128, c0 + t0:c0 + t0 + n])], g5[u],
                                  reads=[self.dbuf["GT"]], writes=[g5[u]])
                            k.dma(k.sp, [(gh[u][:, 0:n], GT[2048 + oc * 128:2048 + (oc + 1) * 128, c0 + t0:c0 + t0 + n])], gh[u],
                                  reads=[self.dbuf["GT"]], writes=[gh[u]])
                            for kc in range(8):
                                k.mm(pa[u][:, 0:n], WBS[:, kc, oc * 128:(oc + 1) * 128], a5[:, kc, 0:n], kc == 0, kc == 7, [WBS, a5], [pa[u]])
                            for kc in range(8):
                                k.mm(pb[u][:, 0:n], WBH[:, kc, oc * 128:(oc + 1) * 128], ah[:, kc, 0:n], kc == 0, kc == 7, [WBH, ah], [pb[u]])
                            k.tt(k.dve, t1[u][:, 0:n], pa[u][:, 0:n], g5[u][:, 0:n], ALU.mult, [pa[u], g5[u]], [t1[u]])
                            k.tt(k.dve, t2[u][:, 0:n], pb[u][:, 0:n], gh[u][:, 0:n], ALU.mult, [pb[u], gh[u]], [t2[u]])
                            k.tt(k.pool, mT[:, oc, t0:t0 + n], t1[u][:, 0:n], t2[u][:, 0:n], ALU.add, [t1[u], t2[u]], [mT])
                WO = k.sb(st, [128, 16, D], BF16, "WO", dma=True)
                k.dma(k.pool, [(WO[:, 2 * i:2 * i + 2, :], wv("w_out")[:, 2 * i:2 * i + 2, :]) for i in range(8)], WO,
                      reads=[self.dbuf["w_out"]], writes=[WO])
                gtm = [k.sb(st, [128, D], F32, "gtm", dma=True) for _ in range(2)]
                for j, bb in enumerate((NB, b)):
                    k.dma(k.sp, [(gtm[j][:], MODS[bb, :, 2 * D:3 * D])], gtm[j], reads=[self.dbuf["MODS"]], writes=[gtm[j]])
                xin = [k.sb(st, [128, D], F32, "xin", dma=True) for _ in range(2)]
                xo = [k.sb(st, [128, D], F32, "xo", dma=True) for _ in range(2)]
                tm = [k.sb(st, [128, 512], F32, "tm") for _ in range(2)]
                pso = [k.ps(st, [128, 512]) for _ in range(4)]
                n_ = 0
                for i in range(TOK // 128):
                    r0 = c0 + i * 128
                    xi_, xo_ = xin[i % 2], xo[i % 2]
                    g_ = gtm[0 if i < CTX // 128 else 1]
                    k.dma(k.sp, [(xi_[:], X[r0:r0 + 128, :])], xi_, reads=[self.dbuf["X"]], writes=[xi_])
                    for cc in range(4):
                        p = pso[n_ % 4]
                        tmv = tm[n_ % 2]
                        n_ += 1
                        cs = slice(cc * 512, (cc + 1) * 512)
                        for kc in range(16):
                            k.mm(p[:], mT[:, kc, i * 128:(i + 1) * 128], WO[:, kc, cs], kc == 0, kc == 15, [mT, WO], [p])
                        k.tt(k.dve, tmv[:], p[:], g_[:, cs], ALU.mult, [p, g_], [tmv])
                        k.tt(k.pool, xo_[:, cs], tmv[:], xi_[:, cs], ALU.add, [tmv, xi_], [xo_])
                    k.dma(k.sp, [(X[r0:r0 + 128, :], xo_[:])], xo_, reads=[xo_], writes=[self.dbuf["X"]], accumulate=True)

    def phase_peer(self, l):
        k = self.k
        X, MODS = self.dram["X"], self.dram["MODS"]
        UTAB = self.dram["peer_u"].rearrange("l e d -> (l e) d")
        VTAB = self.dram["peer_v"].rearrange("l e d -> (l e) d")
        with self.k.scope() as st:
            WQ = k.sb(st, [128, 16, D], BF16, "WQ", dma=True)
            k.dma(k.pool, [(WQ[:, 2 * i:2 * i + 2, :], self.dram["peer_w_q"][l].rearrange("(kc p) n -> p kc n", p=128)[:, 2 * i:2 * i + 2, :])
                           for i in range(8)], WQ, reads=[self.dbuf["peer_w_q"]], writes=[WQ])
            KT = k.sb(st, [128, 16, 128], BF16, "KT", dma=True)
            k.dma(k.pool, [(KT[:], self.dram["peer_kt"][l])], KT, reads=[self.dbuf["peer_kt"]], writes=[KT])
            io16 = k.sb(st, [128, 16], F32, "io16", dma=True)
            k.dma(k.sp, [(io16[:], self.dram["cst2"][:, 0:16])], io16, reads=[self.dbuf["cst2"]], writes=[io16])
            AF_ = k.sb(st, [128, D], F32, "AF", dma=True)
            SF_ = k.sb(st, [128, D], F32, "SF", dma=True)
            GF_ = k.sb(st, [128, D], F32, "GF", dma=True)
            xin = k.sb(st, [128, D], F32, "xin", dma=True)
            tn = k.sb(st, [128, D], F32, "tn", dma=True)
            xo = tn
            tmp = k.sb(st, [128, D], F32, "tmp")
            junk = k.sb(st, [128, D], BF16, "junk")
            tnb = k.sb(st, [128, D], BF16, "tnb")
            tnT = k.sb(st, [128, 16, 128], BF16, "tnT")
            qb = k.sb(st, [128, D], BF16, "qb")
            qT = k.sb(st, [128, 16, 128], BF16, "qT")
            Ssb = k.sb(st, [128, 16, 128], F32, "Ssb")
            ss = k.sb(st, [128, 1], F32, "ss")
            rs = k.sb(st, [128, 1], F32, "rs")
            NU = 3
            UB = [k.sb(st, [128, D], F32, "UB", dma=True) for _ in range(NU)]
            VB = [k.sb(st, [128, D], BF16, "VB", dma=True) for _ in range(NU)]
            DJ = [k.sb(st, [128, 128], BF16, "DJ") for _ in range(2)]
            v8 = k.sb(st, [128, 2, 16], F32, "v8")
            i8 = k.sb(st, [128, 2, 16], U32, "i8")
            i8f = k.sb(st, [128, 2, 16], F32, "i8f")
            srep = k.sb(st, [128, 128], F32, "srep")
            cand = k.sb(st, [128, 256], F32, "cand")
            cand2 = k.sb(st, [128, 256], F32, "cand2")
            sc = k.sb(st, [128, 16], F32, "sc")
            ic = k.sb(st, [128, 16], U32, "ic")
            icf = k.sb(st, [128, 16], F32, "icf")
            hi_i = k.sb(st, [128, 16], I32, "hi_i")
            hif = k.sb(st, [128, 16], F32, "hif")
            lof = k.sb(st, [128, 16], F32, "lof")
            oh = k.sb(st, [128, 16, 16], F32, "oh")
            e1 = k.sb(st, [128, 16], F32, "e1")
            e2 = k.sb(st, [128, 16], F32, "e2")
            EXPf = k.sb(st, [128, 128], F32, "EXPf")
            EXPi = k.sb(st, [128, 128], I32, "EXPi")
            G = k.sb(st, [128, 128], F32, "G")
            nm = k.sb(st, [128, 1], F32, "nm")
            sm = k.sb(st, [128, 1], F32, "sm")
            araw = k.sb(st, [128, 128], F32, "araw")
            coef = k.sb(st, [128, 128], F32, "coef")
            pt = [k.ps(st, [128, 512]) for _ in range(3)]
            pacc = [k.ps(st, [128, 512]) for _ in range(4)]
            n_pt = 0
            cur_bb = None
            n_u = 0
            n_v = 0
            tiles = self.dbg.get("peer_tiles", list(range(NT // 128)))
            for ti in tiles:
                b, i = ti // (TOK // 128), ti % (TOK // 128)
                bb = NB if i < CTX // 128 else b
                if bb != cur_bb:
                    cur_bb = bb
                    k.dma(k.sp, [(AF_[:], MODS[bb, :, 4 * D:5 * D])], AF_, reads=[self.dbuf["MODS"]], writes=[AF_])
                    k.dma(k.sp, [(SF_[:], MODS[bb, :, 3 * D:4 * D])], SF_, reads=[self.dbuf["MODS"]], writes=[SF_])
                    k.dma(k.sp, [(GF_[:], MODS[bb, :, 5 * D:6 * D])], GF_, reads=[self.dbuf["MODS"]], writes=[GF_])
                r0 = ti * 128
                k.dma(k.sp, [(xin[:], X[r0:r0 + 128, :])], xin, reads=[self.dbuf["X"]], writes=[xin])
                self.norm_mod_tile((junk, ss, rs, tmp), xin, AF_, SF_, tn)
                k.cp(k.act, tnb[:], tn[:], [tn], [tnb])
                for q4 in range(4):
                    p = pt[n_pt % 3]
                    n_pt += 1
                    for jj in range(4):
                        kc = q4 * 4 + jj
                        k.mm(p[:, jj * 128:(jj + 1) * 128], tnb[:, kc * 128:(kc + 1) * 128], self.ident_b[:], True, True,
                             [tnb, self.ident_b], [p])
                    k.cp(k.act if q4 % 2 else k.dve, tnT[:, q4 * 4:(q4 + 1) * 4, :], p[:].rearrange("p (a b) -> p a b", a=4), [p], [tnT])
                for cc in range(4):
                    p = pt[n_pt % 3]
                    n_pt += 1
                    for kc in range(16):
                        k.mm(p[:], tnT[:, kc, :], WQ[:, kc, cc * 512:(cc + 1) * 512], kc == 0, kc == 15, [tnT, WQ], [p])
                    k.cp(k.act if cc % 2 else k.dve, qb[:, cc * 512:(cc + 1) * 512], p[:], [p], [qb])
                for q4 in range(4):
                    p = pt[n_pt % 3]
                    n_pt += 1
                    for jj in range(4):
                        kc = q4 * 4 + jj
                        k.mm(p[:, jj * 128:(jj + 1) * 128], qb[:, kc * 128:(kc + 1) * 128], self.ident_b[:], True, True,
                             [qb, self.ident_b], [p])
                    k.cp(k.act if q4 % 2 else k.dve, qT[:, q4 * 4:(q4 + 1) * 4, :], p[:].rearrange("p (a b) -> p a b", a=4), [p], [qT])
                for q4 in range(4):
                    p = pt[n_pt % 3]
                    n_pt += 1
                    for jj in range(4):
                        hh = q4 * 4 + jj
                        k.mm(p[:, jj * 128:(jj + 1) * 128], qT[:, hh, :], KT[:, hh, :], True, True, [qT, KT], [p])
                    k.cp(k.act, Ssb[:, q4 * 4:(q4 + 1) * 4, :], p[:].rearrange("p (a b) -> p a b", a=4), [p], [Ssb])
                for h in range(8):
                    for half in range(2):
                        sv = Ssb[:, 2 * h + half, :]
                        k.op(k.dve, lambda e, sv=sv, half=half: e.max(out=v8[:, half, 0:8], in_=sv), [Ssb], [v8])
                        k.op(k.dve, lambda e, sv=sv, half=half: e.max_index(out=i8[:, half, 0:8], in_max=v8[:, half, 0:8], in_values=sv),
                             [Ssb, v8], [i8])
                        k.op(k.dve, lambda e, sv=sv, half=half: e.match_replace(out=srep[:], in_to_replace=v8[:, half, 0:8],
                                                                              in_values=sv, imm_value=-1e30), [Ssb, v8], [srep])
                        k.op(k.dve, lambda e, half=half: e.max(out=v8[:, half, 8:16], in_=srep[:]), [srep], [v8])
                        k.op(k.dve, lambda e, half=half: e.max_index(out=i8[:, half, 8:16], in_max=v8[:, half, 8:16], in_values=srep[:]),
                             [srep, v8], [i8])
                    k.cp(k.dve, i8f[:], i8[:], [i8], [i8f])
                    k.tt(k.dve, cand[:].rearrange("p (a b) -> p a b", a=16), v8[:, 0, :].unsqueeze(2).broadcast_to([128, 16, 16]),
                         v8[:, 1, :].unsqueeze(1).broadcast_to([128, 16, 16]), ALU.add, [v8], [cand])
                    k.op(k.dve, lambda e: e.max(out=sc[:, 0:8], in_=cand[:]), [cand], [sc])
                    k.op(k.dve, lambda e: e.max_index(out=ic[:, 0:8], in_max=sc[:, 0:8], in_values=cand[:]), [cand, sc], [ic])
                    k.op(k.dve, lambda e: e.match_replace(out=cand2[:], in_to_replace=sc[:, 0:8], in_values=cand[:], imm_value=-1e30),
                         [cand, sc], [cand2])
                    k.op(k.dve, lambda e: e.max(out=sc[:, 8:16], in_=cand2[:]), [cand2], [sc])
                    k.op(k.dve, lambda e: e.max_index(out=ic[:, 8:16], in_max=sc[:, 8:16], in_values=cand2[:]), [cand2, sc], [ic])
                    k.cp(k.dve, icf[:], ic[:], [ic], [icf])
                    k.ts(k.dve, hi_i[:], icf[:], 1.0 / 16, -15.0 / 32, ALU.mult, ALU.add, [icf], [hi_i])
                    k.cp(k.dve, hif[:], hi_i[:], [hi_i], [hif])
                    k.stt(lof[:], hif[:], -16.0, icf[:], ALU.mult, ALU.add, [hif, icf], [lof])
                    for (sel, idxf, eo) in ((hif, i8f[:, 0, :], e1), (lof, i8f[:, 1, :], e2)):
                        k.tt(k.dve, oh[:], sel[:].unsqueeze(2).broadcast_to([128, 16, 16]),
                             io16[:].unsqueeze(1).broadcast_to([128, 16, 16]), ALU.is_equal, [sel, io16], [oh])
                        k.tt(k.dve, oh[:], oh[:], idxf.unsqueeze(1).broadcast_to([128, 16, 16]), ALU.mult, [oh, i8f], [oh])
                        k.op(k.dve, lambda e, eo=eo: e.tensor_reduce(out=eo[:], in_=oh[:], axis=AX.X, op=ALU.add), [oh], [eo])
                    k.stt(EXPf[:, h * 16:(h + 1) * 16], e1[:], 128.0, e2[:], ALU.mult, ALU.add, [e1, e2], [EXPf])
                    k.ts(k.dve, nm[:], sc[:, 0:1], -1.0, None, ALU.mult, None, [sc], [nm])
                    k.op(k.act, lambda e, h=h: e.activation(out=G[:, h * 16:(h + 1) * 16], in_=sc[:], func=AF.Exp, bias=nm[:, 0:1],
                                                            accum_out=sm[:]), [sc, nm], [G, sm])
                    k.op(k.dve, lambda e: e.reciprocal(out=sm[:], in_=sm[:]), [sm], [sm])
                    k.ts(k.dve, G[:, h * 16:(h + 1) * 16], G[:, h * 16:(h + 1) * 16], sm[:, 0:1], None, ALU.mult, None, [G, sm], [G])
                if l > 0:
                    k.ts(k.dve, EXPf[:], EXPf[:], float(l * NEXP), None, ALU.add, None, [EXPf], [EXPf])
                k.cp(k.dve, EXPi[:], EXPf[:], [EXPf], [EXPi])
                for j in range(128):
                    ub = UB[n_u % NU]
                    n_u += 1
                    self.idma(ub, UTAB, EXPi, j, "peer_u")
                    k.op(k.dve, lambda e, ub=ub, j=j: e.scalar_tensor_tensor(
                        out=junk[:], in0=ub[:], scalar=1.0, in1=tn[:], op0=ALU.mult, op1=ALU.mult,
                        accum_out=araw[:, j:j + 1]), [ub, tn], [junk, araw])
                k.actf(coef[:], araw[:], AF.Gelu_apprx_tanh, [araw], [coef])
                k.tt(k.dve, coef[:], coef[:], G[:], ALU.mult, [coef, G], [coef])
                for j in range(128):
                    vb = VB[n_v % NU]
                    dj = DJ[n_v % 2]
                    n_v += 1
                    self.idma(vb, VTAB, EXPi, j, "peer_v")
                    k.ts(k.dve, dj[:], self.ident_b[:], coef[:, j:j + 1], None, ALU.mult, None, [self.ident_b, coef], [dj])
                    for cc in range(4):
                        k.mm(pacc[cc][:], dj[:], vb[:, cc * 512:(cc + 1) * 512], j == 0, j == 127, [dj, vb], [pacc[cc]])
                for cc in range(4):
                    cs = slice(cc * 512, (cc + 1) * 512)
                    k.tt(k.dve, tmp[:, cs], pacc[cc][:], GF_[:, cs], ALU.mult, [pacc[cc], GF_], [tmp])
                    k.tt(k.pool, xo[:, cs], tmp[:, cs], xin[:, cs], ALU.add, [tmp, xin], [xo])
                k.dma(k.sp, [(X[r0:r0 + 128, :], xo[:])], xo, reads=[xo], writes=[self.dbuf["X"]], accumulate=True)

    def idma(self, dst, table, idx_tile, j, tname):
        k = self.k
        Q = k.pool
        ds = dst.dsem
        k._wait(Q, k._need([idx_tile, self.dbuf[tname]], [dst]))
        Q.eng.indirect_dma_start(out=dst[:], out_offset=None, in_=table,
                                 in_offset=bass.IndirectOffsetOnAxis(ap=idx_tile[:, j:j + 1], axis=0)).then_inc(ds.sem, 16)
        ds.count += 16
        k.ninst += 1
        k._mark([idx_tile, self.dbuf[tname]], [dst], ds.key, ds.sem, ds.count)

    def phase_final(self):
        k = self.k
        X, OUT = self.dram["X"], self.dram["out"]
        with self.k.scope() as st:
            gb = k.sb(st, [128, D], F32, "gb", dma=True)
            k.dma(k.sp, [(gb[:], self.dram["final_norm_g"][0].partition_broadcast(128))], gb,
                  reads=[self.dbuf["final_norm_g"]], writes=[gb])
            xin = [k.sb(st, [128, D], F32, "xin", dma=True) for _ in range(2)]
            xo = [k.sb(st, [128, D], F32, "xo", dma=True) for _ in range(2)]
            junk = k.sb(st, [128, D], BF16, "junk")
            ss = [k.sb(st, [128, 1], F32, "ss") for _ in range(2)]
            rs = [k.sb(st, [128, 1], F32, "rs") for _ in range(2)]
            n_ = 0
            for b in range(NB):
                for i in range(SEQ // 128):
                    u = n_ % 2
                    n_ += 1
                    r0 = b * TOK + CTX + i * 128
                    k.dma(k.sp, [(xin[u][:], X[r0:r0 + 128, :])], xin[u], reads=[self.dbuf["X"]], writes=[xin[u]])
                    k.op(k.act, lambda e, u=u: e.activation(out=junk[:], in_=xin[u][:], func=AF.Square, accum_out=ss[u][:]),
                         [xin[u]], [junk, ss[u]])
                    k.ts(k.dve, rs[u][:], ss[u][:], 1.0 / D, EPS, ALU.mult, ALU.add, [ss[u]], [rs[u]])
                    k.actf(rs[u][:], rs[u][:], AF.Sqrt, [rs[u]], [rs[u]])
                    k.op(k.dve, lambda e, u=u: e.reciprocal(out=rs[u][:], in_=rs[u][:]), [rs[u]], [rs[u]])
                    k.stt(xo[u][:], xin[u][:], rs[u][:, 0:1], gb[:], ALU.mult, ALU.mult, [xin[u], rs[u], gb], [xo[u]])
                    k.dma(k.sp, [(OUT[b, i * 128:(i + 1) * 128, :], xo[u][:])], xo[u], reads=[xo[u]],
                          writes=[self.dbuf["out"]], accumulate=True)

def host_consts():
    c = np.zeros((4, 128, 128), np.float32)
    c[0] = np.eye(128, dtype=np.float32)
    i = np.arange(128)
    same = (i[:, None] // 32) == (i[None, :] // 32)
    c[1] = (same & (i[:, None] <= i[None, :])).astype(np.float32)
    c[2] = (same & (i[:, None] >= i[None, :])).astype(np.float32)
    return c


def input_specs(DEPTH):
    return [
        ("cst", [4, 128, 128], F32), ("cst2", [128, 513], F32), ("cst3", [128, 4], F32),
        ("x", [NB, SEQ, D], F32), ("ctx", [NB, CTX, D], F32), ("cvec", [NB + 1, D], F32),
        ("w_ada", [DEPTH, D, 6 * D], F32), ("b_ada", [DEPTH, 6 * D], F32),
        ("norm_mix_g", [DEPTH, D], F32), ("norm_ffn_g", [DEPTH, D], F32), ("w_in", [DEPTH, D, PW], F32),
        ("s5sl", [DEPTH, 2, 128, 3, 32], F32), ("s5bl", [DEPTH, 2, 128, 3, 512], F32),
        ("s5bt", [DEPTH, 2, 128, 2, 8, 128], F32), ("s5ct", [DEPTH, 2, 128, 2, 32, 32], F32),
        ("gam_l", [128, 4, 16], F32), ("ghg_l", [DEPTH, 128, 8], F32), ("s5d_l", [DEPTH, 128, 8], F32),
        ("w_branch_s5", [DEPTH, 1024, D], F32), ("w_branch_hg", [DEPTH, 1024, D], F32), ("w_out", [DEPTH, D, D], F32),
        ("peer_w_q", [DEPTH, D, D], F32), ("peer_kt", [DEPTH, 128, 16, 128], F32),
        ("peer_u", [DEPTH, NEXP, D], F32), ("peer_v", [DEPTH, NEXP, D], F32), ("final_norm_g", [1, D], F32), ("bglu_l", [DEPTH, 128, 8], F32), ("s5_w_glu", [DEPTH, 1024, 1024], F32),
    ]


SCRATCH_SPECS = [
    ("X", [NT, D], F32), ("MODS", [NB + 1, 128, 6 * D], F32), ("UT", [1024, NT], F32), ("QFT", [4096, NT], F32),
    ("VOG", [NT, 1024], F32), ("GT", [4096, NT], F32), ("Y5T", [1024, NT], BF16), ("YHT", [1024, NT], BF16),
]


def build(dbg=None):
    P = Prog(dbg)
    dbg = P.dbg
    feed = dbg.get("feed", ())
    only = dbg.get("only", None)
    need_in = dbg.get("inputs", None)
    for (n, shp, dt) in input_specs(dbg.get("depth_in", DEPTH)):
        if need_in is None or n in need_in:
            P.din(n, shp, dt)
    if only is None or "final" in only:
        P.dout("out", [NB, SEQ, D], F32)
    for (n, shp, dt) in SCRATCH_SPECS:
        if n in feed:
            P.din(n, shp, dt)
        else:
            P.dscr(n, shp, dt)
    run = lambda ph: only is None or ph in only
    with P.es:
        st = P.es
        P.consts(st)
        if run("init"):
            P.phase_init_x()
        if run("hg"):
            P.phase_hg_lb(st)
        nl = dbg.get("layers", DEPTH)
        for l in range(nl):
            if run("mods"):
                P.phase_mods(l)
            if run("win"):
                P.phase_win(l)
            if run("s5"):
                P.phase_s5(l)
            if run("hg"):
                P.phase_hgrn(l)
            if run("merge"):
                P.phase_merge(l)
            if run("peer"):
                P.phase_peer(l)
        if run("final"):
            P.phase_final()
        for nme in dbg.get("copyout", ()):
            P.k.barrier()
            src = P.dram[nme]
            t = P.nc.dram_tensor("co_" + nme, list(src.shape), src.dtype, kind="ExternalOutput").ap()
            ds = P.k.dfree[0]
            nr = src.shape[0]
            step = (nr + 7) // 8
            P.k.dma(P.k.sp, [(t[r:min(r + step, nr)], src[r:min(r + step, nr)]) for r in range(0, nr, step)], ds,
                    reads=[P.dbuf[nme]], writes=[Buf("co")], accumulate=True)
        P.k.barrier()
    return P


def s5_layouts(inputs):
    a_re, a_im, ldt = inputs["s5_a_re"], inputs["s5_a_im"], inputs["s5_log_dt"]
    L = a_re.shape[0]
    def sl(a):
        return a.reshape(L, 2, 32, 2, 64).transpose(0, 1, 3, 4, 2).reshape(L, 2, 128, 32)
    ldt_sl = np.broadcast_to(ldt.reshape(L, 2, 32, 2).transpose(0, 1, 3, 2)[:, :, :, None, :], (L, 2, 2, 64, 32)).reshape(L, 2, 128, 32)
    s5sl = np.stack([sl(a_re), sl(a_im), ldt_sl], axis=3)
    def bl(a):
        t = a.reshape(L, 2, 8, 8, 64).transpose(0, 1, 3, 2, 4)
        return np.broadcast_to(t[:, :, :, None, :, :], (L, 2, 8, 16, 8, 64)).reshape(L, 2, 128, 512)
    ldt_g = np.broadcast_to(ldt[:, :, :, None], a_re.shape)
    s5bl = np.stack([bl(a_re), bl(a_im), bl(ldt_g)], axis=3)
    def btl(Bm):
        t = Bm.reshape(L, 2, 8, 8, 64, 16).transpose(0, 1, 3, 5, 2, 4)
        o = np.zeros((L, 2, 8, 16, 8, 2, 64), np.float32)
        for gl in range(8):
            o[:, :, gl, :, :, gl % 2, :] = t[:, :, gl]
        return o.reshape(L, 2, 128, 8, 128)
    s5bt = np.stack([btl(inputs["s5_b_re"]), btl(inputs["s5_b_im"])], axis=3)
    def ctl(Cm):
        t = Cm.reshape(L, 2, 32, 2, 16, 64)
        o = np.zeros((L, 2, 2, 64, 32, 2, 16), np.float32)
        for s_ in range(2):
            o[:, :, s_, :, :, s_, :] = t[:, :, :, s_].transpose(0, 1, 4, 2, 3)
        return o.reshape(L, 2, 128, 32, 32)
    s5ct = np.stack([ctl(inputs["s5_c_re"]), ctl(inputs["s5_c_im"])], axis=3)
    chl = lambda v: np.ascontiguousarray(v.reshape(L, 8, 128).transpose(0, 2, 1))
    return dict(s5sl=np.ascontiguousarray(s5sl), s5bl=np.ascontiguousarray(s5bl), s5bt=np.ascontiguousarray(s5bt),
                s5ct=np.ascontiguousarray(s5ct), s5d_l=chl(inputs["s5_d"]), bglu_l=chl(inputs["s5_b_glu"]))


def shared_inputs(inputs):
    d = {}
    d["cst"] = host_consts()
    c3 = np.zeros((128, 4), np.float32)
    c3[96:, 0] = 1.0
    d["cst3"] = c3
    d["cst2"] = np.ascontiguousarray(np.broadcast_to(np.arange(513, dtype=np.float32)[None, :], (128, 513)))
    for kname in ("w_ada", "b_ada", "norm_mix_g", "norm_ffn_g", "w_in", "s5_w_glu", "w_branch_s5", "w_branch_hg", "w_out",
                  "peer_w_q", "peer_u", "peer_v"):
        d[kname] = np.ascontiguousarray(inputs[kname])
    d["final_norm_g"] = np.ascontiguousarray(inputs["final_norm_g"][None, :])
    kt = np.stack([inputs["peer_k1"], inputs["peer_k2"]], axis=2)
    d["peer_kt"] = np.ascontiguousarray(kt.transpose(0, 4, 1, 2, 3).reshape(DEPTH, 128, 16, 128))
    d.update(s5_layouts(inputs))
    gam = inputs["hg_lb_gamma"]
    d["gam_l"] = np.ascontiguousarray(gam.reshape(DEPTH, 2, 8, 128).transpose(3, 0, 1, 2).reshape(128, DEPTH, 16))
    d["ghg_l"] = np.ascontiguousarray(inputs["hg_norm_g"].reshape(DEPTH, 8, 128).transpose(0, 2, 1))
    return d


def core_inputs(inputs, core, shared=None):
    b0 = core * NB
    d = dict(shared if shared is not None else shared_inputs(inputs))
    d["x"] = np.ascontiguousarray(inputs["x"][b0:b0 + NB])
    d["ctx"] = np.ascontiguousarray(inputs["ctx"][b0:b0 + NB])
    d["cvec"] = np.ascontiguousarray(np.concatenate([inputs["c"][b0:b0 + NB], inputs["c_ctx"][None, :]], axis=0))
    return d


_PROG_CACHE = {}


def kernel(**inputs):
    inputs = {k_: np.asarray(v) for k_, v in inputs.items()}
    if "prog" not in _PROG_CACHE:
        _PROG_CACHE["prog"] = build()
    P = _PROG_CACHE["prog"]
    shared = shared_inputs(inputs)
    names = [n for (n, _s, _t) in input_specs(DEPTH)]
    in_maps = []
    for c in range(N_CORES):
        d = core_inputs(inputs, c, shared)
        in_maps.append({n: d[n] for n in names})
    res = run_bass_kernel_spmd(P.nc, in_maps, core_ids=list(range(N_CORES)))
    out = np.concatenate([np.asarray(res.results[c]["out"]) for c in range(N_CORES)], axis=0)
    return out.astype(np.float32, copy=False)
```

```python
import contextlib
import os
import numpy as np
import ml_dtypes
import concourse.bass as bass
import concourse.mybir as mybir
from concourse.bass_utils import run_bass_kernel_spmd

F32 = mybir.dt.float32
BF16 = mybir.dt.bfloat16
U32 = mybir.dt.uint32
I32 = mybir.dt.int32
ALU = mybir.AluOpType
AF = mybir.ActivationFunctionType
AX = mybir.AxisListType

D = 2048
NB = int(os.environ.get("KERNEL_NB", "4"))
N_CORES = 16 // NB
CTX = 256
SEQ = 2048
TOK = CTX + SEQ
NT = NB * TOK
DEPTH = 4
EPS = 1e-6
PW = 10240
NKEY = 128
NEXP = 16384
TWO_PI = 6.283185307179586
PI = 3.141592653589793


class Buf:
    __slots__ = ("name", "w", "r")

    def __init__(self, name=""):
        self.name = name
        self.w = {}
        self.r = {}


class EngW:
    def __init__(self, nc, eng, name, es):
        self.eng = eng
        self.name = name
        self.sem = es.enter_context(nc.semaphore("s_" + name))
        self.key = "E" + name
        self.count = 0
        self.waited = {}


class DSem:
    def __init__(self, nc, name, es):
        self.sem = es.enter_context(nc.semaphore(name))
        self.key = "D" + name
        self.count = 0


class Tile:
    def __init__(self, h, buf, dsem=None):
        self.h = h
        self.buf = buf
        self.dsem = dsem

    def __getitem__(self, idx):
        return self.h[idx]


class K:
    def __init__(self, nc, es):
        self.nc = nc
        self.es = es
        self.pe = EngW(nc, nc.tensor, "pe", es)
        self.dve = EngW(nc, nc.vector, "dve", es)
        self.act = EngW(nc, nc.scalar, "act", es)
        self.pool = EngW(nc, nc.gpsimd, "pool", es)
        self.sp = EngW(nc, nc.sync, "sp", es)
        self.engs = [self.pe, self.dve, self.act, self.pool, self.sp]
        self.dsems = [DSem(nc, "d%d" % i, es) for i in range(48)]
        self.dfree = list(self.dsems)
        self.uid = 0
        self.ninst = 0

    def sb(self, st, shape, dtype, name=None, dma=False):
        self.uid += 1
        name = (name or "t") + "_%d" % self.uid
        h = st.enter_context(self.nc.sbuf_tensor(name, list(shape), dtype))
        ds = None
        if dma:
            ds = self.dfree.pop(0)
            st.callback(lambda d=ds: self.dfree.append(d))
        return Tile(h, Buf(name), ds)

    def ps(self, st, shape, dtype=F32, name=None):
        self.uid += 1
        name = (name or "p") + "_%d" % self.uid
        h = st.enter_context(self.nc.psum_tensor(name, list(shape), dtype))
        return Tile(h, Buf(name))

    def _need(self, reads, writes):
        need = {}
        for t in reads:
            b = t.buf if isinstance(t, Tile) else t
            for k, (s, v) in b.w.items():
                if k not in need or need[k][1] < v:
                    need[k] = (s, v)
        for t in writes:
            b = t.buf if isinstance(t, Tile) else t
            for dd in (b.w, b.r):
                for k, (s, v) in dd.items():
                    if k not in need or need[k][1] < v:
                        need[k] = (s, v)
        return need

    def _wait(self, E, need):
        for k, (s, v) in need.items():
            if k == E.key and E.name in ("pe", "sp"):
                continue
            if E.waited.get(k, 0) >= v:
                continue
            E.eng.wait_ge(s, v)
            E.waited[k] = v

    def _mark(self, reads, writes, key, sem, val, accumulate=False):
        for t in reads:
            b = t.buf if isinstance(t, Tile) else t
            b.r[key] = (sem, val)
        for t in writes:
            b = t.buf if isinstance(t, Tile) else t
            if accumulate:
                b.w[key] = (sem, val)
            else:
                b.w = {key: (sem, val)}
                b.r = {}

    def op(self, E, fn, reads=(), writes=()):
        self._wait(E, self._need(reads, writes))
        ins = fn(E.eng)
        E.count += 1
        ins.then_inc(E.sem, 1)
        self._mark(reads, writes, E.key, E.sem, E.count)
        self.ninst += 1
        return ins

    def dma(self, Q, pairs, sem_tile, reads=(), writes=(), accumulate=False, **kw):
        ds = sem_tile.dsem if isinstance(sem_tile, Tile) else sem_tile
        self._wait(Q, self._need(reads, writes))
        for (o, i) in pairs:
            Q.eng.dma_start(out=o, in_=i, **kw).then_inc(ds.sem, 16)
            ds.count += 16
            self.ninst += 1
        self._mark(reads, writes, ds.key, ds.sem, ds.count, accumulate=accumulate)

    @contextlib.contextmanager
    def scope(self):
        with contextlib.ExitStack() as st:
            yield st
            self.barrier()

    def tt(self, E, o, a, b, op, R, W):
        return self.op(E, lambda e: e.tensor_tensor(out=o, in0=a, in1=b, op=op), R, W)

    def ts(self, E, o, a, s1, s2, op0, op1, R, W):
        if s2 is None:
            return self.op(E, lambda e: e.tensor_scalar(out=o, in0=a, scalar1=s1, scalar2=None, op0=op0), R, W)
        return self.op(E, lambda e: e.tensor_scalar(out=o, in0=a, scalar1=s1, scalar2=s2, op0=op0, op1=op1), R, W)

    def stt(self, o, a, s, b, op0, op1, R, W):
        return self.op(self.dve, lambda e: e.scalar_tensor_tensor(out=o, in0=a, scalar=s, in1=b, op0=op0, op1=op1), R, W)

    def actf(self, o, a, func, R, W, bias=None, scale=None):
        kw = {}
        if bias is not None:
            kw["bias"] = bias
        if scale is not None:
            kw["scale"] = scale
        return self.op(self.act, lambda e: e.activation(out=o, in_=a, func=func, **kw), R, W)

    def cp(self, E, o, a, R, W):
        if E is self.act:
            return self.op(E, lambda e: e.copy(out=o, in_=a), R, W)
        return self.op(E, lambda e: e.tensor_copy(out=o, in_=a), R, W)

    def mm(self, o, lhsT, rhs, start, stop, R, W):
        return self.op(self.pe, lambda e: e.matmul(o, lhsT=lhsT, rhs=rhs, start=start, stop=stop), R, W)

    def barrier(self):
        for E in self.engs:
            for F in self.engs:
                if F is E or F.count == 0:
                    continue
                if E.waited.get(F.key, 0) >= F.count:
                    continue
                E.eng.wait_ge(F.sem, F.count)
                E.waited[F.key] = F.count
            for d in self.dsems:
                if d.count == 0 or E.waited.get(d.key, 0) >= d.count:
                    continue
                E.eng.wait_ge(d.sem, d.count)
                E.waited[d.key] = d.count


class Prog:
    def __init__(self, dbg=None):
        self.dbg = dbg or {}
        self.nc = bass.Bass("TRN2", target_bir_lowering=False)
        self.es = contextlib.ExitStack()
        self.k = K(self.nc, self.es)
        self.dram = {}
        self.dbuf = {}

    def din(self, name, shape, dtype=F32):
        t = self.nc.dram_tensor(name, list(shape), dtype, kind="ExternalInput").ap()
        self.dram[name] = t
        self.dbuf[name] = Buf(name)
        return t

    def dout(self, name, shape, dtype=F32):
        t = self.nc.dram_tensor(name, list(shape), dtype, kind="ExternalOutput").ap()
        self.dram[name] = t
        self.dbuf[name] = Buf(name)
        return t

    def dscr(self, name, shape, dtype=F32):
        kind = "ExternalOutput" if name in self.dbg.get("dump", ()) else "Internal"
        t = self.nc.dram_tensor(name, list(shape), dtype, kind=kind).ap()
        self.dram[name] = t
        self.dbuf[name] = Buf(name)
        return t

    def dump(self, name, tile, ap, shape, dtype=F32):
        if name not in self.dbg.get("dumps", ()) or ("dbg_" + name) in self.dbg.get("dumped", []):
            return
        k = self.k
        t = self.nc.dram_tensor("dbg_" + name, list(shape), dtype, kind="ExternalOutput").ap()
        if not hasattr(self, "dbg_ds"):
            self.dbg_ds = k.dfree.pop()
            self.dbg_buf = Buf("dbg")
        k.dma(k.sp, [(t, ap)], self.dbg_ds, reads=[tile], writes=[self.dbg_buf], accumulate=True)
        self.dbg.setdefault("dumped", []).append("dbg_" + name)

    def consts(self, st):
        k = self.k
        c = self.dram["cst"]
        self.ident_f = k.sb(st, [128, 128], F32, "identf", dma=True)
        self.ident_b = k.sb(st, [128, 128], BF16, "identb")
        self.ones_f = k.sb(st, [1, 512], F32, "onesf")
        self.ones_b = k.sb(st, [1, 512], BF16, "onesb")
        k.dma(k.sp, [(self.ident_f[:], c[0])], self.ident_f, reads=[self.dbuf["cst"]], writes=[self.ident_f])
        k.op(k.dve, lambda e: e.tensor_copy(out=self.ident_b[:], in_=self.ident_f[:]), [self.ident_f], [self.ident_b])
        k.op(k.dve, lambda e: e.memset(self.ones_f[:], 1.0), [], [self.ones_f])
        k.op(k.dve, lambda e: e.memset(self.ones_b[:], 1.0), [], [self.ones_b])

    def phase_init_x(self):
        k = self.k
        X = self.dram["X"]
        with self.k.scope() as st:
            ds = k.dfree[0]
            pairs = []
            for b in range(NB):
                pairs.append((X[b * TOK:b * TOK + CTX, :], self.dram["ctx"][b]))
                for q in range(4):
                    pairs.append((X[b * TOK + CTX + q * 512:b * TOK + CTX + (q + 1) * 512, :],
                                  self.dram["x"][b, q * 512:(q + 1) * 512, :]))
            k.dma(k.sp, pairs, ds, reads=[self.dbuf["x"], self.dbuf["ctx"]], writes=[self.dbuf["X"]], accumulate=True)
        k.barrier()

    def phase_silu_c(self, st):
        k = self.k
        self.LB = [k.sb(st, [128, 16, 128], BF16, "LB") for _ in range(NB + 1)]
        with self.k.scope() as s2:
            rows = []
            for b in range(NB + 1):
                r = k.sb(s2, [1, D], F32, "crow", dma=True)
                k.dma(k.sp, [(r[:], self.dram["cvec"][b:b + 1, :])], r, reads=[self.dbuf["cvec"]], writes=[r])
                rb = k.sb(s2, [1, D], BF16, "crowb")
                k.op(k.act, lambda e, r=r, rb=rb: e.activation(out=rb[:], in_=r[:], func=AF.Silu), [r], [rb])
                rows.append(rb)
            pss = [k.ps(s2, [128, 512]) for _ in range(2)]
            n = 0
            for b in range(NB + 1):
                lb = self.LB[b]
                for q in range(4):
                    p = pss[n % 2]
                    n += 1
                    for j in range(4):
                        kc = q * 4 + j
                        k.op(k.pe, lambda e, p=p, j=j, kc=kc, b=b: e.matmul(
                            p[:, j * 128:(j + 1) * 128], lhsT=rows[b][0:1, kc * 128:(kc + 1) * 128],
                            rhs=self.ones_b[0:1, 0:128], start=True, stop=True), [rows[b], self.ones_b], [p])
                    k.op(k.dve, lambda e, p=p, lb=lb, q=q: e.tensor_copy(
                        out=lb[:, q * 4:(q + 1) * 4, :], in_=p[:].rearrange("p (a b) -> p a b", a=4)), [p], [lb])
            k.barrier()

    def phase_mods(self, l):
        k = self.k
        wada = self.dram["w_ada"][l].rearrange("(kc p) n -> p kc n", p=128)
        MODS = self.dram["MODS"]
        with self.k.scope() as st:
            self.phase_silu_c(st)
            brow = k.sb(st, [1, 6 * D], F32, "brow", dma=True)
            k.dma(k.sp, [(brow[:], self.dram["b_ada"][l:l + 1, :])], brow, reads=[self.dbuf["b_ada"]], writes=[brow])
            grow = k.sb(st, [1, 2 * D], F32, "grow", dma=True)
            k.dma(k.sp, [(grow[:, 0:D], self.dram["norm_mix_g"][l:l + 1, :]),
                         (grow[:, D:2 * D], self.dram["norm_ffn_g"][l:l + 1, :])], grow,
                  reads=[self.dbuf["norm_mix_g"]], writes=[grow])
            G = k.sb(st, [128, 2 * D], F32, "G")
            pss = [k.ps(st, [128, 512]) for _ in range(4)]
            for q in range(8):
                p = pss[q % 4]
                k.op(k.pe, lambda e, p=p, q=q: e.matmul(p[:], lhsT=self.ones_f[0:1, 0:128],
                                                       rhs=grow[0:1, q * 512:(q + 1) * 512], start=True, stop=True),
                     [grow, self.ones_f], [p])
                k.op(k.act, lambda e, p=p, q=q: e.copy(out=G[:, q * 512:(q + 1) * 512], in_=p[:]), [p], [G])
            was = [k.sb(st, [128, 16, 512], BF16, "WA", dma=True) for _ in range(2)]
            stg = [k.sb(st, [128, 512], F32, "stg", dma=True) for _ in range(4)]
            n = 0
            for nci in range(24):
                wa = was[nci % 2]
                k.dma(k.pool, [(wa[:, 0:8, :], wada[:, 0:8, nci * 512:(nci + 1) * 512]),
                               (wa[:, 8:16, :], wada[:, 8:16, nci * 512:(nci + 1) * 512])], wa,
                      reads=[self.dbuf["w_ada"]], writes=[wa])
                seg = nci // 4
                col = (nci % 4) * 512
                for b in range(NB + 1):
                    p = pss[n % 4]
                    sg = stg[n % 4]
                    n += 1
                    for kc in range(16):
                        k.op(k.pe, lambda e, p=p, kc=kc, b=b, wa=wa: e.matmul(
                            p[:], lhsT=self.LB[b][:, kc, :], rhs=wa[:, kc, :], start=(kc == 0), stop=False),
                            [self.LB[b], wa], [p])
                    k.op(k.pe, lambda e, p=p, nci=nci: e.matmul(
                        p[:], lhsT=self.ones_f[0:1, 0:128], rhs=brow[0:1, nci * 512:(nci + 1) * 512],
                        start=False, stop=True), [brow, self.ones_f], [p])
                    if seg in (1, 4):
                        g0 = (0 if seg == 1 else D) + col
                        k.op(k.dve, lambda e, p=p, sg=sg, g0=g0: e.scalar_tensor_tensor(
                            out=sg[:], in0=p[:], scalar=1.0, in1=G[:, g0:g0 + 512], op0=ALU.add, op1=ALU.mult),
                            [p, G], [sg])
                    else:
                        k.op(k.act, lambda e, p=p, sg=sg: e.copy(out=sg[:], in_=p[:]), [p], [sg])
                    k.dma(k.sp, [(MODS[b, :, nci * 512:(nci + 1) * 512], sg[:])], sg,
                          reads=[sg], writes=[self.dbuf["MODS"]], accumulate=True)
        k.barrier()

    def norm_mod_tile(self, st_tiles, xin, A, SH, xn_out):
        k = self.k
        junk, ss, rs, tmp = st_tiles
        k.op(k.act, lambda e: e.activation(out=junk[:], in_=xin[:], func=AF.Square, accum_out=ss[:]), [xin], [junk, ss])
        k.op(k.dve, lambda e: e.tensor_scalar(out=rs[:], in0=ss[:], scalar1=1.0 / D, scalar2=EPS, op0=ALU.mult, op1=ALU.add),
             [ss], [rs])
        k.op(k.act, lambda e: e.activation(out=rs[:], in_=rs[:], func=AF.Sqrt), [rs], [rs])
        k.op(k.dve, lambda e: e.reciprocal(out=rs[:], in_=rs[:]), [rs], [rs])
        k.op(k.dve, lambda e: e.scalar_tensor_tensor(out=tmp[:], in0=xin[:], scalar=rs[:, 0:1], in1=A[:],
                                                     op0=ALU.mult, op1=ALU.mult), [xin, rs, A], [tmp])
        k.op(k.pool, lambda e: e.tensor_tensor(out=xn_out[:], in0=tmp[:], in1=SH[:], op=ALU.add), [tmp, SH], [xn_out])

    def phase_win(self, l):
        k = self.k
        X = self.dram["X"]
        MODS = self.dram["MODS"]
        win = self.dram["w_in"][l].rearrange("(kc p) n -> p kc n", p=128)
        UT, QFT, VOG, GT = self.dram["UT"], self.dram["QFT"], self.dram["VOG"], self.dram["GT"]
        for b in range(NB):
            with self.k.scope() as st:
                XT = k.sb(st, [128, 16, TOK], BF16, "XT")
                pss = [k.ps(st, [128, 512]) for _ in range(4)]
                with self.k.scope() as s1:
                    AM = [k.sb(s1, [128, D], F32, "AM", dma=True) for _ in range(2)]
                    SM = [k.sb(s1, [128, D], F32, "SM", dma=True) for _ in range(2)]
                    for j, bb in enumerate((NB, b)):
                        k.dma(k.sp, [(AM[j][:], MODS[bb, :, D:2 * D])], AM[j], reads=[self.dbuf["MODS"]], writes=[AM[j]])
                        k.dma(k.sp, [(SM[j][:], MODS[bb, :, 0:D])], SM[j], reads=[self.dbuf["MODS"]], writes=[SM[j]])
                    xins = [k.sb(s1, [128, D], F32, "xin", dma=True) for _ in range(2)]
                    junk = k.sb(s1, [128, D], BF16, "junk")
                    tmp = k.sb(s1, [128, D], F32, "tmp")
                    xns = [k.sb(s1, [128, D], BF16, "xn") for _ in range(2)]
                    sss = [k.sb(s1, [128, 1], F32, "ss") for _ in range(2)]
                    rss = [k.sb(s1, [128, 1], F32, "rs") for _ in range(2)]
                    for i in range(TOK // 128):
                        xin = xins[i % 2]
                        r0 = b * TOK + i * 128
                        k.dma(k.sp, [(xin[:], X[r0:r0 + 128, :])], xin, reads=[self.dbuf["X"]], writes=[xin])
                        j = 0 if i < CTX // 128 else 1
                        xn = xns[i % 2]
                        self.norm_mod_tile((junk, sss[i % 2], rss[i % 2], tmp), xin, AM[j], SM[j], xn)
                        for q in range(4):
                            p = pss[q]
                            for jj in range(4):
                                kc = q * 4 + jj
                                k.op(k.pe, lambda e, p=p, jj=jj, kc=kc, xn=xn: e.matmul(
                                    p[:, jj * 128:(jj + 1) * 128], lhsT=xn[:, kc * 128:(kc + 1) * 128],
                                    rhs=self.ident_b[:], start=True, stop=True), [xn, self.ident_b], [p])
                            E = k.act if q % 2 == 0 else k.dve
                            if E is k.act:
                                k.op(E, lambda e, p=p, q=q, i=i: e.copy(
                                    out=XT[:, q * 4:(q + 1) * 4, i * 128:(i + 1) * 128],
                                    in_=p[:].rearrange("p (a b) -> p a b", a=4)), [p], [XT])
                            else:
                                k.op(E, lambda e, p=p, q=q, i=i: e.tensor_copy(
                                    out=XT[:, q * 4:(q + 1) * 4, i * 128:(i + 1) * 128],
                                    in_=p[:].rearrange("p (a b) -> p a b", a=4)), [p], [XT])
                ws = [k.sb(st, [128, 16, 512], BF16, "W", dma=True) for _ in range(2)]
                stg = [k.sb(st, [128, 512], F32, "stg", dma=True) for _ in range(4)]
                n = 0
                blocks = [(0, 256)] + [(256 + q * 512, 512) for q in range(4)]
                for ci in range(20):
                    w = ws[ci % 2]
                    c0 = ci * 512
                    k.dma(k.pool, [(w[:, 0:8, :], win[:, 0:8, c0:c0 + 512]), (w[:, 8:16, :], win[:, 8:16, c0:c0 + 512])],
                          w, reads=[self.dbuf["w_in"]], writes=[w])
                    if ci in (4, 5):
                        dcol = c0 - 2048
                        for i in range(TOK // 128):
                            p = pss[n % 4]
                            sg = stg[n % 4]
                            n += 1
                            for kc in range(16):
                                k.op(k.pe, lambda e, p=p, kc=kc, i=i, w=w: e.matmul(
                                    p[:], lhsT=XT[:, kc, i * 128:(i + 1) * 128], rhs=w[:, kc, :],
                                    start=(kc == 0), stop=(kc == 15)), [XT, w], [p])
                            if n % 2 == 0:
                                k.op(k.act, lambda e, p=p, sg=sg: e.copy(out=sg[:], in_=p[:]), [p], [sg])
                            else:
                                k.op(k.dve, lambda e, p=p, sg=sg: e.tensor_copy(out=sg[:], in_=p[:]), [p], [sg])
                            r0 = b * TOK + i * 128
                            k.dma(k.sp, [(VOG[r0:r0 + 128, dcol:dcol + 512], sg[:])], sg, reads=[sg],
                                  writes=[self.dbuf["VOG"]], accumulate=True)
                    else:
                        if ci < 2:
                            dst, drow, sig = UT, c0, False
                        elif ci < 4:
                            dst, drow, sig = QFT, c0 - 1024, False
                        elif ci < 8:
                            dst, drow, sig = QFT, 1024 + c0 - 3072, False
                        elif ci < 10:
                            dst, drow, sig = QFT, 2048 + c0 - 4096, False
                        elif ci < 12:
                            dst, drow, sig = QFT, 3072 + c0 - 5120, False
                        else:
                            dst, drow, sig = GT, c0 - 6144, True
                        dname = {id(UT): "UT", id(QFT): "QFT", id(GT): "GT"}[id(dst)]
                        for sub in range(4):
                            for (t0, tn) in blocks:
                                p = pss[n % 4]
                                sg = stg[n % 4]
                                n += 1
                                for kc in range(16):
                                    k.op(k.pe, lambda e, p=p, kc=kc, w=w, sub=sub, t0=t0, tn=tn: e.matmul(
                                        p[:, 0:tn], lhsT=w[:, kc, sub * 128:(sub + 1) * 128], rhs=XT[:, kc, t0:t0 + tn],
                                        start=(kc == 0), stop=(kc == 15)), [XT, w], [p])
                                if sig:
                                    k.op(k.act, lambda e, p=p, sg=sg, tn=tn: e.activation(
                                        out=sg[:, 0:tn], in_=p[:, 0:tn], func=AF.Sigmoid), [p], [sg])
                                elif n % 2 == 0:
                                    k.op(k.act, lambda e, p=p, sg=sg, tn=tn: e.copy(out=sg[:, 0:tn], in_=p[:, 0:tn]), [p], [sg])
                                else:
                                    k.op(k.dve, lambda e, p=p, sg=sg, tn=tn: e.tensor_copy(out=sg[:, 0:tn], in_=p[:, 0:tn]),
                                         [p], [sg])
                                rr = drow + sub * 128
                                k.dma(k.sp, [(dst[rr:rr + 128, b * TOK + t0:b * TOK + t0 + tn], sg[:, 0:tn])], sg,
                                      reads=[sg], writes=[self.dbuf[dname]], accumulate=True)
            k.barrier()


    def ang_reduce(self, xT, xap, kiT, kiap, kfT, kfap):
        k = self.k
        k.ts(k.dve, kiap, xap, 1.0 / TWO_PI, None, ALU.mult, None, [xT], [kiT])
        k.cp(k.dve, kfap, kiap, [kiT], [kfT])
        k.stt(xap, kfap, -TWO_PI, xap, ALU.mult, ALU.add, [kfT, xT], [xT])
        k.ts(k.dve, xap, xap, -3.14159, 3.14159, ALU.max, ALU.min, [xT], [xT])

    def s5_params(self, l, st):
        k = self.k
        prm = []
        for d in range(2):
            prm.append(dict(BT=k.sb(st, [128, 2, 8, 128], BF16, "BTb"), BT3=k.sb(st, [128, 2, 8, 128], BF16, "BT3"),
                            CT=k.sb(st, [128, 2, 32, 64], BF16, "CTb"),
                            R=k.sb(st, [128, 32], F32, "RSL"), TH=k.sb(st, [128, 32], F32, "THR")))
        msk = k.sb(st, [128, 4], F32, "msk", dma=True)
        k.dma(k.sp, [(msk[:], self.dram["cst3"][:, :])], msk, reads=[self.dbuf["cst3"]], writes=[msk])
        with self.k.scope() as s2:
            for d in range(2):
                P = prm[d]
                sl = k.sb(s2, [128, 3, 32], F32, "sl", dma=True)
                bl = k.sb(s2, [128, 3, 512], F32, "bl", dma=True)
                bt = k.sb(s2, [128, 2, 8, 128], F32, "bt", dma=True)
                ct = k.sb(s2, [128, 2, 32, 32], F32, "ct", dma=True)
                k.dma(k.sp, [(sl[:], self.dram["s5sl"][l, d])], sl, reads=[self.dbuf["s5sl"]], writes=[sl])
                k.dma(k.sp, [(bl[:], self.dram["s5bl"][l, d])], bl, reads=[self.dbuf["s5bl"]], writes=[bl])
                k.dma(k.sp, [(bt[:], self.dram["s5bt"][l, d])], bt, reads=[self.dbuf["s5bt"]], writes=[bt])
                k.dma(k.sp, [(ct[:], self.dram["s5ct"][l, d])], ct, reads=[self.dbuf["s5ct"]], writes=[ct])
                k.op(k.pool, lambda e, P=P: e.memset(P["CT"][:], 0.0), [], [P["CT"]])
                k.cp(k.pool, P["CT"][:, :, :, 32:64], ct[:], [ct], [P["CT"]])
                w1 = k.sb(s2, [128, 32], F32, "w1")
                wi = k.sb(s2, [128, 32], I32, "wi")
                wf = k.sb(s2, [128, 32], F32, "wf")
                k.actf(w1[:], sl[:, 2, :], AF.Exp, [sl], [w1])
                k.tt(k.dve, P["TH"][:], sl[:, 1, :], w1[:], ALU.mult, [sl, w1], [P["TH"]])
                k.tt(k.dve, w1[:], sl[:, 0, :], w1[:], ALU.mult, [sl, w1], [w1])
                k.actf(P["R"][:], w1[:], AF.Exp, [w1], [P["R"]])
                self.ang_reduce(P["TH"], P["TH"][:], wi, wi[:], wf, wf[:])
                N = 512
                dt = k.sb(s2, [128, N], F32, "dt")
                r = k.sb(s2, [128, N], F32, "r")
                th = k.sb(s2, [128, N], F32, "th")
                th2 = k.sb(s2, [128, N], F32, "th2")
                ki = k.sb(s2, [128, N], I32, "ki")
                kf = k.sb(s2, [128, N], F32, "kf")
                sn = k.sb(s2, [128, N], F32, "sn")
                cs = k.sb(s2, [128, N], F32, "cs")
                t1 = k.sb(s2, [128, N], F32, "t1")
                t2 = k.sb(s2, [128, N], F32, "t2")
                cR = k.sb(s2, [128, N], F32, "cR")
                cI = k.sb(s2, [128, N], F32, "cI")
                are, aim = bl[:, 0, :], bl[:, 1, :]
                k.actf(dt[:], bl[:, 2, :], AF.Exp, [bl], [dt])
                k.tt(k.dve, th[:], aim, dt[:], ALU.mult, [bl, dt], [th])
                k.tt(k.dve, dt[:], are, dt[:], ALU.mult, [bl, dt], [dt])
                k.actf(r[:], dt[:], AF.Exp, [dt], [r])
                self.ang_reduce(th, th[:], ki, ki[:], kf, kf[:])
                k.ts(k.dve, th2[:], th[:], PI / 2, None, ALU.add, None, [th], [th2])
                self.ang_reduce(th2, th2[:], ki, ki[:], kf, kf[:])
                k.actf(sn[:], th[:], AF.Sin, [th], [sn])
                k.actf(cs[:], th2[:], AF.Sin, [th2], [cs])
                k.tt(k.dve, cs[:], r[:], cs[:], ALU.mult, [r, cs], [cs])
                k.ts(k.dve, cs[:], cs[:], -1.0, None, ALU.add, None, [cs], [cs])
                k.tt(k.dve, sn[:], r[:], sn[:], ALU.mult, [r, sn], [sn])
                k.tt(k.dve, t1[:], are, are, ALU.mult, [bl], [t1])
                k.tt(k.dve, t2[:], aim, aim, ALU.mult, [bl], [t2])
                k.tt(k.dve, t1[:], t1[:], t2[:], ALU.add, [t1, t2], [t1])
                k.op(k.dve, lambda e: e.reciprocal(out=t1[:], in_=t1[:]), [t1], [t1])
                k.tt(k.dve, cR[:], cs[:], are, ALU.mult, [cs, bl], [cR])
                k.tt(k.dve, t2[:], sn[:], aim, ALU.mult, [sn, bl], [t2])
                k.tt(k.dve, cR[:], cR[:], t2[:], ALU.add, [cR, t2], [cR])
                k.tt(k.dve, cR[:], cR[:], t1[:], ALU.mult, [cR, t1], [cR])
                k.tt(k.dve, cI[:], sn[:], are, ALU.mult, [sn, bl], [cI])
                k.tt(k.dve, t2[:], cs[:], aim, ALU.mult, [cs, bl], [t2])
                k.tt(k.dve, cI[:], cI[:], t2[:], ALU.subtract, [cI, t2], [cI])
                k.tt(k.dve, cI[:], cI[:], t1[:], ALU.mult, [cI, t1], [cI])
                bc = lambda t: t[:].rearrange("p (k q) -> p k q", k=8).unsqueeze(2).broadcast_to([128, 8, 2, 64])
                v4 = lambda ap: ap.rearrange("p k (s q) -> p k s q", s=2)
                u1 = k.sb(s2, [128, 8, 128], F32, "u1")
                u2 = k.sb(s2, [128, 8, 128], F32, "u2")
                k.tt(k.dve, v4(u1[:]), v4(bt[:, 0]), bc(cR), ALU.mult, [bt, cR], [u1])
                k.tt(k.dve, v4(u2[:]), v4(bt[:, 1]), bc(cI), ALU.mult, [bt, cI], [u2])
                k.tt(k.dve, P["BT"][:, 0], u1[:], u2[:], ALU.subtract, [u1, u2], [P["BT"]])
                k.tt(k.dve, v4(u1[:]), v4(bt[:, 1]), bc(cR), ALU.mult, [bt, cR], [u1])
                k.tt(k.dve, v4(u2[:]), v4(bt[:, 0]), bc(cI), ALU.mult, [bt, cI], [u2])
                k.tt(k.dve, P["BT"][:, 1], u1[:], u2[:], ALU.add, [u1, u2], [P["BT"]])
                k.ts(k.dve, P["BT3"][:], P["BT"][:], msk[:, 0:1], None, ALU.mult, None, [P["BT"], msk], [P["BT3"]])
                if d == 0:
                    self.dump("R", P["R"], P["R"][:], [128, 32])
                    self.dump("TH", P["TH"], P["TH"][:], [128, 32])
                    self.dump("cR", cR, cR[:], [128, 512])
                    self.dump("cI", cI, cI[:], [128, 512])
                    self.dump("BT", P["BT"], P["BT"][:], [128, 2, 8, 128], BF16)
                    self.dump("CT", P["CT"], P["CT"][:], [128, 2, 32, 64], BF16)
        return prm

    def phase_s5(self, l):
        k = self.k
        UT, Y5T = self.dram["UT"], self.dram["Y5T"]
        with self.k.scope() as st:
            prm = self.s5_params(l, st)
            iot = k.sb(st, [128, 513], F32, "iota", dma=True)
            k.dma(k.sp, [(iot[:], self.dram["cst2"][:, :])], iot, reads=[self.dbuf["cst2"]], writes=[iot])
            dsk = k.sb(st, [128, 8], F32, "dsk", dma=True)
            bgl = k.sb(st, [128, 8], F32, "bgl", dma=True)
            k.dma(k.sp, [(dsk[:], self.dram["s5d_l"][l])], dsk, reads=[self.dbuf["s5d_l"]], writes=[dsk])
            k.dma(k.sp, [(bgl[:], self.dram["bglu_l"][l])], bgl, reads=[self.dbuf["bglu_l"]], writes=[bgl])
            WG = None
            for b in range(NB):
                with self.k.scope() as sb_:
                    Y = k.sb(sb_, [128, 8, TOK], F32, "Y")
                    uTb = k.sb(sb_, [128, 8, TOK], BF16, "uTb", dma=True)
                    k.dma(k.pool, [(uTb[:, kk, :], UT[kk * 128:(kk + 1) * 128, b * TOK:(b + 1) * TOK]) for kk in range(8)],
                          uTb, reads=[self.dbuf["UT"]], writes=[uTb], max_dma_last_dim=4096)
                    with self.k.scope() as sc:
                        self.s5_scan(sc, prm, iot, Y, uTb)
                    if b == 1:
                        self.dump("Y", Y, Y[:], [128, 8, TOK])
                    self.s5_glu(sb_, l, b, Y, uTb, dsk, bgl, WG)
        k.barrier()

    def s5_scan(self, sc, prm, iot, Y, uTb):
        k = self.k
        SIN = [k.sb(sc, [128, 513], F32, "SIN") for _ in range(2)]
        COS = [k.sb(sc, [128, 513], F32, "COS") for _ in range(2)]
        ang = k.sb(sc, [128, 513], F32, "ang")
        ki = k.sb(sc, [128, 513], I32, "aki")
        kf = k.sb(sc, [128, 513], F32, "akf")
        XR = [k.ps(sc, [128, 512]) for _ in range(2)]
        XI = [k.ps(sc, [128, 512]) for _ in range(2)]
        YP = [k.ps(sc, [128, 512]) for _ in range(2)]
        t1 = k.sb(sc, [128, 512], F32, "m1")
        t2 = k.sb(sc, [128, 512], F32, "m2")
        t3 = k.sb(sc, [128, 512], F32, "m3")
        t4 = k.sb(sc, [128, 512], F32, "m4")
        xr = [k.sb(sc, [128, 512], F32, "xr") for _ in range(2)]
        xi = [k.sb(sc, [128, 512], F32, "xi") for _ in range(2)]
        gR = [k.sb(sc, [128, 512], F32, "gR") for _ in range(2)]
        gI = [k.sb(sc, [128, 512], F32, "gI") for _ in range(2)]
        hR = [k.sb(sc, [128, 512], BF16, "hR") for _ in range(2)]
        hI = [k.sb(sc, [128, 512], BF16, "hI") for _ in range(2)]
        ini = [k.sb(sc, [128, 2], F32, "ini") for _ in range(2)]
        us = k.sb(sc, [128, 2], F32, "us")
        blocks_f = [(0, 256)] + [(256 + q * 512, 512) for q in range(4)]
        blocks_b = [(0, 256)] + [(256 + q * 512, 512) for q in (3, 2, 1, 0)]
        n_unit = 0
        n_tab = 0
        for d in range(2):
            P = prm[d]
            for j in [4 * a + b_ for a in range(8) for b_ in (3, 2, 1, 0)]:
                kk, jl = j // 4, j % 4
                rows = slice(32 * jl, 32 * jl + 32) if jl < 3 else slice(64, 128)
                BTt = P["BT"] if jl < 3 else P["BT3"]
                ccols = slice(32, 64) if jl < 3 else slice(0, 64)
                S, C = SIN[n_tab % 2], COS[n_tab % 2]
                n_tab += 1
                k.ts(k.dve, ang[:], iot[:], P["TH"][:, j:j + 1], None, ALU.mult, None, [iot, P["TH"]], [ang])
                self.ang_reduce(ang, ang[:], ki, ki[:], kf, kf[:])
                k.actf(S[:], ang[:], AF.Sin, [ang], [S])
                k.ts(k.dve, ang[:], ang[:], PI / 2, None, ALU.add, None, [ang], [ang])
                self.ang_reduce(ang, ang[:], ki, ki[:], kf, kf[:])
                k.actf(C[:], ang[:], AF.Sin, [ang], [C])
                if d == 0 and j == 3:
                    self.dump("SIN", S, S[:], [128, 513])
                    self.dump("COS", C, C[:], [128, 513])
                first = True
                for (t0, n) in (blocks_f if d == 0 else blocks_b):
                    u = n_unit % 2
                    n_unit += 1
                    if d == 0:
                        cols = slice(t0, t0 + n)
                    else:
                        cols = slice(t0 + n - 1, (t0 - 1) if t0 > 0 else None, -1)
                    k.mm(XR[u][:, 0:n], BTt[rows, 0, kk, :], uTb[rows, kk, cols], True, True, [BTt, uTb], [XR[u]])
                    k.mm(XI[u][:, 0:n], BTt[rows, 1, kk, :], uTb[rows, kk, cols], True, True, [BTt, uTb], [XI[u]])
                    k.tt(k.dve, t1[:, 0:n], XR[u][:, 0:n], C[:, 0:n], ALU.mult, [XR[u], C], [t1])
                    k.tt(k.dve, t2[:, 0:n], XI[u][:, 0:n], S[:, 0:n], ALU.mult, [XI[u], S], [t2])
                    k.tt(k.dve, t3[:, 0:n], XI[u][:, 0:n], C[:, 0:n], ALU.mult, [XI[u], C], [t3])
                    k.tt(k.dve, t4[:, 0:n], XR[u][:, 0:n], S[:, 0:n], ALU.mult, [XR[u], S], [t4])
                    k.tt(k.pool, xr[u][:, 0:n], t1[:, 0:n], t2[:, 0:n], ALU.add, [t1, t2], [xr[u]])
                    k.tt(k.pool, xi[u][:, 0:n], t3[:, 0:n], t4[:, 0:n], ALU.subtract, [t3, t4], [xi[u]])
                    rb = P["R"][:, j:j + 1].broadcast_to([128, n])
                    iv = ini[0]
                    i0 = 0.0 if first else iv[:, 0:1]
                    i1 = 0.0 if first else iv[:, 1:2]
                    rd = [P["R"], xr[u]] + ([] if first else [iv])
                    k.op(k.dve, lambda e, u=u, n=n, rb=rb, i0=i0: e.tensor_tensor_scan(
                        out=gR[u][:, 0:n], data0=rb, data1=xr[u][:, 0:n], initial=i0, op0=ALU.mult, op1=ALU.add),
                        rd, [gR[u]])
                    rd = [P["R"], xi[u]] + ([] if first else [iv])
                    k.op(k.dve, lambda e, u=u, n=n, rb=rb, i1=i1: e.tensor_tensor_scan(
                        out=gI[u][:, 0:n], data0=rb, data1=xi[u][:, 0:n], initial=i1, op0=ALU.mult, op1=ALU.add),
                        rd, [gI[u]])
                    first = False
                    cT, sT = C[:, n:n + 1], S[:, n:n + 1]
                    k.ts(k.dve, us[:, 0:1], gI[u][:, n - 1:n], sT, None, ALU.mult, None, [gI[u], S], [us])
                    k.ts(k.dve, us[:, 1:2], gI[u][:, n - 1:n], cT, None, ALU.mult, None, [gI[u], C], [us])
                    k.stt(iv[:, 0:1], gR[u][:, n - 1:n], cT, us[:, 0:1], ALU.mult, ALU.subtract, [gR[u], C, us], [iv])
                    k.stt(iv[:, 1:2], gR[u][:, n - 1:n], sT, us[:, 1:2], ALU.mult, ALU.add, [gR[u], S, us], [iv])
                    k.tt(k.dve, t1[:, 0:n], gR[u][:, 0:n], C[:, 0:n], ALU.mult, [gR[u], C], [t1])
                    k.tt(k.pool, t2[:, 0:n], gI[u][:, 0:n], S[:, 0:n], ALU.mult, [gI[u], S], [t2])
                    k.tt(k.pool, hR[u][:, 0:n], t1[:, 0:n], t2[:, 0:n], ALU.subtract, [t1, t2], [hR[u]])
                    k.tt(k.pool, t3[:, 0:n], gR[u][:, 0:n], S[:, 0:n], ALU.mult, [gR[u], S], [t3])
                    k.tt(k.dve, t4[:, 0:n], gI[u][:, 0:n], C[:, 0:n], ALU.mult, [gI[u], C], [t4])
                    k.stt(hI[u][:, 0:n], t3[:, 0:n], -1.0, t4[:, 0:n], ALU.mult, ALU.subtract, [t3, t4], [hI[u]])
                    if d == 0 and j == 3 and t0 == 256:
                        self.dump("xr", xr[u], xr[u][:], [128, 512])
                        self.dump("gR", gR[u], gR[u][:], [128, 512])
                        self.dump("hR", hR[u], hR[u][:], [128, 512], BF16)
                    k.mm(YP[u][rows, 0:n], P["CT"][:, 0, j, ccols], hR[u][:, 0:n], True, False, [P["CT"], hR[u]], [YP[u]])
                    k.mm(YP[u][rows, 0:n], P["CT"][:, 1, j, ccols], hI[u][:, 0:n], False, True, [P["CT"], hI[u]], [YP[u]])
                    if d == 0:
                        k.cp(k.act, Y[rows, kk, t0:t0 + n], YP[u][rows, 0:n], [YP[u]], [Y])
                    else:
                        k.tt(k.dve, Y[rows, kk, t0:t0 + n], YP[u][rows, n - 1::-1], Y[rows, kk, t0:t0 + n], ALU.add,
                             [YP[u], Y], [Y])

    def s5_glu(self, sb_, l, b, Y, uTb, dsk, bgl, WG):
        k = self.k
        UT, Y5T = self.dram["UT"], self.dram["Y5T"]
        WG = k.sb(sb_, [128, 8, 1024], BF16, "WG", dma=True)
        k.dma(k.pool, [(WG[:], self.dram["s5_w_glu"][l].rearrange("(kc p) n -> p kc n", p=128))], WG,
              reads=[self.dbuf["s5_w_glu"]], writes=[WG])
        uf = [k.sb(sb_, [128, TOK], F32, "uf", dma=True) for _ in range(2)]
        z = uTb
        for kk in range(8):
            f = uf[kk % 2]
            k.dma(k.sp, [(f[:], UT[kk * 128:(kk + 1) * 128, b * TOK:(b + 1) * TOK])], f, reads=[self.dbuf["UT"]], writes=[f])
            k.stt(Y[:, kk, :], f[:], dsk[:, kk:kk + 1], Y[:, kk, :], ALU.mult, ALU.add, [f, dsk, Y], [Y])
            k.actf(z[:, kk, :], Y[:, kk, :], AF.Gelu_apprx_tanh, [Y], [z])
        pss = [k.ps(sb_, [128, 512]) for _ in range(2)]
        sg = [k.sb(sb_, [128, 512], F32, "sg") for _ in range(2)]
        ob = [k.sb(sb_, [128, 512], BF16, "ob", dma=True) for _ in range(2)]
        blocks = [(0, 256)] + [(256 + q * 512, 512) for q in range(4)]
        n_ = 0
        for oc in range(8):
            for (t0, n) in blocks:
                p = pss[n_ % 2]
                s_ = sg[n_ % 2]
                o = ob[n_ % 2]
                n_ += 1
                for kc in range(8):
                    k.mm(p[:, 0:n], WG[:, kc, oc * 128:(oc + 1) * 128], z[:, kc, t0:t0 + n], kc == 0, kc == 7, [WG, z], [p])
                k.actf(s_[:, 0:n], p[:, 0:n], AF.Sigmoid, [p, bgl], [s_], bias=bgl[:, oc:oc + 1])
                k.tt(k.dve, o[:, 0:n], s_[:, 0:n], z[:, oc, t0:t0 + n], ALU.mult, [s_, z], [o])
                k.dma(k.sp, [(Y5T[oc * 128:(oc + 1) * 128, b * TOK + t0:b * TOK + t0 + n], o[:, 0:n])], o,
                      reads=[o], writes=[self.dbuf["Y5T"]], accumulate=True)


    def phase_hg_lb(self, st):
        k = self.k
        self.LBT = k.sb(st, [128, DEPTH, 16], F32, "LBT")
        self.OML = k.sb(st, [128, DEPTH, 16], F32, "OML")
        with self.k.scope() as s2:
            gam = k.sb(s2, [128, DEPTH, 16], F32, "gam", dma=True)
            k.dma(k.sp, [(gam[:], self.dram["gam_l"][:, :, :])], gam, reads=[self.dbuf["gam_l"]], writes=[gam])
            e = k.sb(s2, [128, DEPTH, 16], F32, "ge")
            sm = k.sb(s2, [128, 16], F32, "gs")
            k.actf(e[:], gam[:], AF.Exp, [gam], [e])
            k.tt(k.dve, sm[:], e[:, 0, :], e[:, 1, :], ALU.add, [e], [sm])
            for i in range(2, DEPTH):
                k.tt(k.dve, sm[:], sm[:], e[:, i, :], ALU.add, [e, sm], [sm])
            k.op(k.dve, lambda en: en.reciprocal(out=sm[:], in_=sm[:]), [sm], [sm])
            k.op(k.dve, lambda en: en.memset(self.LBT[:], 0.0), [], [self.LBT])
            for i in range(1, DEPTH):
                k.tt(k.dve, e[:, i, :], e[:, i, :], sm[:], ALU.mult, [e, sm], [e])
                k.tt(k.dve, self.LBT[:, i, :], self.LBT[:, i - 1, :], e[:, i, :], ALU.add, [e, self.LBT], [self.LBT])
            k.ts(k.dve, self.OML[:], self.LBT[:], -1.0, 1.0, ALU.mult, ALU.add, [self.LBT], [self.OML])

    def phase_hgrn(self, l):
        k = self.k
        with self.k.scope() as st:
            ghg = k.sb(st, [128, 8], F32, "ghg", dma=True)
            k.dma(k.sp, [(ghg[:], self.dram["ghg_l"][l])], ghg, reads=[self.dbuf["ghg_l"]], writes=[ghg])
            pat = k.sb(st, [128, 32], F32, "pat")
            k.op(k.dve, lambda e: e.memset(pat[:], 1.0), [], [pat])
            k.op(k.dve, lambda e: e.memset(pat[:, 0:1], 0.0), [], [pat])
            mask = k.sb(st, [128, TOK + 32], F32, "mask")
            k.cp(k.dve, mask[:].rearrange("p (n s) -> p n s", s=32), pat[:].unsqueeze(1).broadcast_to([128, TOK // 32 + 1, 32]),
                 [pat], [mask])
            mk = k.sb(st, [64, 2, 64], F32, "mk", dma=True)
            k.dma(k.sp, [(mk[:, 0, :], self.dram["cst"][1, 0:64, 0:64]), (mk[:, 1, :], self.dram["cst"][2, 0:64, 0:64])], mk,
                  reads=[self.dbuf["cst"]], writes=[mk])
            onesq = k.sb(st, [128, 128], F32, "onesq")
            k.op(k.dve, lambda e: e.memset(onesq[:], 1.0), [], [onesq])
            for b in range(NB):
                for h in range(8):
                    if "hg_heads" in self.dbg and (b, h) not in self.dbg["hg_heads"]:
                        continue
                    with self.k.scope() as sh:
                        self.hg_head(sh, l, b, h, ghg, mask, mk, onesq)

    def hg_head(self, sh, l, b, h, ghg, mask, mk, onesq):
        k = self.k
        QFT, VOG, YHT = self.dram["QFT"], self.dram["VOG"], self.dram["YHT"]
        c0 = b * TOK
        NCH = TOK // 32
        NG = TOK // 64
        qraw = k.sb(sh, [128, TOK], F32, "qraw", dma=True)
        fraw = k.sb(sh, [128, TOK], F32, "fraw", dma=True)
        qP = k.sb(sh, [128, TOK], F32, "qP")
        sogP = k.sb(sh, [128, TOK], F32, "sogP")
        T1 = k.sb(sh, [128, TOK], F32, "T1")
        T2 = k.sb(sh, [128, TOK], F32, "T2")
        T3 = k.sb(sh, [128, TOK], F32, "T3")
        T4 = k.sb(sh, [128, TOK], F32, "T4")
        T5 = k.sb(sh, [128, TOK], F32, "T5")
        A = k.sb(sh, [128, TOK], BF16, "A")
        Bt = k.sb(sh, [128, TOK], BF16, "B")
        KD = k.sb(sh, [64, NG, 128], BF16, "KD")
        VT = k.sb(sh, [64, NG, 128], BF16, "VT", dma=True)
        QB = [k.sb(sh, [128, TOK], BF16, "QB") for _ in range(2)]
        SIN_ = [k.sb(sh, [128, NCH, 128], BF16, "Sin") for _ in range(2)]
        SC = [k.sb(sh, [64, NG, 64], BF16, "SC") for _ in range(2)]
        dec = k.sb(sh, [128, NCH], F32, "dec")
        S = k.sb(sh, [128, 128], F32, "S")
        YR = k.sb(sh, [128, TOK], BF16, "YR", dma=True)
        PT = [k.ps(sh, [128, 512]) for _ in range(2)]
        KV = [k.ps(sh, [128, 512]) for _ in range(4)]
        PO = [k.ps(sh, [128, 512]) for _ in range(2)]
        rows = slice(h * 128, (h + 1) * 128)
        k.dma(k.sp, [(qraw[:], QFT[rows, c0:c0 + TOK])], qraw, reads=[self.dbuf["QFT"]], writes=[qraw])
        k.dma(k.sp, [(fraw[:], QFT[3072 + h * 128:3072 + (h + 1) * 128, c0:c0 + TOK])], fraw, reads=[self.dbuf["QFT"]], writes=[fraw])
        vsrc = VOG[c0 + CTX:c0 + TOK, rows].rearrange("(r g c) d -> c r g d", g=32, c=2)
        k.dma(k.pool, [(VT[0:64, 0:4, :], VOG[c0:c0 + CTX, rows].rearrange("(g s) d -> s g d", s=64)),
                       (VT[0:32, 4:NG, :], vsrc[0]), (VT[32:64, 4:NG, :], vsrc[1])], VT,
              reads=[self.dbuf["VOG"]], writes=[VT])

        def toP(func, o, i):
            k.actf(o[:, 0:CTX], i[:, 0:CTX], func, [i], [o])
            k.actf(o[:, CTX:].rearrange("p (c r) -> p c r", r=32), i[:, CTX:].rearrange("p (r c) -> p c r", c=64), func, [i], [o])

        toP(AF.Silu, qP, qraw)
        toP(AF.Silu, sogP, fraw)
        segs = [(0, CTX), (CTX, TOK)]
        n_pt = 0
        n_kv = 0
        for d in range(2):
            col = d * 8 + h
            r0 = 1024 * (1 + d) + h * 128
            k.dma(k.sp, [(fraw[:], QFT[r0:r0 + 128, c0:c0 + TOK])], fraw, reads=[self.dbuf["QFT"]], writes=[fraw])
            toP(AF.Sigmoid, T1, fraw)
            k.ts(k.dve, T1[:], T1[:], self.OML[:, l, col:col + 1], self.LBT[:, l, col:col + 1], ALU.mult, ALU.add,
                 [T1, self.OML, self.LBT], [T1])
            k.actf(T2[:], T1[:], AF.Ln, [T1], [T2])
            k.ts(k.pool, T1[:], T1[:], -1.0, 1.0, ALU.mult, ALU.add, [T1], [T1])
            for (a, e_) in segs:
                sl = slice(a, e_) if d == 0 else slice(e_ - 1, (a - 1) if a > 0 else None, -1)
                slm = slice(a, e_) if d == 0 else slice(e_, a, -1)
                k.op(k.dve, lambda en, sl=sl, slm=slm: en.tensor_tensor_scan(out=T3[:, sl], data0=mask[:, slm], data1=T2[:, sl],
                                                                 initial=0.0, op0=ALU.mult, op1=ALU.add), [mask, T2], [T3])
            b3 = T3[:].rearrange("p (n s) -> p n s", s=32)
            iL, iR = (31, 15) if d == 0 else (0, 16)
            BL3 = b3[:, :, iL:iL + 1].broadcast_to([128, NCH, 32])
            BR3 = b3[:, :, iR:iR + 1].broadcast_to([128, NCH, 32])
            v3 = lambda t: t[:].rearrange("p (n s) -> p n s", s=32)
            k.tt(k.dve, v3(T4), BL3, b3, ALU.subtract, [T3], [T4])
            k.actf(T4[:], T4[:], AF.Exp, [T4], [T4])
            k.tt(k.pool, A[:], T1[:], T4[:], ALU.mult, [T1, T4], [A])
            k.actf(dec[:], b3[:, :, iL], AF.Exp, [T3], [dec])
            if d == self.dbg.get("hg_dd", 0):
                self.dump("hg_f", T1, T1[:], [128, TOK])
                self.dump("hg_b", T3, T3[:], [128, TOK])
                self.dump("hg_dec", dec, dec[:], [128, NCH])
                self.dump("hg_kdec", A, A[:], [128, TOK], BF16)
                self.dump("hg_qP", qP, qP[:], [128, TOK])
                self.dump("hg_VT", VT, VT[:], [64, NG, 128], BF16)
            if self.dbg.get("hg_stop", 9) <= 1:
                continue
            for q in range(NG // 4):
                p = PT[n_pt % 2]
                n_pt += 1
                for jj in range(4):
                    g = q * 4 + jj
                    k.mm(p[0:64, jj * 128:(jj + 1) * 128], A[:, 64 * g:64 * g + 64], self.ident_b[:], True, True,
                         [A, self.ident_b], [p])
                k.cp(k.act, KD[:, q * 4:(q + 1) * 4, :], p[0:64, :].rearrange("p (a b) -> p a b", a=4), [p], [KD])
            if self.dbg.get("hg_sub", 9) <= 1:
                continue
            order = list(range(NCH)) if d == 0 else (list(range(7, -1, -1)) + list(range(NCH - 1, 7, -1)))
            k.op(k.dve, lambda en: en.memset(S[:], 0.0), [], [S])
            for i8 in range(0, NCH, 8):
                pc = [KV[(n_kv % 2) * 2 + 0], KV[(n_kv % 2) * 2 + 1]]
                n_kv += 1
                for jj in range(8):
                    n = order[i8 + jj]
                    g, c = n // 2, n % 2
                    sl_ = (jj // 2) * 128
                    k.mm(pc[c][:, sl_:sl_ + 128], KD[32 * c:32 * c + 32, g, :], VT[32 * c:32 * c + 32, g, :], True, True,
                         [KD, VT], [pc[c]])
                if self.dbg.get("hg_sub", 9) <= 2:
                    continue
                for jj in range(8):
                    n = order[i8 + jj]
                    c = n % 2
                    sl_ = (jj // 2) * 128
                    k.cp(k.act, SIN_[d][:, n, :], S[:], [S], [SIN_[d]])
                    k.stt(S[:], S[:], dec[:, n:n + 1], pc[c][:, sl_:sl_ + 128], ALU.mult, ALU.add, [S, dec, pc[c]], [S])
            if self.dbg.get("hg_stop", 9) <= 2:
                continue
            k.tt(k.dve, v3(T4), b3, BR3, ALU.subtract, [T3], [T4])
            k.actf(T5[:], T4[:], AF.Exp, [T4], [T5])
            k.actf(T4[:], T4[:], AF.Exp, [T4], [T4], scale=-1.0)
            k.tt(k.pool, A[:], qP[:], T5[:], ALU.mult, [qP, T5], [A])
            k.tt(k.dve, Bt[:], T1[:], T4[:], ALU.mult, [T1, T4], [Bt])
            for q in range((NG + 7) // 8):
                p = PT[n_pt % 2]
                n_pt += 1
                ng = min(8, NG - q * 8)
                for jj in range(ng):
                    g = q * 8 + jj
                    k.mm(p[0:64, jj * 64:(jj + 1) * 64], Bt[:, 64 * g:64 * g + 64], A[:, 64 * g:64 * g + 64], True, True,
                         [A, Bt], [p])
                k.tt(k.dve, SC[d][:, q * 8:q * 8 + ng, :], p[0:64, 0:ng * 64].rearrange("p (a b) -> p a b", b=64),
                     mk[:, d, :].unsqueeze(1).broadcast_to([64, ng, 64]), ALU.mult, [p, mk], [SC[d]])
            k.actf(T5[:], T3[:], AF.Exp, [T3], [T5])
            k.tt(k.pool, QB[d][:], qP[:], T5[:], ALU.mult, [qP, T5], [QB[d]])
            if d == self.dbg.get("hg_dd", 0):
                self.dump("hg_KD", KD, KD[:], [64, NG, 128], BF16)
                self.dump("hg_sin", SIN_[d], SIN_[d][:], [128, NCH, 128], BF16)
                self.dump("hg_sc", SC[d], SC[d][:], [64, NG, 64], BF16)
                self.dump("hg_qb", QB[d], QB[d][:], [128, TOK], BF16)
        if self.dbg.get("hg_stop", 9) <= 3:
            return
        n_po = 0
        for q in range((NG + 7) // 8):
            p = PO[n_po % 2]
            n_po += 1
            ng = min(8, NG - q * 8)
            for jj in range(ng):
                g = q * 8 + jj
                o = lambda a_, b_: p[:, jj * 64 + a_:jj * 64 + b_]
                k.mm(o(0, 64), VT[0:64, g, :], SC[0][:, g, :], True, False, [VT, SC[0]], [p])
                k.mm(o(0, 64), VT[0:64, g, :], SC[1][:, g, :], False, False, [VT, SC[1]], [p])
                for d in range(2):
                    for c in range(2):
                        n = 2 * g + c
                        k.mm(o(32 * c, 32 * c + 32), SIN_[d][:, n, :], QB[d][:, 32 * n:32 * n + 32], False, (d == 1 and c == 1),
                             [SIN_[d], QB[d]], [p])
            k.tt(k.dve, T1[:, q * 512:q * 512 + ng * 64], p[:, 0:ng * 64], sogP[:, q * 512:q * 512 + ng * 64], ALU.mult,
                 [p, sogP], [T1])
        self.dump("hg_y", T1, T1[:], [128, TOK])
        if self.dbg.get("hg_stop", 9) <= 4:
            return
        k.actf(T2[:], T1[:], AF.Square, [T1], [T2])
        blocks = [(q * 512, 512) for q in range(4)] + [(2048, 256)]
        for (t0, n) in blocks:
            p = PT[n_pt % 2]
            n_pt += 1
            k.mm(p[:, 0:n], onesq[:], T2[:, t0:t0 + n], True, True, [onesq, T2], [p])
            k.ts(k.dve, T3[:, t0:t0 + n], p[:, 0:n], 1.0 / 128, EPS, ALU.mult, ALU.add, [p], [T3])
        k.actf(T3[:], T3[:], AF.Sqrt, [T3], [T3])
        k.op(k.dve, lambda en: en.reciprocal(out=T3[:], in_=T3[:]), [T3], [T3])
        k.stt(T2[:], T1[:], ghg[:, h:h + 1], T3[:], ALU.mult, ALU.mult, [T1, ghg, T3], [T2])
        k.cp(k.act, YR[:, 0:CTX], T2[:, 0:CTX], [T2], [YR])
        k.cp(k.pool, YR[:, CTX:].rearrange("p (r c) -> p c r", c=64), T2[:, CTX:].rearrange("p (c r) -> p c r", r=32), [T2], [YR])
        k.dma(k.sp, [(YHT[rows, c0:c0 + TOK], YR[:])], YR, reads=[YR], writes=[self.dbuf["YHT"]], accumulate=True)


    def phase_merge(self, l):
        k = self.k
        X, MODS, GT, Y5T, YHT = (self.dram[n] for n in ("X", "MODS", "GT", "Y5T", "YHT"))
        blocks = [(0, 256)] + [(256 + q * 512, 512) for q in range(4)]
        wv = lambda name: self.dram[name][l].rearrange("(kc p) n -> p kc n", p=128)
        for b in range(NB):
            c0 = b * TOK
            with self.k.scope() as st:
                mT = k.sb(st, [128, 16, TOK], BF16, "mT")
                with self.k.scope() as s1:
                    WBS = k.sb(s1, [128, 8, D], BF16, "WBS", dma=True)
                    WBH = k.sb(s1, [128, 8, D], BF16, "WBH", dma=True)
                    for (W, nm) in ((WBS, "w_branch_s5"), (WBH, "w_branch_hg")):
                        k.dma(k.pool, [(W[:, 2 * i:2 * i + 2, :], wv(nm)[:, 2 * i:2 * i + 2, :]) for i in range(4)], W,
                              reads=[self.dbuf[nm]], writes=[W])
                    y5 = [k.sb(s1, [128, 8, 512], BF16, "y5", dma=True) for _ in range(2)]
                    yh = [k.sb(s1, [128, 8, 512], BF16, "yh", dma=True) for _ in range(2)]
                    g5 = [k.sb(s1, [128, 512], F32, "g5", dma=True) for _ in range(2)]
                    gh = [k.sb(s1, [128, 512], F32, "gh", dma=True) for _ in range(2)]
                    t1 = [k.sb(s1, [128, 512], F32, "t1") for _ in range(2)]
                    t2 = [k.sb(s1, [128, 512], F32, "t2") for _ in range(2)]
                    pa = [k.ps(s1, [128, 512]) for _ in range(2)]
                    pb = [k.ps(s1, [128, 512]) for _ in range(2)]
                    n_ = 0
                    for bi, (t0, n) in enumerate(blocks):
                        a5, ah = y5[bi % 2], yh[bi % 2]
                        k.dma(k.sp, [(a5[:, :, 0:n], Y5T[:, c0 + t0:c0 + t0 + n].rearrange("(kc p) t -> p kc t", p=128))], a5,
                              reads=[self.dbuf["Y5T"]], writes=[a5])
                        k.dma(k.sp, [(ah[:, :, 0:n], YHT[:, c0 + t0:c0 + t0 + n].rearrange("(kc p) t -> p kc t", p=128))], ah,
                              reads=[self.dbuf["YHT"]], writes=[ah])
                        for oc in range(16):
                            u = n_ % 2
                            n_ += 1
                            k.dma(k.sp, [(g5[u][:, 0:n], GT[oc * 128:(oc + 1) * 128, c0 + t0:c0 + t0 + n])], g5[u],
                                  reads=[self.dbuf["GT"]], writes=[g5[u]])
                            k.dma(k.sp, [(gh[u][:, 0:n], GT[2048 + oc * 128:2048 + (oc + 1) * 128, c0 + t0:c0 + t0 + n])], gh[u],
                                  reads=[self.dbuf["GT"]], writes=[gh[u]])
                            for kc in range(8):
                                k.mm(pa[u][:, 0:n], WBS[:, kc, oc * 128:(oc + 1) * 128], a5[:, kc, 0:n], kc == 0, kc == 7, [WBS, a5], [pa[u]])
                            for kc in range(8):
                                k.mm(pb[u][:, 0:n], WBH[:, kc, oc * 128:(oc + 1) * 128], ah[:, kc, 0:n], kc == 0, kc == 7, [WBH, ah], [pb[u]])
                            k.tt(k.dve, t1[u][:, 0:n], pa[u][:, 0:n], g5[u][:, 0:n], ALU.mult, [pa[u], g5[u]], [t1[u]])
                            k.tt(k.dve, t2[u][:, 0:n], pb[u][:, 0:n], gh[u][:, 0:n], ALU.mult, [pb[u], gh[u]], [t2[u]])
                            k.tt(k.pool, mT[:, oc, t0:t0 + n], t1[u][:, 0:n], t2[u][:, 0:n], ALU.add, [t1[u], t2[u]], [mT])
                WO = k.sb(st, [128, 16, D], BF16, "WO", dma=True)
                k.dma(k.pool, [(WO[:, 2 * i:2 * i + 2, :], wv("w_out")[:, 2 * i:2 * i + 2, :]) for i in range(8)], WO,
                      reads=[self.dbuf["w_out"]], writes=[WO])
                gtm = [k.sb(st, [128, D], F32, "gtm", dma=True) for _ in range(2)]
                for j, bb in enumerate((NB, b)):
                    k.dma(k.sp, [(gtm[j][:], MODS[bb, :, 2 * D:3 * D])], gtm[j], reads=[self.dbuf["MODS"]], writes=[gtm[j]])
                xin = [k.sb(st, [128, D], F32, "xin", dma=True) for _ in range(2)]
                xo = [k.sb(st, [128, D], F32, "xo", dma=True) for _ in range(2)]
                tm = [k.sb(st, [128, 512], F32, "tm") for _ in range(2)]
                pso = [k.ps(st, [128, 512]) for _ in range(4)]
                n_ = 0
                for i in range(TOK // 128):
                    r0 = c0 + i * 128
                    xi_, xo_ = xin[i % 2], xo[i % 2]
                    g_ = gtm[0 if i < CTX // 128 else 1]
                    k.dma(k.sp, [(xi_[:], X[r0:r0 + 128, :])], xi_, reads=[self.dbuf["X"]], writes=[xi_])
                    for cc in range(4):
                        p = pso[n_ % 4]
                        tmv = tm[n_ % 2]
                        n_ += 1
                        cs = slice(cc * 512, (cc + 1) * 512)
                        for kc in range(16):
                            k.mm(p[:], mT[:, kc, i * 128:(i + 1) * 128], WO[:, kc, cs], kc == 0, kc == 15, [mT, WO], [p])
                        k.tt(k.dve, tmv[:], p[:], g_[:, cs], ALU.mult, [p, g_], [tmv])
                        k.tt(k.pool, xo_[:, cs], tmv[:], xi_[:, cs], ALU.add, [tmv, xi_], [xo_])
                    k.dma(k.sp, [(X[r0:r0 + 128, :], xo_[:])], xo_, reads=[xo_], writes=[self.dbuf["X"]], accumulate=True)

    def phase_peer(self, l):
        k = self.k
        X, MODS = self.dram["X"], self.dram["MODS"]
        UTAB, VTAB = self.dram["U16"], self.dram["V16"]
        with self.k.scope() as st:
            cds = k.dfree.pop(0)
            st.callback(lambda d_=cds: k.dfree.append(d_))
            for (src, dst, nm_, dn) in ((self.dram["peer_u"][l], UTAB, "peer_u", "U16"), (self.dram["peer_v"][l], VTAB, "peer_v", "V16")):
                k.dma(k.pool, [(dst[r_:r_ + 1024, :], src[r_:r_ + 1024, :]) for r_ in range(0, NEXP, 1024)], cds,
                      reads=[self.dbuf[nm_]], writes=[self.dbuf[dn]], accumulate=True)
            WQ = k.sb(st, [128, 16, D], BF16, "WQ", dma=True)
            k.dma(k.pool, [(WQ[:, 2 * i:2 * i + 2, :], self.dram["peer_w_q"][l].rearrange("(kc p) n -> p kc n", p=128)[:, 2 * i:2 * i + 2, :])
                           for i in range(8)], WQ, reads=[self.dbuf["peer_w_q"]], writes=[WQ])
            KT = k.sb(st, [128, 16, 128], BF16, "KT", dma=True)
            k.dma(k.pool, [(KT[:], self.dram["peer_kt"][l])], KT, reads=[self.dbuf["peer_kt"]], writes=[KT])
            io16 = k.sb(st, [128, 16], F32, "io16", dma=True)
            k.dma(k.sp, [(io16[:], self.dram["cst2"][:, 0:16])], io16, reads=[self.dbuf["cst2"]], writes=[io16])
            AF_ = k.sb(st, [128, D], F32, "AF", dma=True)
            SF_ = k.sb(st, [128, D], F32, "SF", dma=True)
            GF_ = k.sb(st, [128, D], F32, "GF", dma=True)
            xin = k.sb(st, [128, D], F32, "xin", dma=True)
            tn = k.sb(st, [128, D], F32, "tn", dma=True)
            xo = tn
            tmp = k.sb(st, [128, D], F32, "tmp")
            junk = k.sb(st, [128, D], BF16, "junk")
            tnb = k.sb(st, [128, D], BF16, "tnb")
            tnT = k.sb(st, [128, 16, 128], BF16, "tnT")
            qb = k.sb(st, [128, D], BF16, "qb")
            qT = k.sb(st, [128, 16, 128], BF16, "qT")
            Ssb = k.sb(st, [128, 16, 128], F32, "Ssb")
            ss = k.sb(st, [128, 1], F32, "ss")
            rs = k.sb(st, [128, 1], F32, "rs")
            NU = 4
            UB = [k.sb(st, [128, D], BF16, "UB", dma=True) for _ in range(NU)]
            VB = [k.sb(st, [128, D], BF16, "VB", dma=True) for _ in range(NU)]
            DJ = [k.sb(st, [128, 128], BF16, "DJ") for _ in range(2)]
            v8 = k.sb(st, [128, 2, 16], F32, "v8")
            i8 = k.sb(st, [128, 2, 16], U32, "i8")
            i8f = k.sb(st, [128, 2, 16], F32, "i8f")
            srep = k.sb(st, [128, 128], F32, "srep")
            cand = k.sb(st, [128, 256], F32, "cand")
            cand2 = k.sb(st, [128, 256], F32, "cand2")
            sc = k.sb(st, [128, 16], F32, "sc")
            ic = k.sb(st, [128, 16], U32, "ic")
            icf = k.sb(st, [128, 16], F32, "icf")
            hi_i = k.sb(st, [128, 16], I32, "hi_i")
            hif = k.sb(st, [128, 16], F32, "hif")
            lof = k.sb(st, [128, 16], F32, "lof")
            oh = k.sb(st, [128, 16, 16], F32, "oh")
            e1 = k.sb(st, [128, 16], F32, "e1")
            e2 = k.sb(st, [128, 16], F32, "e2")
            EXPf = k.sb(st, [128, 128], F32, "EXPf")
            EXPi = k.sb(st, [128, 128], I32, "EXPi")
            G = k.sb(st, [128, 128], F32, "G")
            nm = k.sb(st, [128, 1], F32, "nm")
            sm = k.sb(st, [128, 1], F32, "sm")
            araw = k.sb(st, [128, 128], F32, "araw")
            coef = k.sb(st, [128, 128], F32, "coef")
            pt = [k.ps(st, [128, 512]) for _ in range(3)]
            pacc = [k.ps(st, [128, 512]) for _ in range(4)]
            n_pt = 0
            cur_bb = None
            n_u = 0
            n_v = 0
            tiles = self.dbg.get("peer_tiles", list(range(NT // 128)))
            for ti in tiles:
                b, i = ti // (TOK // 128), ti % (TOK // 128)
                bb = NB if i < CTX // 128 else b
                if bb != cur_bb:
                    cur_bb = bb
                    k.dma(k.sp, [(AF_[:], MODS[bb, :, 4 * D:5 * D])], AF_, reads=[self.dbuf["MODS"]], writes=[AF_])
                    k.dma(k.sp, [(SF_[:], MODS[bb, :, 3 * D:4 * D])], SF_, reads=[self.dbuf["MODS"]], writes=[SF_])
                    k.dma(k.sp, [(GF_[:], MODS[bb, :, 5 * D:6 * D])], GF_, reads=[self.dbuf["MODS"]], writes=[GF_])
                r0 = ti * 128
                k.dma(k.sp, [(xin[:], X[r0:r0 + 128, :])], xin, reads=[self.dbuf["X"]], writes=[xin])
                self.norm_mod_tile((junk, ss, rs, tmp), xin, AF_, SF_, tn)
                k.cp(k.act, tnb[:], tn[:], [tn], [tnb])
                for q4 in range(4):
                    p = pt[n_pt % 3]
                    n_pt += 1
                    for jj in range(4):
                        kc = q4 * 4 + jj
                        k.mm(p[:, jj * 128:(jj + 1) * 128], tnb[:, kc * 128:(kc + 1) * 128], self.ident_b[:], True, True,
                             [tnb, self.ident_b], [p])
                    k.cp(k.act if q4 % 2 else k.dve, tnT[:, q4 * 4:(q4 + 1) * 4, :], p[:].rearrange("p (a b) -> p a b", a=4), [p], [tnT])
                for cc in range(4):
                    p = pt[n_pt % 3]
                    n_pt += 1
                    for kc in range(16):
                        k.mm(p[:], tnT[:, kc, :], WQ[:, kc, cc * 512:(cc + 1) * 512], kc == 0, kc == 15, [tnT, WQ], [p])
                    k.cp(k.act if cc % 2 else k.dve, qb[:, cc * 512:(cc + 1) * 512], p[:], [p], [qb])
                for q4 in range(4):
                    p = pt[n_pt % 3]
                    n_pt += 1
                    for jj in range(4):
                        kc = q4 * 4 + jj
                        k.mm(p[:, jj * 128:(jj + 1) * 128], qb[:, kc * 128:(kc + 1) * 128], self.ident_b[:], True, True,
                             [qb, self.ident_b], [p])
                    k.cp(k.act if q4 % 2 else k.dve, qT[:, q4 * 4:(q4 + 1) * 4, :], p[:].rearrange("p (a b) -> p a b", a=4), [p], [qT])
                for q4 in range(4):
                    p = pt[n_pt % 3]
                    n_pt += 1
                    for jj in range(4):
                        hh = q4 * 4 + jj
                        k.mm(p[:, jj * 128:(jj + 1) * 128], qT[:, hh, :], KT[:, hh, :], True, True, [qT, KT], [p])
                    k.cp(k.act, Ssb[:, q4 * 4:(q4 + 1) * 4, :], p[:].rearrange("p (a b) -> p a b", a=4), [p], [Ssb])
                for h in range(8):
                    for half in range(2):
                        sv = Ssb[:, 2 * h + half, :]
                        k.op(k.dve, lambda e, sv=sv, half=half: e.max(out=v8[:, half, 0:8], in_=sv), [Ssb], [v8])
                        k.op(k.dve, lambda e, sv=sv, half=half: e.max_index(out=i8[:, half, 0:8], in_max=v8[:, half, 0:8], in_values=sv),
                             [Ssb, v8], [i8])
                        k.op(k.dve, lambda e, sv=sv, half=half: e.match_replace(out=srep[:], in_to_replace=v8[:, half, 0:8],
                                                                              in_values=sv, imm_value=-1e30), [Ssb, v8], [srep])
                        k.op(k.dve, lambda e, half=half: e.max(out=v8[:, half, 8:16], in_=srep[:]), [srep], [v8])
                        k.op(k.dve, lambda e, half=half: e.max_index(out=i8[:, half, 8:16], in_max=v8[:, half, 8:16], in_values=srep[:]),
                             [srep, v8], [i8])
                    k.cp(k.dve, i8f[:], i8[:], [i8], [i8f])
                    k.tt(k.dve, cand[:].rearrange("p (a b) -> p a b", a=16), v8[:, 0, :].unsqueeze(2).broadcast_to([128, 16, 16]),
                         v8[:, 1, :].unsqueeze(1).broadcast_to([128, 16, 16]), ALU.add, [v8], [cand])
                    k.op(k.dve, lambda e: e.max(out=sc[:, 0:8], in_=cand[:]), [cand], [sc])
                    k.op(k.dve, lambda e: e.max_index(out=ic[:, 0:8], in_max=sc[:, 0:8], in_values=cand[:]), [cand, sc], [ic])
                    k.op(k.dve, lambda e: e.match_replace(out=cand2[:], in_to_replace=sc[:, 0:8], in_values=cand[:], imm_value=-1e30),
                         [cand, sc], [cand2])
                    k.op(k.dve, lambda e: e.max(out=sc[:, 8:16], in_=cand2[:]), [cand2], [sc])
                    k.op(k.dve, lambda e: e.max_index(out=ic[:, 8:16], in_max=sc[:, 8:16], in_values=cand2[:]), [cand2, sc], [ic])
                    k.cp(k.dve, icf[:], ic[:], [ic], [icf])
                    k.ts(k.dve, hi_i[:], icf[:], 1.0 / 16, -15.0 / 32, ALU.mult, ALU.add, [icf], [hi_i])
                    k.cp(k.dve, hif[:], hi_i[:], [hi_i], [hif])
                    k.stt(lof[:], hif[:], -16.0, icf[:], ALU.mult, ALU.add, [hif, icf], [lof])
                    for (sel, idxf, eo) in ((hif, i8f[:, 0, :], e1), (lof, i8f[:, 1, :], e2)):
                        k.tt(k.dve, oh[:], sel[:].unsqueeze(2).broadcast_to([128, 16, 16]),
                             io16[:].unsqueeze(1).broadcast_to([128, 16, 16]), ALU.is_equal, [sel, io16], [oh])
                        k.tt(k.dve, oh[:], oh[:], idxf.unsqueeze(1).broadcast_to([128, 16, 16]), ALU.mult, [oh, i8f], [oh])
                        k.op(k.dve, lambda e, eo=eo: e.tensor_reduce(out=eo[:], in_=oh[:], axis=AX.X, op=ALU.add), [oh], [eo])
                    k.stt(EXPf[:, h * 16:(h + 1) * 16], e1[:], 128.0, e2[:], ALU.mult, ALU.add, [e1, e2], [EXPf])
                    k.ts(k.dve, nm[:], sc[:, 0:1], -1.0, None, ALU.mult, None, [sc], [nm])
                    k.op(k.act, lambda e, h=h: e.activation(out=G[:, h * 16:(h + 1) * 16], in_=sc[:], func=AF.Exp, bias=nm[:, 0:1],
                                                            accum_out=sm[:]), [sc, nm], [G, sm])
                    k.op(k.dve, lambda e: e.reciprocal(out=sm[:], in_=sm[:]), [sm], [sm])
                    k.ts(k.dve, G[:, h * 16:(h + 1) * 16], G[:, h * 16:(h + 1) * 16], sm[:, 0:1], None, ALU.mult, None, [G, sm], [G])
                k.cp(k.dve, EXPi[:], EXPf[:], [EXPf], [EXPi])
                for j in range(128):
                    ub = UB[n_u % NU]
                    n_u += 1
                    self.idma(ub, UTAB, EXPi, j, "U16")
                    k.op(k.dve, lambda e, ub=ub, j=j: e.scalar_tensor_tensor(
                        out=junk[:], in0=ub[:], scalar=1.0, in1=tn[:], op0=ALU.mult, op1=ALU.mult,
                        accum_out=araw[:, j:j + 1]), [ub, tn], [junk, araw])
                k.actf(coef[:], araw[:], AF.Gelu_apprx_tanh, [araw], [coef])
                k.tt(k.dve, coef[:], coef[:], G[:], ALU.mult, [coef, G], [coef])
                for j in range(128):
                    vb = VB[n_v % NU]
                    dj = DJ[n_v % 2]
                    n_v += 1
                    self.idma(vb, VTAB, EXPi, j, "V16")
                    k.ts(k.dve, dj[:], self.ident_b[:], coef[:, j:j + 1], None, ALU.mult, None, [self.ident_b, coef], [dj])
                    for cc in range(4):
                        k.mm(pacc[cc][:], dj[:], vb[:, cc * 512:(cc + 1) * 512], j == 0, j == 127, [dj, vb], [pacc[cc]])
                for cc in range(4):
                    cs = slice(cc * 512, (cc + 1) * 512)
                    k.tt(k.dve, tmp[:, cs], pacc[cc][:], GF_[:, cs], ALU.mult, [pacc[cc], GF_], [tmp])
                    k.tt(k.pool, xo[:, cs], tmp[:, cs], xin[:, cs], ALU.add, [tmp, xin], [xo])
                k.dma(k.sp, [(X[r0:r0 + 128, :], xo[:])], xo, reads=[xo], writes=[self.dbuf["X"]], accumulate=True)

    def idma(self, dst, table, idx_tile, j, tname):
        k = self.k
        Q = k.pool
        ds = dst.dsem
        k._wait(Q, k._need([idx_tile, self.dbuf[tname]], [dst]))
        Q.eng.indirect_dma_start(out=dst[:], out_offset=None, in_=table,
                                 in_offset=bass.IndirectOffsetOnAxis(ap=idx_tile[:, j:j + 1], axis=0)).then_inc(ds.sem, 16)
        ds.count += 16
        k.ninst += 1
        k._mark([idx_tile, self.dbuf[tname]], [dst], ds.key, ds.sem, ds.count)

    def phase_final(self):
        k = self.k
        X, OUT = self.dram["X"], self.dram["out"]
        with self.k.scope() as st:
            gb = k.sb(st, [128, D], F32, "gb", dma=True)
            k.dma(k.sp, [(gb[:], self.dram["final_norm_g"][0].partition_broadcast(128))], gb,
                  reads=[self.dbuf["final_norm_g"]], writes=[gb])
            xin = [k.sb(st, [128, D], F32, "xin", dma=True) for _ in range(2)]
            xo = [k.sb(st, [128, D], F32, "xo", dma=True) for _ in range(2)]
            junk = k.sb(st, [128, D], BF16, "junk")
            ss = [k.sb(st, [128, 1], F32, "ss") for _ in range(2)]
            rs = [k.sb(st, [128, 1], F32, "rs") for _ in range(2)]
            n_ = 0
            for b in range(NB):
                for i in range(SEQ // 128):
                    u = n_ % 2
                    n_ += 1
                    r0 = b * TOK + CTX + i * 128
                    k.dma(k.sp, [(xin[u][:], X[r0:r0 + 128, :])], xin[u], reads=[self.dbuf["X"]], writes=[xin[u]])
                    k.op(k.act, lambda e, u=u: e.activation(out=junk[:], in_=xin[u][:], func=AF.Square, accum_out=ss[u][:]),
                         [xin[u]], [junk, ss[u]])
                    k.ts(k.dve, rs[u][:], ss[u][:], 1.0 / D, EPS, ALU.mult, ALU.add, [ss[u]], [rs[u]])
                    k.actf(rs[u][:], rs[u][:], AF.Sqrt, [rs[u]], [rs[u]])
                    k.op(k.dve, lambda e, u=u: e.reciprocal(out=rs[u][:], in_=rs[u][:]), [rs[u]], [rs[u]])
                    k.stt(xo[u][:], xin[u][:], rs[u][:, 0:1], gb[:], ALU.mult, ALU.mult, [xin[u], rs[u], gb], [xo[u]])
                    k.dma(k.sp, [(OUT[b, i * 128:(i + 1) * 128, :], xo[u][:])], xo[u], reads=[xo[u]],
                          writes=[self.dbuf["out"]], accumulate=True)

def host_consts():
    c = np.zeros((4, 128, 128), np.float32)
    c[0] = np.eye(128, dtype=np.float32)
    i = np.arange(128)
    same = (i[:, None] // 32) == (i[None, :] // 32)
    c[1] = (same & (i[:, None] <= i[None, :])).astype(np.float32)
    c[2] = (same & (i[:, None] >= i[None, :])).astype(np.float32)
    return c


def input_specs(DEPTH):
    return [
        ("cst", [4, 128, 128], F32), ("cst2", [128, 513], F32), ("cst3", [128, 4], F32),
        ("x", [NB, SEQ, D], F32), ("ctx", [NB, CTX, D], F32), ("cvec", [NB + 1, D], F32),
        ("w_ada", [DEPTH, D, 6 * D], F32), ("b_ada", [DEPTH, 6 * D], F32),
        ("norm_mix_g", [DEPTH, D], F32), ("norm_ffn_g", [DEPTH, D], F32), ("w_in", [DEPTH, D, PW], F32),
        ("s5sl", [DEPTH, 2, 128, 3, 32], F32), ("s5bl", [DEPTH, 2, 128, 3, 512], F32),
        ("s5bt", [DEPTH, 2, 128, 2, 8, 128], F32), ("s5ct", [DEPTH, 2, 128, 2, 32, 32], F32),
        ("gam_l", [128, 4, 16], F32), ("ghg_l", [DEPTH, 128, 8], F32), ("s5d_l", [DEPTH, 128, 8], F32),
        ("w_branch_s5", [DEPTH, 1024, D], F32), ("w_branch_hg", [DEPTH, 1024, D], F32), ("w_out", [DEPTH, D, D], F32),
        ("peer_w_q", [DEPTH, D, D], F32), ("peer_kt", [DEPTH, 128, 16, 128], F32),
        ("peer_u", [DEPTH, NEXP, D], F32), ("peer_v", [DEPTH, NEXP, D], F32), ("final_norm_g", [1, D], F32), ("bglu_l", [DEPTH, 128, 8], F32), ("s5_w_glu", [DEPTH, 1024, 1024], F32),
    ]


SCRATCH_SPECS = [
    ("X", [NT, D], F32), ("MODS", [NB + 1, 128, 6 * D], F32), ("UT", [1024, NT], F32), ("QFT", [4096, NT], F32),
    ("VOG", [NT, 1024], F32), ("GT", [4096, NT], F32), ("Y5T", [1024, NT], BF16), ("YHT", [1024, NT], BF16),
    ("U16", [NEXP, D], BF16), ("V16", [NEXP, D], BF16),
]


def build(dbg=None):
    P = Prog(dbg)
    dbg = P.dbg
    feed = dbg.get("feed", ())
    only = dbg.get("only", None)
    need_in = dbg.get("inputs", None)
    for (n, shp, dt) in input_specs(dbg.get("depth_in", DEPTH)):
        if need_in is None or n in need_in:
            P.din(n, shp, dt)
    if only is None or "final" in only:
        P.dout("out", [NB, SEQ, D], F32)
    for (n, shp, dt) in SCRATCH_SPECS:
        if n in feed:
            P.din(n, shp, dt)
        else:
            P.dscr(n, shp, dt)
    run = lambda ph: only is None or ph in only
    with P.es:
        st = P.es
        P.consts(st)
        if run("init"):
            P.phase_init_x()
        if run("hg"):
            P.phase_hg_lb(st)
        nl = dbg.get("layers", DEPTH)
        for l in range(nl):
            if run("mods"):
                P.phase_mods(l)
            if run("win"):
                P.phase_win(l)
            if run("s5"):
                P.phase_s5(l)
            if run("hg"):
                P.phase_hgrn(l)
            if run("merge"):
                P.phase_merge(l)
            if run("peer"):
                P.phase_peer(l)
        if run("final"):
            P.phase_final()
        for nme in dbg.get("copyout", ()):
            P.k.barrier()
            src = P.dram[nme]
            t = P.nc.dram_tensor("co_" + nme, list(src.shape), src.dtype, kind="ExternalOutput").ap()
            ds = P.k.dfree[0]
            nr = src.shape[0]
            step = (nr + 7) // 8
            P.k.dma(P.k.sp, [(t[r:min(r + step, nr)], src[r:min(r + step, nr)]) for r in range(0, nr, step)], ds,
                    reads=[P.dbuf[nme]], writes=[Buf("co")], accumulate=True)
        P.k.barrier()
    return P


def s5_layouts(inputs):
    a_re, a_im, ldt = inputs["s5_a_re"], inputs["s5_a_im"], inputs["s5_log_dt"]
    L = a_re.shape[0]
    def sl(a):
        return a.reshape(L, 2, 32, 2, 64).transpose(0, 1, 3, 4, 2).reshape(L, 2, 128, 32)
    ldt_sl = np.broadcast_to(ldt.reshape(L, 2, 32, 2).transpose(0, 1, 3, 2)[:, :, :, None, :], (L, 2, 2, 64, 32)).reshape(L, 2, 128, 32)
    s5sl = np.stack([sl(a_re), sl(a_im), ldt_sl], axis=3)
    def bl(a):
        t = a.reshape(L, 2, 8, 8, 64).transpose(0, 1, 3, 2, 4)
        return np.broadcast_to(t[:, :, :, None, :, :], (L, 2, 8, 16, 8, 64)).reshape(L, 2, 128, 512)
    ldt_g = np.broadcast_to(ldt[:, :, :, None], a_re.shape)
    s5bl = np.stack([bl(a_re), bl(a_im), bl(ldt_g)], axis=3)
    def btl(Bm):
        t = Bm.reshape(L, 2, 8, 8, 64, 16).transpose(0, 1, 3, 5, 2, 4)
        o = np.zeros((L, 2, 8, 16, 8, 2, 64), np.float32)
        for gl in range(8):
            o[:, :, gl, :, :, gl % 2, :] = t[:, :, gl]
        return o.reshape(L, 2, 128, 8, 128)
    s5bt = np.stack([btl(inputs["s5_b_re"]), btl(inputs["s5_b_im"])], axis=3)
    def ctl(Cm):
        t = Cm.reshape(L, 2, 32, 2, 16, 64)
        o = np.zeros((L, 2, 2, 64, 32, 2, 16), np.float32)
        for s_ in range(2):
            o[:, :, s_, :, :, s_, :] = t[:, :, :, s_].transpose(0, 1, 4, 2, 3)
        return o.reshape(L, 2, 128, 32, 32)
    s5ct = np.stack([ctl(inputs["s5_c_re"]), ctl(inputs["s5_c_im"])], axis=3)
    chl = lambda v: np.ascontiguousarray(v.reshape(L, 8, 128).transpose(0, 2, 1))
    return dict(s5sl=np.ascontiguousarray(s5sl), s5bl=np.ascontiguousarray(s5bl), s5bt=np.ascontiguousarray(s5bt),
                s5ct=np.ascontiguousarray(s5ct), s5d_l=chl(inputs["s5_d"]), bglu_l=chl(inputs["s5_b_glu"]))


def shared_inputs(inputs):
    d = {}
    d["cst"] = host_consts()
    c3 = np.zeros((128, 4), np.float32)
    c3[96:, 0] = 1.0
    d["cst3"] = c3
    d["cst2"] = np.ascontiguousarray(np.broadcast_to(np.arange(513, dtype=np.float32)[None, :], (128, 513)))
    for kname in ("w_ada", "b_ada", "norm_mix_g", "norm_ffn_g", "w_in", "s5_w_glu", "w_branch_s5", "w_branch_hg", "w_out",
                  "peer_w_q", "peer_u", "peer_v"):
        d[kname] = np.ascontiguousarray(inputs[kname])
    d["final_norm_g"] = np.ascontiguousarray(inputs["final_norm_g"][None, :])
    kt = np.stack([inputs["peer_k1"], inputs["peer_k2"]], axis=2)
    d["peer_kt"] = np.ascontiguousarray(kt.transpose(0, 4, 1, 2, 3).reshape(DEPTH, 128, 16, 128))
    d.update(s5_layouts(inputs))
    gam = inputs["hg_lb_gamma"]
    d["gam_l"] = np.ascontiguousarray(gam.reshape(DEPTH, 2, 8, 128).transpose(3, 0, 1, 2).reshape(128, DEPTH, 16))
    d["ghg_l"] = np.ascontiguousarray(inputs["hg_norm_g"].reshape(DEPTH, 8, 128).transpose(0, 2, 1))
    return d


def core_inputs(inputs, core, shared=None):
    b0 = core * NB
    d = dict(shared if shared is not None else shared_inputs(inputs))
    d["x"] = np.ascontiguousarray(inputs["x"][b0:b0 + NB])
    d["ctx"] = np.ascontiguousarray(inputs["ctx"][b0:b0 + NB])
    d["cvec"] = np.ascontiguousarray(np.concatenate([inputs["c"][b0:b0 + NB], inputs["c_ctx"][None, :]], axis=0))
    return d


_PROG_CACHE = {}


def kernel(**inputs):
    inputs = {k_: np.asarray(v) for k_, v in inputs.items()}
    if "prog" not in _PROG_CACHE:
        _PROG_CACHE["prog"] = build()
    P = _PROG_CACHE["prog"]
    shared = shared_inputs(inputs)
    names = [n for (n, _s, _t) in input_specs(DEPTH)]
    in_maps = []
    for c in range(N_CORES):
        d = core_inputs(inputs, c, shared)
        in_maps.append({n: d[n] for n in names})
    res = run_bass_kernel_spmd(P.nc, in_maps, core_ids=list(range(N_CORES)))
    out = np.concatenate([np.asarray(res.results[c]["out"]) for c in range(N_CORES)], axis=0)
    return out.astype(np.float32, copy=False)
```

```python
import contextlib
import os
import numpy as np
import ml_dtypes
import concourse.bass as bass
import concourse.mybir as mybir
from concourse.bass_utils import run_bass_kernel_spmd

F32 = mybir.dt.float32
BF16 = mybir.dt.bfloat16
U32 = mybir.dt.uint32
I32 = mybir.dt.int32
ALU = mybir.AluOpType
AF = mybir.ActivationFunctionType
AX = mybir.AxisListType

D = 2048
NB = int(os.environ.get("KERNEL_NB", "4"))
N_CORES = 16 // NB
CTX = 256
SEQ = 2048
TOK = CTX + SEQ
NT = NB * TOK
DEPTH = 4
EPS = 1e-6
PW = 10240
NKEY = 128
NEXP = 16384
TWO_PI = 6.283185307179586
PI = 3.141592653589793


class Buf:
    __slots__ = ("name", "w", "r")

    def __init__(self, name=""):
        self.name = name
        self.w = {}
        self.r = {}


class EngW:
    def __init__(self, nc, eng, name, es):
        self.eng = eng
        self.name = name
        self.sem = es.enter_context(nc.semaphore("s_" + name))
        self.key = "E" + name
        self.count = 0
        self.waited = {}


class DSem:
    def __init__(self, nc, name, es):
        self.sem = es.enter_context(nc.semaphore(name))
        self.key = "D" + name
        self.count = 0


class Tile:
    def __init__(self, h, buf, dsem=None):
        self.h = h
        self.buf = buf
        self.dsem = dsem

    def __getitem__(self, idx):
        return self.h[idx]


class K:
    def __init__(self, nc, es):
        self.nc = nc
        self.es = es
        self.pe = EngW(nc, nc.tensor, "pe", es)
        self.dve = EngW(nc, nc.vector, "dve", es)
        self.act = EngW(nc, nc.scalar, "act", es)
        self.pool = EngW(nc, nc.gpsimd, "pool", es)
        self.sp = EngW(nc, nc.sync, "sp", es)
        self.engs = [self.pe, self.dve, self.act, self.pool, self.sp]
        self.dsems = [DSem(nc, "d%d" % i, es) for i in range(48)]
        self.dfree = list(self.dsems)
        self.uid = 0
        self.ninst = 0

    def sb(self, st, shape, dtype, name=None, dma=False):
        self.uid += 1
        name = (name or "t") + "_%d" % self.uid
        h = st.enter_context(self.nc.sbuf_tensor(name, list(shape), dtype))
        ds = None
        if dma:
            ds = self.dfree.pop(0)
            st.callback(lambda d=ds: self.dfree.append(d))
        return Tile(h, Buf(name), ds)

    def ps(self, st, shape, dtype=F32, name=None):
        self.uid += 1
        name = (name or "p") + "_%d" % self.uid
        h = st.enter_context(self.nc.psum_tensor(name, list(shape), dtype))
        return Tile(h, Buf(name))

    def _need(self, reads, writes):
        need = {}
        for t in reads:
            b = t.buf if isinstance(t, Tile) else t
            for k, (s, v) in b.w.items():
                if k not in need or need[k][1] < v:
                    need[k] = (s, v)
        for t in writes:
            b = t.buf if isinstance(t, Tile) else t
            for dd in (b.w, b.r):
                for k, (s, v) in dd.items():
                    if k not in need or need[k][1] < v:
                        need[k] = (s, v)
        return need

    def _wait(self, E, need):
        for k, (s, v) in need.items():
            if k == E.key and E.name in ("pe", "sp"):
                continue
            if E.waited.get(k, 0) >= v:
                continue
            E.eng.wait_ge(s, v)
            E.waited[k] = v

    def _mark(self, reads, writes, key, sem, val, accumulate=False):
        for t in reads:
            b = t.buf if isinstance(t, Tile) else t
            b.r[key] = (sem, val)
        for t in writes:
            b = t.buf if isinstance(t, Tile) else t
            if accumulate:
                b.w[key] = (sem, val)
            else:
                b.w = {key: (sem, val)}
                b.r = {}

    def op(self, E, fn, reads=(), writes=()):
        self._wait(E, self._need(reads, writes))
        ins = fn(E.eng)
        E.count += 1
        ins.then_inc(E.sem, 1)
        self._mark(reads, writes, E.key, E.sem, E.count)
        self.ninst += 1
        return ins

    def dma(self, Q, pairs, sem_tile, reads=(), writes=(), accumulate=False, **kw):
        ds = sem_tile.dsem if isinstance(sem_tile, Tile) else sem_tile
        self._wait(Q, self._need(reads, writes))
        for (o, i) in pairs:
            Q.eng.dma_start(out=o, in_=i, **kw).then_inc(ds.sem, 16)
            ds.count += 16
            self.ninst += 1
        self._mark(reads, writes, ds.key, ds.sem, ds.count, accumulate=accumulate)

    @contextlib.contextmanager
    def scope(self):
        with contextlib.ExitStack() as st:
            yield st
            self.barrier()

    def tt(self, E, o, a, b, op, R, W):
        return self.op(E, lambda e: e.tensor_tensor(out=o, in0=a, in1=b, op=op), R, W)

    def ts(self, E, o, a, s1, s2, op0, op1, R, W):
        if s2 is None:
            return self.op(E, lambda e: e.tensor_scalar(out=o, in0=a, scalar1=s1, scalar2=None, op0=op0), R, W)
        return self.op(E, lambda e: e.tensor_scalar(out=o, in0=a, scalar1=s1, scalar2=s2, op0=op0, op1=op1), R, W)

    def stt(self, o, a, s, b, op0, op1, R, W):
        return self.op(self.dve, lambda e: e.scalar_tensor_tensor(out=o, in0=a, scalar=s, in1=b, op0=op0, op1=op1), R, W)

    def actf(self, o, a, func, R, W, bias=None, scale=None):
        kw = {}
        if bias is not None:
            kw["bias"] = bias
        if scale is not None:
            kw["scale"] = scale
        return self.op(self.act, lambda e: e.activation(out=o, in_=a, func=func, **kw), R, W)

    def cp(self, E, o, a, R, W):
        if E is self.act:
            return self.op(E, lambda e: e.copy(out=o, in_=a), R, W)
        return self.op(E, lambda e: e.tensor_copy(out=o, in_=a), R, W)

    def mm(self, o, lhsT, rhs, start, stop, R, W):
        return self.op(self.pe, lambda e: e.matmul(o, lhsT=lhsT, rhs=rhs, start=start, stop=stop), R, W)

    def barrier(self):
        for E in self.engs:
            for F in self.engs:
                if F is E or F.count == 0:
                    continue
                if E.waited.get(F.key, 0) >= F.count:
                    continue
                E.eng.wait_ge(F.sem, F.count)
                E.waited[F.key] = F.count
            for d in self.dsems:
                if d.count == 0 or E.waited.get(d.key, 0) >= d.count:
                    continue
                E.eng.wait_ge(d.sem, d.count)
                E.waited[d.key] = d.count


class Prog:
    def __init__(self, dbg=None):
        self.dbg = dbg or {}
        self.nc = bass.Bass("TRN2", target_bir_lowering=False)
        self.es = contextlib.ExitStack()
        self.k = K(self.nc, self.es)
        self.dram = {}
        self.dbuf = {}

    def din(self, name, shape, dtype=F32):
        t = self.nc.dram_tensor(name, list(shape), dtype, kind="ExternalInput").ap()
        self.dram[name] = t
        self.dbuf[name] = Buf(name)
        return t

    def dout(self, name, shape, dtype=F32):
        t = self.nc.dram_tensor(name, list(shape), dtype, kind="ExternalOutput").ap()
        self.dram[name] = t
        self.dbuf[name] = Buf(name)
        return t

    def dscr(self, name, shape, dtype=F32):
        kind = "ExternalOutput" if name in self.dbg.get("dump", ()) else "Internal"
        t = self.nc.dram_tensor(name, list(shape), dtype, kind=kind).ap()
        self.dram[name] = t
        self.dbuf[name] = Buf(name)
        return t

    def dump(self, name, tile, ap, shape, dtype=F32):
        if name not in self.dbg.get("dumps", ()) or ("dbg_" + name) in self.dbg.get("dumped", []):
            return
        k = self.k
        t = self.nc.dram_tensor("dbg_" + name, list(shape), dtype, kind="ExternalOutput").ap()
        if not hasattr(self, "dbg_ds"):
            self.dbg_ds = k.dfree.pop()
            self.dbg_buf = Buf("dbg")
        k.dma(k.sp, [(t, ap)], self.dbg_ds, reads=[tile], writes=[self.dbg_buf], accumulate=True)
        self.dbg.setdefault("dumped", []).append("dbg_" + name)

    def consts(self, st):
        k = self.k
        c = self.dram["cst"]
        self.ident_f = k.sb(st, [128, 128], F32, "identf", dma=True)
        self.ident_b = k.sb(st, [128, 128], BF16, "identb")
        self.ones_f = k.sb(st, [1, 512], F32, "onesf")
        self.ones_b = k.sb(st, [1, 512], BF16, "onesb")
        k.dma(k.sp, [(self.ident_f[:], c[0])], self.ident_f, reads=[self.dbuf["cst"]], writes=[self.ident_f])
        k.op(k.dve, lambda e: e.tensor_copy(out=self.ident_b[:], in_=self.ident_f[:]), [self.ident_f], [self.ident_b])
        k.op(k.dve, lambda e: e.memset(self.ones_f[:], 1.0), [], [self.ones_f])
        k.op(k.dve, lambda e: e.memset(self.ones_b[:], 1.0), [], [self.ones_b])

    def phase_init_x(self):
        k = self.k
        X = self.dram["X"]
        with self.k.scope() as st:
            ds = k.dfree[0]
            pairs = []
            for b in range(NB):
                pairs.append((X[b * TOK:b * TOK + CTX, :], self.dram["ctx"][b]))
                for q in range(4):
                    pairs.append((X[b * TOK + CTX + q * 512:b * TOK + CTX + (q + 1) * 512, :],
                                  self.dram["x"][b, q * 512:(q + 1) * 512, :]))
            k.dma(k.sp, pairs, ds, reads=[self.dbuf["x"], self.dbuf["ctx"]], writes=[self.dbuf["X"]], accumulate=True)
        k.barrier()

    def phase_silu_c(self, st):
        k = self.k
        self.LB = [k.sb(st, [128, 16, 128], BF16, "LB") for _ in range(NB + 1)]
        with self.k.scope() as s2:
            rows = []
            for b in range(NB + 1):
                r = k.sb(s2, [1, D], F32, "crow", dma=True)
                k.dma(k.sp, [(r[:], self.dram["cvec"][b:b + 1, :])], r, reads=[self.dbuf["cvec"]], writes=[r])
                rb = k.sb(s2, [1, D], BF16, "crowb")
                k.op(k.act, lambda e, r=r, rb=rb: e.activation(out=rb[:], in_=r[:], func=AF.Silu), [r], [rb])
                rows.append(rb)
            pss = [k.ps(s2, [128, 512]) for _ in range(2)]
            n = 0
            for b in range(NB + 1):
                lb = self.LB[b]
                for q in range(4):
                    p = pss[n % 2]
                    n += 1
                    for j in range(4):
                        kc = q * 4 + j
                        k.op(k.pe, lambda e, p=p, j=j, kc=kc, b=b: e.matmul(
                            p[:, j * 128:(j + 1) * 128], lhsT=rows[b][0:1, kc * 128:(kc + 1) * 128],
                            rhs=self.ones_b[0:1, 0:128], start=True, stop=True), [rows[b], self.ones_b], [p])
                    k.op(k.dve, lambda e, p=p, lb=lb, q=q: e.tensor_copy(
                        out=lb[:, q * 4:(q + 1) * 4, :], in_=p[:].rearrange("p (a b) -> p a b", a=4)), [p], [lb])
            k.barrier()

    def phase_mods(self, l):
        k = self.k
        wada = self.dram["w_ada"][l].rearrange("(kc p) n -> p kc n", p=128)
        MODS = self.dram["MODS"]
        with self.k.scope() as st:
            self.phase_silu_c(st)
            brow = k.sb(st, [1, 6 * D], F32, "brow", dma=True)
            k.dma(k.sp, [(brow[:], self.dram["b_ada"][l:l + 1, :])], brow, reads=[self.dbuf["b_ada"]], writes=[brow])
            grow = k.sb(st, [1, 2 * D], F32, "grow", dma=True)
            k.dma(k.sp, [(grow[:, 0:D], self.dram["norm_mix_g"][l:l + 1, :]),
                         (grow[:, D:2 * D], self.dram["norm_ffn_g"][l:l + 1, :])], grow,
                  reads=[self.dbuf["norm_mix_g"]], writes=[grow])
            G = k.sb(st, [128, 2 * D], F32, "G")
            pss = [k.ps(st, [128, 512]) for _ in range(4)]
            for q in range(8):
                p = pss[q % 4]
                k.op(k.pe, lambda e, p=p, q=q: e.matmul(p[:], lhsT=self.ones_f[0:1, 0:128],
                                                       rhs=grow[0:1, q * 512:(q + 1) * 512], start=True, stop=True),
                     [grow, self.ones_f], [p])
                k.op(k.act, lambda e, p=p, q=q: e.copy(out=G[:, q * 512:(q + 1) * 512], in_=p[:]), [p], [G])
            was = [k.sb(st, [128, 16, 512], BF16, "WA", dma=True) for _ in range(2)]
            stg = [k.sb(st, [128, 512], F32, "stg", dma=True) for _ in range(4)]
            n = 0
            for nci in range(24):
                wa = was[nci % 2]
                k.dma(k.pool, [(wa[:, 0:8, :], wada[:, 0:8, nci * 512:(nci + 1) * 512]),
                               (wa[:, 8:16, :], wada[:, 8:16, nci * 512:(nci + 1) * 512])], wa,
                      reads=[self.dbuf["w_ada"]], writes=[wa])
                seg = nci // 4
                col = (nci % 4) * 512
                for b in range(NB + 1):
                    p = pss[n % 4]
                    sg = stg[n % 4]
                    n += 1
                    for kc in range(16):
                        k.op(k.pe, lambda e, p=p, kc=kc, b=b, wa=wa: e.matmul(
                            p[:], lhsT=self.LB[b][:, kc, :], rhs=wa[:, kc, :], start=(kc == 0), stop=False),
                            [self.LB[b], wa], [p])
                    k.op(k.pe, lambda e, p=p, nci=nci: e.matmul(
                        p[:], lhsT=self.ones_f[0:1, 0:128], rhs=brow[0:1, nci * 512:(nci + 1) * 512],
                        start=False, stop=True), [brow, self.ones_f], [p])
                    if seg in (1, 4):
                        g0 = (0 if seg == 1 else D) + col
                        k.op(k.dve, lambda e, p=p, sg=sg, g0=g0: e.scalar_tensor_tensor(
                            out=sg[:], in0=p[:], scalar=1.0, in1=G[:, g0:g0 + 512], op0=ALU.add, op1=ALU.mult),
                            [p, G], [sg])
                    else:
                        k.op(k.act, lambda e, p=p, sg=sg: e.copy(out=sg[:], in_=p[:]), [p], [sg])
                    k.dma(k.sp, [(MODS[b, :, nci * 512:(nci + 1) * 512], sg[:])], sg,
                          reads=[sg], writes=[self.dbuf["MODS"]], accumulate=True)
        k.barrier()

    def norm_mod_tile(self, st_tiles, xin, A, SH, xn_out):
        k = self.k
        junk, ss, rs, tmp = st_tiles
        k.op(k.act, lambda e: e.activation(out=junk[:], in_=xin[:], func=AF.Square, accum_out=ss[:]), [xin], [junk, ss])
        k.op(k.dve, lambda e: e.tensor_scalar(out=rs[:], in0=ss[:], scalar1=1.0 / D, scalar2=EPS, op0=ALU.mult, op1=ALU.add),
             [ss], [rs])
        k.op(k.act, lambda e: e.activation(out=rs[:], in_=rs[:], func=AF.Sqrt), [rs], [rs])
        k.op(k.dve, lambda e: e.reciprocal(out=rs[:], in_=rs[:]), [rs], [rs])
        k.op(k.dve, lambda e: e.scalar_tensor_tensor(out=tmp[:], in0=xin[:], scalar=rs[:, 0:1], in1=A[:],
                                                     op0=ALU.mult, op1=ALU.mult), [xin, rs, A], [tmp])
        k.op(k.pool, lambda e: e.tensor_tensor(out=xn_out[:], in0=tmp[:], in1=SH[:], op=ALU.add), [tmp, SH], [xn_out])

    def phase_win(self, l):
        k = self.k
        X = self.dram["X"]
        MODS = self.dram["MODS"]
        win = self.dram["w_in"][l].rearrange("(kc p) n -> p kc n", p=128)
        UT, QFT, VOG, GT = self.dram["UT"], self.dram["QFT"], self.dram["VOG"], self.dram["GT"]
        for b in range(NB):
            with self.k.scope() as st:
                XT = k.sb(st, [128, 16, TOK], BF16, "XT")
                pss = [k.ps(st, [128, 512]) for _ in range(4)]
                with self.k.scope() as s1:
                    AM = [k.sb(s1, [128, D], F32, "AM", dma=True) for _ in range(2)]
                    SM = [k.sb(s1, [128, D], F32, "SM", dma=True) for _ in range(2)]
                    for j, bb in enumerate((NB, b)):
                        k.dma(k.sp, [(AM[j][:], MODS[bb, :, D:2 * D])], AM[j], reads=[self.dbuf["MODS"]], writes=[AM[j]])
                        k.dma(k.sp, [(SM[j][:], MODS[bb, :, 0:D])], SM[j], reads=[self.dbuf["MODS"]], writes=[SM[j]])
                    xins = [k.sb(s1, [128, D], F32, "xin", dma=True) for _ in range(2)]
                    junk = k.sb(s1, [128, D], BF16, "junk")
                    tmp = k.sb(s1, [128, D], F32, "tmp")
                    xns = [k.sb(s1, [128, D], BF16, "xn") for _ in range(2)]
                    sss = [k.sb(s1, [128, 1], F32, "ss") for _ in range(2)]
                    rss = [k.sb(s1, [128, 1], F32, "rs") for _ in range(2)]
                    for i in range(TOK // 128):
                        xin = xins[i % 2]
                        r0 = b * TOK + i * 128
                        k.dma(k.sp, [(xin[:], X[r0:r0 + 128, :])], xin, reads=[self.dbuf["X"]], writes=[xin])
                        j = 0 if i < CTX // 128 else 1
                        xn = xns[i % 2]
                        self.norm_mod_tile((junk, sss[i % 2], rss[i % 2], tmp), xin, AM[j], SM[j], xn)
                        for q in range(4):
                            p = pss[q]
                            for jj in range(4):
                                kc = q * 4 + jj
                                k.op(k.pe, lambda e, p=p, jj=jj, kc=kc, xn=xn: e.matmul(
                                    p[:, jj * 128:(jj + 1) * 128], lhsT=xn[:, kc * 128:(kc + 1) * 128],
                                    rhs=self.ident_b[:], start=True, stop=True), [xn, self.ident_b], [p])
                            E = k.act if q % 2 == 0 else k.dve
                            if E is k.act:
                                k.op(E, lambda e, p=p, q=q, i=i: e.copy(
                                    out=XT[:, q * 4:(q + 1) * 4, i * 128:(i + 1) * 128],
                                    in_=p[:].rearrange("p (a b) -> p a b", a=4)), [p], [XT])
                            else:
                                k.op(E, lambda e, p=p, q=q, i=i: e.tensor_copy(
                                    out=XT[:, q * 4:(q + 1) * 4, i * 128:(i + 1) * 128],
                                    in_=p[:].rearrange("p (a b) -> p a b", a=4)), [p], [XT])
                ws = [k.sb(st, [128, 16, 512], BF16, "W", dma=True) for _ in range(2)]
                stg = [k.sb(st, [128, 512], F32, "stg", dma=True) for _ in range(4)]
                n = 0
                blocks = [(0, 256)] + [(256 + q * 512, 512) for q in range(4)]
                for ci in range(20):
                    w = ws[ci % 2]
                    c0 = ci * 512
                    k.dma(k.pool, [(w[:, 0:8, :], win[:, 0:8, c0:c0 + 512]), (w[:, 8:16, :], win[:, 8:16, c0:c0 + 512])],
                          w, reads=[self.dbuf["w_in"]], writes=[w])
                    if ci in (4, 5):
                        dcol = c0 - 2048
                        for i in range(TOK // 128):
                            p = pss[n % 4]
                            sg = stg[n % 4]
                            n += 1
                            for kc in range(16):
                                k.op(k.pe, lambda e, p=p, kc=kc, i=i, w=w: e.matmul(
                                    p[:], lhsT=XT[:, kc, i * 128:(i + 1) * 128], rhs=w[:, kc, :],
                                    start=(kc == 0), stop=(kc == 15)), [XT, w], [p])
                            if n % 2 == 0:
                                k.op(k.act, lambda e, p=p, sg=sg: e.copy(out=sg[:], in_=p[:]), [p], [sg])
                            else:
                                k.op(k.dve, lambda e, p=p, sg=sg: e.tensor_copy(out=sg[:], in_=p[:]), [p], [sg])
                            r0 = b * TOK + i * 128
                            k.dma(k.sp, [(VOG[r0:r0 + 128, dcol:dcol + 512], sg[:])], sg, reads=[sg],
                                  writes=[self.dbuf["VOG"]], accumulate=True)
                    else:
                        if ci < 2:
                            dst, drow, sig = UT, c0, False
                        elif ci < 4:
                            dst, drow, sig = QFT, c0 - 1024, False
                        elif ci < 8:
                            dst, drow, sig = QFT, 1024 + c0 - 3072, False
                        elif ci < 10:
                            dst, drow, sig = QFT, 2048 + c0 - 4096, False
                        elif ci < 12:
                            dst, drow, sig = QFT, 3072 + c0 - 5120, False
                        else:
                            dst, drow, sig = GT, c0 - 6144, True
                        dname = {id(UT): "UT", id(QFT): "QFT", id(GT): "GT"}[id(dst)]
                        for sub in range(4):
                            for (t0, tn) in blocks:
                                p = pss[n % 4]
                                sg = stg[n % 4]
                                n += 1
                                for kc in range(16):
                                    k.op(k.pe, lambda e, p=p, kc=kc, w=w, sub=sub, t0=t0, tn=tn: e.matmul(
                                        p[:, 0:tn], lhsT=w[:, kc, sub * 128:(sub + 1) * 128], rhs=XT[:, kc, t0:t0 + tn],
                                        start=(kc == 0), stop=(kc == 15)), [XT, w], [p])
                                if sig:
                                    k.op(k.act, lambda e, p=p, sg=sg, tn=tn: e.activation(
                                        out=sg[:, 0:tn], in_=p[:, 0:tn], func=AF.Sigmoid), [p], [sg])
                                elif n % 2 == 0:
                                    k.op(k.act, lambda e, p=p, sg=sg, tn=tn: e.copy(out=sg[:, 0:tn], in_=p[:, 0:tn]), [p], [sg])
                                else:
                                    k.op(k.dve, lambda e, p=p, sg=sg, tn=tn: e.tensor_copy(out=sg[:, 0:tn], in_=p[:, 0:tn]),
                                         [p], [sg])
                                rr = drow + sub * 128
                                k.dma(k.sp, [(dst[rr:rr + 128, b * TOK + t0:b * TOK + t0 + tn], sg[:, 0:tn])], sg,
                                      reads=[sg], writes=[self.dbuf[dname]], accumulate=True)
            k.barrier()


    def ang_reduce(self, xT, xap, kiT, kiap, kfT, kfap):
        k = self.k
        k.ts(k.dve, kiap, xap, 1.0 / TWO_PI, None, ALU.mult, None, [xT], [kiT])
        k.cp(k.dve, kfap, kiap, [kiT], [kfT])
        k.stt(xap, kfap, -TWO_PI, xap, ALU.mult, ALU.add, [kfT, xT], [xT])
        k.ts(k.dve, xap, xap, -3.14159, 3.14159, ALU.max, ALU.min, [xT], [xT])

    def s5_params(self, l, st):
        k = self.k
        prm = []
        for d in range(2):
            prm.append(dict(BT=k.sb(st, [128, 2, 8, 128], BF16, "BTb"), BT3=k.sb(st, [128, 2, 8, 128], BF16, "BT3"),
                            CT=k.sb(st, [128, 2, 32, 64], BF16, "CTb"),
                            R=k.sb(st, [128, 32], F32, "RSL"), TH=k.sb(st, [128, 32], F32, "THR")))
        msk = k.sb(st, [128, 4], F32, "msk", dma=True)
        k.dma(k.sp, [(msk[:], self.dram["cst3"][:, :])], msk, reads=[self.dbuf["cst3"]], writes=[msk])
        with self.k.scope() as s2:
            for d in range(2):
                P = prm[d]
                sl = k.sb(s2, [128, 3, 32], F32, "sl", dma=True)
                bl = k.sb(s2, [128, 3, 512], F32, "bl", dma=True)
                bt = k.sb(s2, [128, 2, 8, 128], F32, "bt", dma=True)
                ct = k.sb(s2, [128, 2, 32, 32], F32, "ct", dma=True)
                k.dma(k.sp, [(sl[:], self.dram["s5sl"][l, d])], sl, reads=[self.dbuf["s5sl"]], writes=[sl])
                k.dma(k.sp, [(bl[:], self.dram["s5bl"][l, d])], bl, reads=[self.dbuf["s5bl"]], writes=[bl])
                k.dma(k.sp, [(bt[:], self.dram["s5bt"][l, d])], bt, reads=[self.dbuf["s5bt"]], writes=[bt])
                k.dma(k.sp, [(ct[:], self.dram["s5ct"][l, d])], ct, reads=[self.dbuf["s5ct"]], writes=[ct])
                k.op(k.pool, lambda e, P=P: e.memset(P["CT"][:], 0.0), [], [P["CT"]])
                k.cp(k.pool, P["CT"][:, :, :, 32:64], ct[:], [ct], [P["CT"]])
                w1 = k.sb(s2, [128, 32], F32, "w1")
                wi = k.sb(s2, [128, 32], I32, "wi")
                wf = k.sb(s2, [128, 32], F32, "wf")
                k.actf(w1[:], sl[:, 2, :], AF.Exp, [sl], [w1])
                k.tt(k.dve, P["TH"][:], sl[:, 1, :], w1[:], ALU.mult, [sl, w1], [P["TH"]])
                k.tt(k.dve, w1[:], sl[:, 0, :], w1[:], ALU.mult, [sl, w1], [w1])
                k.actf(P["R"][:], w1[:], AF.Exp, [w1], [P["R"]])
                self.ang_reduce(P["TH"], P["TH"][:], wi, wi[:], wf, wf[:])
                N = 512
                dt = k.sb(s2, [128, N], F32, "dt")
                r = k.sb(s2, [128, N], F32, "r")
                th = k.sb(s2, [128, N], F32, "th")
                th2 = k.sb(s2, [128, N], F32, "th2")
                ki = k.sb(s2, [128, N], I32, "ki")
                kf = k.sb(s2, [128, N], F32, "kf")
                sn = k.sb(s2, [128, N], F32, "sn")
                cs = k.sb(s2, [128, N], F32, "cs")
                t1 = k.sb(s2, [128, N], F32, "t1")
                t2 = k.sb(s2, [128, N], F32, "t2")
                cR = k.sb(s2, [128, N], F32, "cR")
                cI = k.sb(s2, [128, N], F32, "cI")
                are, aim = bl[:, 0, :], bl[:, 1, :]
                k.actf(dt[:], bl[:, 2, :], AF.Exp, [bl], [dt])
                k.tt(k.dve, th[:], aim, dt[:], ALU.mult, [bl, dt], [th])
                k.tt(k.dve, dt[:], are, dt[:], ALU.mult, [bl, dt], [dt])
                k.actf(r[:], dt[:], AF.Exp, [dt], [r])
                self.ang_reduce(th, th[:], ki, ki[:], kf, kf[:])
                k.ts(k.dve, th2[:], th[:], PI / 2, None, ALU.add, None, [th], [th2])
                self.ang_reduce(th2, th2[:], ki, ki[:], kf, kf[:])
                k.actf(sn[:], th[:], AF.Sin, [th], [sn])
                k.actf(cs[:], th2[:], AF.Sin, [th2], [cs])
                k.tt(k.dve, cs[:], r[:], cs[:], ALU.mult, [r, cs], [cs])
                k.ts(k.dve, cs[:], cs[:], -1.0, None, ALU.add, None, [cs], [cs])
                k.tt(k.dve, sn[:], r[:], sn[:], ALU.mult, [r, sn], [sn])
                k.tt(k.dve, t1[:], are, are, ALU.mult, [bl], [t1])
                k.tt(k.dve, t2[:], aim, aim, ALU.mult, [bl], [t2])
                k.tt(k.dve, t1[:], t1[:], t2[:], ALU.add, [t1, t2], [t1])
                k.op(k.dve, lambda e: e.reciprocal(out=t1[:], in_=t1[:]), [t1], [t1])
                k.tt(k.dve, cR[:], cs[:], are, ALU.mult, [cs, bl], [cR])
                k.tt(k.dve, t2[:], sn[:], aim, ALU.mult, [sn, bl], [t2])
                k.tt(k.dve, cR[:], cR[:], t2[:], ALU.add, [cR, t2], [cR])
                k.tt(k.dve, cR[:], cR[:], t1[:], ALU.mult, [cR, t1], [cR])
                k.tt(k.dve, cI[:], sn[:], are, ALU.mult, [sn, bl], [cI])
                k.tt(k.dve, t2[:], cs[:], aim, ALU.mult, [cs, bl], [t2])
                k.tt(k.dve, cI[:], cI[:], t2[:], ALU.subtract, [cI, t2], [cI])
                k.tt(k.dve, cI[:], cI[:], t1[:], ALU.mult, [cI, t1], [cI])
                bc = lambda t: t[:].rearrange("p (k q) -> p k q", k=8).unsqueeze(2).broadcast_to([128, 8, 2, 64])
                v4 = lambda ap: ap.rearrange("p k (s q) -> p k s q", s=2)
                u1 = k.sb(s2, [128, 8, 128], F32, "u1")
                u2 = k.sb(s2, [128, 8, 128], F32, "u2")
                k.tt(k.dve, v4(u1[:]), v4(bt[:, 0]), bc(cR), ALU.mult, [bt, cR], [u1])
                k.tt(k.dve, v4(u2[:]), v4(bt[:, 1]), bc(cI), ALU.mult, [bt, cI], [u2])
                k.tt(k.dve, P["BT"][:, 0], u1[:], u2[:], ALU.subtract, [u1, u2], [P["BT"]])
                k.tt(k.dve, v4(u1[:]), v4(bt[:, 1]), bc(cR), ALU.mult, [bt, cR], [u1])
                k.tt(k.dve, v4(u2[:]), v4(bt[:, 0]), bc(cI), ALU.mult, [bt, cI], [u2])
                k.tt(k.dve, P["BT"][:, 1], u1[:], u2[:], ALU.add, [u1, u2], [P["BT"]])
                k.ts(k.dve, P["BT3"][:], P["BT"][:], msk[:, 0:1], None, ALU.mult, None, [P["BT"], msk], [P["BT3"]])
                if d == 0:
                    self.dump("R", P["R"], P["R"][:], [128, 32])
                    self.dump("TH", P["TH"], P["TH"][:], [128, 32])
                    self.dump("cR", cR, cR[:], [128, 512])
                    self.dump("cI", cI, cI[:], [128, 512])
                    self.dump("BT", P["BT"], P["BT"][:], [128, 2, 8, 128], BF16)
                    self.dump("CT", P["CT"], P["CT"][:], [128, 2, 32, 64], BF16)
        return prm

    def phase_s5(self, l):
        k = self.k
        UT, Y5T = self.dram["UT"], self.dram["Y5T"]
        with self.k.scope() as st:
            prm = self.s5_params(l, st)
            iot = k.sb(st, [128, 513], F32, "iota", dma=True)
            k.dma(k.sp, [(iot[:], self.dram["cst2"][:, :])], iot, reads=[self.dbuf["cst2"]], writes=[iot])
            dsk = k.sb(st, [128, 8], F32, "dsk", dma=True)
            bgl = k.sb(st, [128, 8], F32, "bgl", dma=True)
            k.dma(k.sp, [(dsk[:], self.dram["s5d_l"][l])], dsk, reads=[self.dbuf["s5d_l"]], writes=[dsk])
            k.dma(k.sp, [(bgl[:], self.dram["bglu_l"][l])], bgl, reads=[self.dbuf["bglu_l"]], writes=[bgl])
            WG = None
            for b in range(NB):
                with self.k.scope() as sb_:
                    Y = k.sb(sb_, [128, 8, TOK], F32, "Y")
                    uTb = k.sb(sb_, [128, 8, TOK], BF16, "uTb", dma=True)
                    k.dma(k.pool, [(uTb[:, kk, :], UT[kk * 128:(kk + 1) * 128, b * TOK:(b + 1) * TOK]) for kk in range(8)],
                          uTb, reads=[self.dbuf["UT"]], writes=[uTb], max_dma_last_dim=4096)
                    with self.k.scope() as sc:
                        self.s5_scan(sc, prm, iot, Y, uTb)
                    if b == 1:
                        self.dump("Y", Y, Y[:], [128, 8, TOK])
                    self.s5_glu(sb_, l, b, Y, uTb, dsk, bgl, WG)
        k.barrier()

    def s5_scan(self, sc, prm, iot, Y, uTb):
        k = self.k
        SIN = [k.sb(sc, [128, 513], F32, "SIN") for _ in range(2)]
        COS = [k.sb(sc, [128, 513], F32, "COS") for _ in range(2)]
        ang = k.sb(sc, [128, 513], F32, "ang")
        ki = k.sb(sc, [128, 513], I32, "aki")
        kf = k.sb(sc, [128, 513], F32, "akf")
        XR = [k.ps(sc, [128, 512]) for _ in range(2)]
        XI = [k.ps(sc, [128, 512]) for _ in range(2)]
        YP = [k.ps(sc, [128, 512]) for _ in range(2)]
        T = [[k.sb(sc, [128, 512], F32, "m") for _ in range(4)] for _ in range(2)]
        xr = [k.sb(sc, [128, 512], F32, "xr") for _ in range(2)]
        xi = [k.sb(sc, [128, 512], F32, "xi") for _ in range(2)]
        gR = [k.sb(sc, [128, 512], F32, "gR") for _ in range(2)]
        gI = [k.sb(sc, [128, 512], F32, "gI") for _ in range(2)]
        hR = [k.sb(sc, [128, 512], BF16, "hR") for _ in range(2)]
        hI = [k.sb(sc, [128, 512], BF16, "hI") for _ in range(2)]
        ini = [k.sb(sc, [128, 2], F32, "ini") for _ in range(2)]
        us = [k.sb(sc, [128, 2], F32, "us") for _ in range(2)]
        blocks_f = [(0, 256)] + [(256 + q * 512, 512) for q in range(4)]
        blocks_b = [(0, 256)] + [(256 + q * 512, 512) for q in (3, 2, 1, 0)]
        jlist = [4 * a + b_ for a in range(8) for b_ in (3, 2, 1, 0)]
        for d in range(2):
            P = prm[d]
            for pi in range(0, 32, 2):
                st_ = []
                for s_, j in enumerate((jlist[pi], jlist[pi + 1])):
                    kk, jl = j // 4, j % 4
                    rows = slice(32 * jl, 32 * jl + 32) if jl < 3 else slice(64, 128)
                    BTt = P["BT"] if jl < 3 else P["BT3"]
                    ccols = slice(32, 64) if jl < 3 else slice(0, 64)
                    S, C = SIN[s_], COS[s_]
                    k.ts(k.dve, ang[:], iot[:], P["TH"][:, j:j + 1], None, ALU.mult, None, [iot, P["TH"]], [ang])
                    self.ang_reduce(ang, ang[:], ki, ki[:], kf, kf[:])
                    k.actf(S[:], ang[:], AF.Sin, [ang], [S])
                    k.ts(k.dve, ang[:], ang[:], PI / 2, None, ALU.add, None, [ang], [ang])
                    self.ang_reduce(ang, ang[:], ki, ki[:], kf, kf[:])
                    k.actf(C[:], ang[:], AF.Sin, [ang], [C])
                    st_.append((j, kk, rows, BTt, ccols, S, C))
                for bi, (t0, n) in enumerate(blocks_f if d == 0 else blocks_b):
                    first = (bi == 0)
                    cols = slice(t0, t0 + n) if d == 0 else slice(t0 + n - 1, (t0 - 1) if t0 > 0 else None, -1)
                    for u, (j, kk, rows, BTt, ccols, S, C) in enumerate(st_):
                        k.mm(XR[u][:, 0:n], BTt[rows, 0, kk, :], uTb[rows, kk, cols], True, True, [BTt, uTb], [XR[u]])
                        k.mm(XI[u][:, 0:n], BTt[rows, 1, kk, :], uTb[rows, kk, cols], True, True, [BTt, uTb], [XI[u]])
                    for u, (j, kk, rows, BTt, ccols, S, C) in enumerate(st_):
                        t1, t2, t3, t4 = T[u]
                        k.tt(k.dve, t1[:, 0:n], XR[u][:, 0:n], C[:, 0:n], ALU.mult, [XR[u], C], [t1])
                        k.tt(k.dve, t2[:, 0:n], XI[u][:, 0:n], S[:, 0:n], ALU.mult, [XI[u], S], [t2])
                        k.tt(k.dve, t3[:, 0:n], XI[u][:, 0:n], C[:, 0:n], ALU.mult, [XI[u], C], [t3])
                        k.tt(k.dve, t4[:, 0:n], XR[u][:, 0:n], S[:, 0:n], ALU.mult, [XR[u], S], [t4])
                    for u in range(2):
                        t1, t2, t3, t4 = T[u]
                        k.tt(k.pool, xr[u][:, 0:n], t1[:, 0:n], t2[:, 0:n], ALU.add, [t1, t2], [xr[u]])
                        k.tt(k.pool, xi[u][:, 0:n], t3[:, 0:n], t4[:, 0:n], ALU.subtract, [t3, t4], [xi[u]])
                    for u, (j, kk, rows, BTt, ccols, S, C) in enumerate(st_):
                        rb = P["R"][:, j:j + 1].broadcast_to([128, n])
                        iv = ini[u]
                        i0 = 0.0 if first else iv[:, 0:1]
                        i1 = 0.0 if first else iv[:, 1:2]
                        rd = [P["R"], xr[u]] + ([] if first else [iv])
                        k.op(k.dve, lambda e, u=u, n=n, rb=rb, i0=i0: e.tensor_tensor_scan(
                            out=gR[u][:, 0:n], data0=rb, data1=xr[u][:, 0:n], initial=i0, op0=ALU.mult, op1=ALU.add),
                            rd, [gR[u]])
                        rd = [P["R"], xi[u]] + ([] if first else [iv])
                        k.op(k.dve, lambda e, u=u, n=n, rb=rb, i1=i1: e.tensor_tensor_scan(
                            out=gI[u][:, 0:n], data0=rb, data1=xi[u][:, 0:n], initial=i1, op0=ALU.mult, op1=ALU.add),
                            rd, [gI[u]])
                    for u, (j, kk, rows, BTt, ccols, S, C) in enumerate(st_):
                        iv, us_ = ini[u], us[u]
                        cT, sT = C[:, n:n + 1], S[:, n:n + 1]
                        k.ts(k.dve, us_[:, 0:1], gI[u][:, n - 1:n], sT, None, ALU.mult, None, [gI[u], S], [us_])
                        k.ts(k.dve, us_[:, 1:2], gI[u][:, n - 1:n], cT, None, ALU.mult, None, [gI[u], C], [us_])
                        k.stt(iv[:, 0:1], gR[u][:, n - 1:n], cT, us_[:, 0:1], ALU.mult, ALU.subtract, [gR[u], C, us_], [iv])
                        k.stt(iv[:, 1:2], gR[u][:, n - 1:n], sT, us_[:, 1:2], ALU.mult, ALU.add, [gR[u], S, us_], [iv])
                    for u, (j, kk, rows, BTt, ccols, S, C) in enumerate(st_):
                        t1, t2, t3, t4 = T[u]
                        k.tt(k.dve, t1[:, 0:n], gR[u][:, 0:n], C[:, 0:n], ALU.mult, [gR[u], C], [t1])
                        k.tt(k.dve, t4[:, 0:n], gI[u][:, 0:n], C[:, 0:n], ALU.mult, [gI[u], C], [t4])
                        k.tt(k.pool, t2[:, 0:n], gI[u][:, 0:n], S[:, 0:n], ALU.mult, [gI[u], S], [t2])
                        k.tt(k.pool, hR[u][:, 0:n], t1[:, 0:n], t2[:, 0:n], ALU.subtract, [t1, t2], [hR[u]])
                        k.tt(k.pool, t3[:, 0:n], gR[u][:, 0:n], S[:, 0:n], ALU.mult, [gR[u], S], [t3])
                    for u in range(2):
                        t1, t2, t3, t4 = T[u]
                        k.stt(hI[u][:, 0:n], t3[:, 0:n], -1.0, t4[:, 0:n], ALU.mult, ALU.subtract, [t3, t4], [hI[u]])
                    for u, (j, kk, rows, BTt, ccols, S, C) in enumerate(st_):
                        k.mm(YP[u][rows, 0:n], P["CT"][:, 0, j, ccols], hR[u][:, 0:n], True, False, [P["CT"], hR[u]], [YP[u]])
                        k.mm(YP[u][rows, 0:n], P["CT"][:, 1, j, ccols], hI[u][:, 0:n], False, True, [P["CT"], hI[u]], [YP[u]])
                    for u, (j, kk, rows, BTt, ccols, S, C) in enumerate(st_):
                        if d == 0:
                            k.cp(k.act, Y[rows, kk, t0:t0 + n], YP[u][rows, 0:n], [YP[u]], [Y])
                        else:
                            k.tt(k.dve, Y[rows, kk, t0:t0 + n], YP[u][rows, n - 1::-1], Y[rows, kk, t0:t0 + n], ALU.add,
                                 [YP[u], Y], [Y])

    def s5_glu(self, sb_, l, b, Y, uTb, dsk, bgl, WG):
        k = self.k
        UT, Y5T = self.dram["UT"], self.dram["Y5T"]
        WG = k.sb(sb_, [128, 8, 1024], BF16, "WG", dma=True)
        k.dma(k.pool, [(WG[:], self.dram["s5_w_glu"][l].rearrange("(kc p) n -> p kc n", p=128))], WG,
              reads=[self.dbuf["s5_w_glu"]], writes=[WG])
        uf = [k.sb(sb_, [128, TOK], F32, "uf", dma=True) for _ in range(2)]
        z = uTb
        for kk in range(8):
            f = uf[kk % 2]
            k.dma(k.sp, [(f[:], UT[kk * 128:(kk + 1) * 128, b * TOK:(b + 1) * TOK])], f, reads=[self.dbuf["UT"]], writes=[f])
            k.stt(Y[:, kk, :], f[:], dsk[:, kk:kk + 1], Y[:, kk, :], ALU.mult, ALU.add, [f, dsk, Y], [Y])
            k.actf(z[:, kk, :], Y[:, kk, :], AF.Gelu_apprx_tanh, [Y], [z])
        pss = [k.ps(sb_, [128, 512]) for _ in range(2)]
        sg = [k.sb(sb_, [128, 512], F32, "sg") for _ in range(2)]
        ob = [k.sb(sb_, [128, 512], BF16, "ob", dma=True) for _ in range(2)]
        blocks = [(0, 256)] + [(256 + q * 512, 512) for q in range(4)]
        n_ = 0
        for oc in range(8):
            for (t0, n) in blocks:
                p = pss[n_ % 2]
                s_ = sg[n_ % 2]
                o = ob[n_ % 2]
                n_ += 1
                for kc in range(8):
                    k.mm(p[:, 0:n], WG[:, kc, oc * 128:(oc + 1) * 128], z[:, kc, t0:t0 + n], kc == 0, kc == 7, [WG, z], [p])
                k.actf(s_[:, 0:n], p[:, 0:n], AF.Sigmoid, [p, bgl], [s_], bias=bgl[:, oc:oc + 1])
                k.tt(k.dve, o[:, 0:n], s_[:, 0:n], z[:, oc, t0:t0 + n], ALU.mult, [s_, z], [o])
                k.dma(k.sp, [(Y5T[oc * 128:(oc + 1) * 128, b * TOK + t0:b * TOK + t0 + n], o[:, 0:n])], o,
                      reads=[o], writes=[self.dbuf["Y5T"]], accumulate=True)


    def phase_hg_lb(self, st):
        k = self.k
        self.LBT = k.sb(st, [128, DEPTH, 16], F32, "LBT")
        self.OML = k.sb(st, [128, DEPTH, 16], F32, "OML")
        with self.k.scope() as s2:
            gam = k.sb(s2, [128, DEPTH, 16], F32, "gam", dma=True)
            k.dma(k.sp, [(gam[:], self.dram["gam_l"][:, :, :])], gam, reads=[self.dbuf["gam_l"]], writes=[gam])
            e = k.sb(s2, [128, DEPTH, 16], F32, "ge")
            sm = k.sb(s2, [128, 16], F32, "gs")
            k.actf(e[:], gam[:], AF.Exp, [gam], [e])
            k.tt(k.dve, sm[:], e[:, 0, :], e[:, 1, :], ALU.add, [e], [sm])
            for i in range(2, DEPTH):
                k.tt(k.dve, sm[:], sm[:], e[:, i, :], ALU.add, [e, sm], [sm])
            k.op(k.dve, lambda en: en.reciprocal(out=sm[:], in_=sm[:]), [sm], [sm])
            k.op(k.dve, lambda en: en.memset(self.LBT[:], 0.0), [], [self.LBT])
            for i in range(1, DEPTH):
                k.tt(k.dve, e[:, i, :], e[:, i, :], sm[:], ALU.mult, [e, sm], [e])
                k.tt(k.dve, self.LBT[:, i, :], self.LBT[:, i - 1, :], e[:, i, :], ALU.add, [e, self.LBT], [self.LBT])
            k.ts(k.dve, self.OML[:], self.LBT[:], -1.0, 1.0, ALU.mult, ALU.add, [self.LBT], [self.OML])

    def phase_hgrn(self, l):
        k = self.k
        with self.k.scope() as st:
            ghg = k.sb(st, [128, 8], F32, "ghg", dma=True)
            k.dma(k.sp, [(ghg[:], self.dram["ghg_l"][l])], ghg, reads=[self.dbuf["ghg_l"]], writes=[ghg])
            pat = k.sb(st, [128, 32], F32, "pat")
            k.op(k.dve, lambda e: e.memset(pat[:], 1.0), [], [pat])
            k.op(k.dve, lambda e: e.memset(pat[:, 0:1], 0.0), [], [pat])
            mask = k.sb(st, [128, TOK + 32], F32, "mask")
            k.cp(k.dve, mask[:].rearrange("p (n s) -> p n s", s=32), pat[:].unsqueeze(1).broadcast_to([128, TOK // 32 + 1, 32]),
                 [pat], [mask])
            mk = k.sb(st, [64, 2, 64], F32, "mk", dma=True)
            k.dma(k.sp, [(mk[:, 0, :], self.dram["cst"][1, 0:64, 0:64]), (mk[:, 1, :], self.dram["cst"][2, 0:64, 0:64])], mk,
                  reads=[self.dbuf["cst"]], writes=[mk])
            onesq = k.sb(st, [128, 128], F32, "onesq")
            k.op(k.dve, lambda e: e.memset(onesq[:], 1.0), [], [onesq])
            for b in range(NB):
                for h in range(8):
                    if "hg_heads" in self.dbg and (b, h) not in self.dbg["hg_heads"]:
                        continue
                    with self.k.scope() as sh:
                        self.hg_head(sh, l, b, h, ghg, mask, mk, onesq)

    def hg_head(self, sh, l, b, h, ghg, mask, mk, onesq):
        k = self.k
        QFT, VOG, YHT = self.dram["QFT"], self.dram["VOG"], self.dram["YHT"]
        c0 = b * TOK
        NCH = TOK // 32
        NG = TOK // 64
        qraw = k.sb(sh, [128, TOK], F32, "qraw", dma=True)
        fraw = k.sb(sh, [128, TOK], F32, "fraw", dma=True)
        qP = k.sb(sh, [128, TOK], F32, "qP")
        sogP = k.sb(sh, [128, TOK], F32, "sogP")
        T1 = k.sb(sh, [128, TOK], F32, "T1")
        T2 = k.sb(sh, [128, TOK], F32, "T2")
        T3 = k.sb(sh, [128, TOK], F32, "T3")
        T4 = k.sb(sh, [128, TOK], F32, "T4")
        T5 = k.sb(sh, [128, TOK], F32, "T5")
        A = k.sb(sh, [128, TOK], BF16, "A")
        Bt = k.sb(sh, [128, TOK], BF16, "B")
        KD = k.sb(sh, [64, NG, 128], BF16, "KD")
        VT = k.sb(sh, [64, NG, 128], BF16, "VT", dma=True)
        QB = [k.sb(sh, [128, TOK], BF16, "QB") for _ in range(2)]
        SIN_ = [k.sb(sh, [128, NCH, 128], BF16, "Sin") for _ in range(2)]
        SC = [k.sb(sh, [64, NG, 64], BF16, "SC") for _ in range(2)]
        dec = k.sb(sh, [128, NCH], F32, "dec")
        S = k.sb(sh, [128, 128], F32, "S")
        YR = k.sb(sh, [128, TOK], BF16, "YR", dma=True)
        PT = [k.ps(sh, [128, 512]) for _ in range(2)]
        KV = [k.ps(sh, [128, 512]) for _ in range(4)]
        PO = [k.ps(sh, [128, 512]) for _ in range(2)]
        rows = slice(h * 128, (h + 1) * 128)
        k.dma(k.sp, [(qraw[:], QFT[rows, c0:c0 + TOK])], qraw, reads=[self.dbuf["QFT"]], writes=[qraw])
        k.dma(k.sp, [(fraw[:], QFT[3072 + h * 128:3072 + (h + 1) * 128, c0:c0 + TOK])], fraw, reads=[self.dbuf["QFT"]], writes=[fraw])
        vsrc = VOG[c0 + CTX:c0 + TOK, rows].rearrange("(r g c) d -> c r g d", g=32, c=2)
        k.dma(k.pool, [(VT[0:64, 0:4, :], VOG[c0:c0 + CTX, rows].rearrange("(g s) d -> s g d", s=64)),
                       (VT[0:32, 4:NG, :], vsrc[0]), (VT[32:64, 4:NG, :], vsrc[1])], VT,
              reads=[self.dbuf["VOG"]], writes=[VT])

        def toP(func, o, i):
            k.actf(o[:, 0:CTX], i[:, 0:CTX], func, [i], [o])
            k.actf(o[:, CTX:].rearrange("p (c r) -> p c r", r=32), i[:, CTX:].rearrange("p (r c) -> p c r", c=64), func, [i], [o])

        toP(AF.Silu, qP, qraw)
        toP(AF.Silu, sogP, fraw)
        segs = [(0, CTX), (CTX, TOK)]
        n_pt = 0
        n_kv = 0
        for d in range(2):
            col = d * 8 + h
            r0 = 1024 * (1 + d) + h * 128
            k.dma(k.sp, [(fraw[:], QFT[r0:r0 + 128, c0:c0 + TOK])], fraw, reads=[self.dbuf["QFT"]], writes=[fraw])
            toP(AF.Sigmoid, T1, fraw)
            k.ts(k.dve, T1[:], T1[:], self.OML[:, l, col:col + 1], self.LBT[:, l, col:col + 1], ALU.mult, ALU.add,
                 [T1, self.OML, self.LBT], [T1])
            k.actf(T2[:], T1[:], AF.Ln, [T1], [T2])
            k.ts(k.pool, T1[:], T1[:], -1.0, 1.0, ALU.mult, ALU.add, [T1], [T1])
            for (a, e_) in segs:
                sl = slice(a, e_) if d == 0 else slice(e_ - 1, (a - 1) if a > 0 else None, -1)
                slm = slice(a, e_) if d == 0 else slice(e_, a, -1)
                k.op(k.dve, lambda en, sl=sl, slm=slm: en.tensor_tensor_scan(out=T3[:, sl], data0=mask[:, slm], data1=T2[:, sl],
                                                                 initial=0.0, op0=ALU.mult, op1=ALU.add), [mask, T2], [T3])
            b3 = T3[:].rearrange("p (n s) -> p n s", s=32)
            iL, iR = (31, 15) if d == 0 else (0, 16)
            BL3 = b3[:, :, iL:iL + 1].broadcast_to([128, NCH, 32])
            BR3 = b3[:, :, iR:iR + 1].broadcast_to([128, NCH, 32])
            v3 = lambda t: t[:].rearrange("p (n s) -> p n s", s=32)
            k.tt(k.dve, v3(T4), BL3, b3, ALU.subtract, [T3], [T4])
            k.actf(T4[:], T4[:], AF.Exp, [T4], [T4])
            k.tt(k.pool, A[:], T1[:], T4[:], ALU.mult, [T1, T4], [A])
            k.actf(dec[:], b3[:, :, iL], AF.Exp, [T3], [dec])
            if d == self.dbg.get("hg_dd", 0):
                self.dump("hg_f", T1, T1[:], [128, TOK])
                self.dump("hg_b", T3, T3[:], [128, TOK])
                self.dump("hg_dec", dec, dec[:], [128, NCH])
                self.dump("hg_kdec", A, A[:], [128, TOK], BF16)
                self.dump("hg_qP", qP, qP[:], [128, TOK])
                self.dump("hg_VT", VT, VT[:], [64, NG, 128], BF16)
            if self.dbg.get("hg_stop", 9) <= 1:
                continue
            for q in range(NG // 4):
                p = PT[n_pt % 2]
                n_pt += 1
                for jj in range(4):
                    g = q * 4 + jj
                    k.mm(p[0:64, jj * 128:(jj + 1) * 128], A[:, 64 * g:64 * g + 64], self.ident_b[:], True, True,
                         [A, self.ident_b], [p])
                k.cp(k.act, KD[:, q * 4:(q + 1) * 4, :], p[0:64, :].rearrange("p (a b) -> p a b", a=4), [p], [KD])
            if self.dbg.get("hg_sub", 9) <= 1:
                continue
            order = list(range(NCH)) if d == 0 else (list(range(7, -1, -1)) + list(range(NCH - 1, 7, -1)))
            k.op(k.dve, lambda en: en.memset(S[:], 0.0), [], [S])
            for i8 in range(0, NCH, 8):
                pc = [KV[(n_kv % 2) * 2 + 0], KV[(n_kv % 2) * 2 + 1]]
                n_kv += 1
                for jj in range(8):
                    n = order[i8 + jj]
                    g, c = n // 2, n % 2
                    sl_ = (jj // 2) * 128
                    k.mm(pc[c][:, sl_:sl_ + 128], KD[32 * c:32 * c + 32, g, :], VT[32 * c:32 * c + 32, g, :], True, True,
                         [KD, VT], [pc[c]])
                if self.dbg.get("hg_sub", 9) <= 2:
                    continue
                for jj in range(8):
                    n = order[i8 + jj]
                    c = n % 2
                    sl_ = (jj // 2) * 128
                    k.cp(k.act, SIN_[d][:, n, :], S[:], [S], [SIN_[d]])
                    k.stt(S[:], S[:], dec[:, n:n + 1], pc[c][:, sl_:sl_ + 128], ALU.mult, ALU.add, [S, dec, pc[c]], [S])
            if self.dbg.get("hg_stop", 9) <= 2:
                continue
            k.tt(k.dve, v3(T4), b3, BR3, ALU.subtract, [T3], [T4])
            k.actf(T5[:], T4[:], AF.Exp, [T4], [T5])
            k.actf(T4[:], T4[:], AF.Exp, [T4], [T4], scale=-1.0)
            k.tt(k.pool, A[:], qP[:], T5[:], ALU.mult, [qP, T5], [A])
            k.tt(k.dve, Bt[:], T1[:], T4[:], ALU.mult, [T1, T4], [Bt])
            for q in range((NG + 7) // 8):
                p = PT[n_pt % 2]
                n_pt += 1
                ng = min(8, NG - q * 8)
                for jj in range(ng):
                    g = q * 8 + jj
                    k.mm(p[0:64, jj * 64:(jj + 1) * 64], Bt[:, 64 * g:64 * g + 64], A[:, 64 * g:64 * g + 64], True, True,
                         [A, Bt], [p])
                k.tt(k.dve, SC[d][:, q * 8:q * 8 + ng, :], p[0:64, 0:ng * 64].rearrange("p (a b) -> p a b", b=64),
                     mk[:, d, :].unsqueeze(1).broadcast_to([64, ng, 64]), ALU.mult, [p, mk], [SC[d]])
            k.actf(T5[:], T3[:], AF.Exp, [T3], [T5])
            k.tt(k.pool, QB[d][:], qP[:], T5[:], ALU.mult, [qP, T5], [QB[d]])
            if d == self.dbg.get("hg_dd", 0):
                self.dump("hg_KD", KD, KD[:], [64, NG, 128], BF16)
                self.dump("hg_sin", SIN_[d], SIN_[d][:], [128, NCH, 128], BF16)
                self.dump("hg_sc", SC[d], SC[d][:], [64, NG, 64], BF16)
                self.dump("hg_qb", QB[d], QB[d][:], [128, TOK], BF16)
        if self.dbg.get("hg_stop", 9) <= 3:
            return
        n_po = 0
        for q in range((NG + 7) // 8):
            p = PO[n_po % 2]
            n_po += 1
            ng = min(8, NG - q * 8)
            for jj in range(ng):
                g = q * 8 + jj
                o = lambda a_, b_: p[:, jj * 64 + a_:jj * 64 + b_]
                k.mm(o(0, 64), VT[0:64, g, :], SC[0][:, g, :], True, False, [VT, SC[0]], [p])
                k.mm(o(0, 64), VT[0:64, g, :], SC[1][:, g, :], False, False, [VT, SC[1]], [p])
                for d in range(2):
                    for c in range(2):
                        n = 2 * g + c
                        k.mm(o(32 * c, 32 * c + 32), SIN_[d][:, n, :], QB[d][:, 32 * n:32 * n + 32], False, (d == 1 and c == 1),
                             [SIN_[d], QB[d]], [p])
            k.tt(k.dve, T1[:, q * 512:q * 512 + ng * 64], p[:, 0:ng * 64], sogP[:, q * 512:q * 512 + ng * 64], ALU.mult,
                 [p, sogP], [T1])
        self.dump("hg_y", T1, T1[:], [128, TOK])
        if self.dbg.get("hg_stop", 9) <= 4:
            return
        k.actf(T2[:], T1[:], AF.Square, [T1], [T2])
        blocks = [(q * 512, 512) for q in range(4)] + [(2048, 256)]
        for (t0, n) in blocks:
            p = PT[n_pt % 2]
            n_pt += 1
            k.mm(p[:, 0:n], onesq[:], T2[:, t0:t0 + n], True, True, [onesq, T2], [p])
            k.ts(k.dve, T3[:, t0:t0 + n], p[:, 0:n], 1.0 / 128, EPS, ALU.mult, ALU.add, [p], [T3])
        k.actf(T3[:], T3[:], AF.Sqrt, [T3], [T3])
        k.op(k.dve, lambda en: en.reciprocal(out=T3[:], in_=T3[:]), [T3], [T3])
        k.stt(T2[:], T1[:], ghg[:, h:h + 1], T3[:], ALU.mult, ALU.mult, [T1, ghg, T3], [T2])
        k.cp(k.act, YR[:, 0:CTX], T2[:, 0:CTX], [T2], [YR])
        k.cp(k.pool, YR[:, CTX:].rearrange("p (r c) -> p c r", c=64), T2[:, CTX:].rearrange("p (c r) -> p c r", r=32), [T2], [YR])
        k.dma(k.sp, [(YHT[rows, c0:c0 + TOK], YR[:])], YR, reads=[YR], writes=[self.dbuf["YHT"]], accumulate=True)


    def phase_merge(self, l):
        k = self.k
        X, MODS, GT, Y5T, YHT = (self.dram[n] for n in ("X", "MODS", "GT", "Y5T", "YHT"))
        blocks = [(0, 256)] + [(256 + q * 512, 512) for q in range(4)]
        wv = lambda name: self.dram[name][l].rearrange("(kc p) n -> p kc n", p=128)
        for b in range(NB):
            c0 = b * TOK
            with self.k.scope() as st:
                mT = k.sb(st, [128, 16, TOK], BF16, "mT")
                with self.k.scope() as s1:
                    WBS = k.sb(s1, [128, 8, D], BF16, "WBS", dma=True)
                    WBH = k.sb(s1, [128, 8, D], BF16, "WBH", dma=True)
                    for (W, nm) in ((WBS, "w_branch_s5"), (WBH, "w_branch_hg")):
                        k.dma(k.pool, [(W[:, 2 * i:2 * i + 2, :], wv(nm)[:, 2 * i:2 * i + 2, :]) for i in range(4)], W,
                              reads=[self.dbuf[nm]], writes=[W])
                    y5 = [k.sb(s1, [128, 8, 512], BF16, "y5", dma=True) for _ in range(2)]
                    yh = [k.sb(s1, [128, 8, 512], BF16, "yh", dma=True) for _ in range(2)]
                    g5 = [k.sb(s1, [128, 512], F32, "g5", dma=True) for _ in range(2)]
                    gh = [k.sb(s1, [128, 512], F32, "gh", dma=True) for _ in range(2)]
                    t1 = [k.sb(s1, [128, 512], F32, "t1") for _ in range(2)]
                    t2 = [k.sb(s1, [128, 512], F32, "t2") for _ in range(2)]
                    pa = [k.ps(s1, [128, 512]) for _ in range(2)]
                    pb = [k.ps(s1, [128, 512]) for _ in range(2)]
                    n_ = 0
                    for bi, (t0, n) in enumerate(blocks):
                        a5, ah = y5[bi % 2], yh[bi % 2]
                        k.dma(k.sp, [(a5[:, :, 0:n], Y5T[:, c0 + t0:c0 + t0 + n].rearrange("(kc p) t -> p kc t", p=128))], a5,
                              reads=[self.dbuf["Y5T"]], writes=[a5])
                        k.dma(k.sp, [(ah[:, :, 0:n], YHT[:, c0 + t0:c0 + t0 + n].rearrange("(kc p) t -> p kc t", p=128))], ah,
                              reads=[self.dbuf["YHT"]], writes=[ah])
                        for oc in range(16):
                            u = n_ % 2
                            n_ += 1
                            k.dma(k.sp, [(g5[u][:, 0:n], GT[oc * 128:(oc + 1) * 128, c0 + t0:c0 + t0 + n])], g5[u],
                                  reads=[self.dbuf["GT"]], writes=[g5[u]])
                            k.dma(k.sp, [(gh[u][:, 0:n], GT[2048 + oc * 128:2048 + (oc + 1) * 128, c0 + t0:c0 + t0 + n])], gh[u],
                                  reads=[self.dbuf["GT"]], writes=[gh[u]])
                            for kc in range(8):
                                k.mm(pa[u][:, 0:n], WBS[:, kc, oc * 128:(oc + 1) * 128], a5[:, kc, 0:n], kc == 0, kc == 7, [WBS, a5], [pa[u]])
                            for kc in range(8):
                                k.mm(pb[u][:, 0:n], WBH[:, kc, oc * 128:(oc + 1) * 128], ah[:, kc, 0:n], kc == 0, kc == 7, [WBH, ah], [pb[u]])
                            k.tt(k.dve, t1[u][:, 0:n], pa[u][:, 0:n], g5[u][:, 0:n], ALU.mult, [pa[u], g5[u]], [t1[u]])
                            k.tt(k.dve, t2[u][:, 0:n], pb[u][:, 0:n], gh[u][:, 0:n], ALU.mult, [pb[u], gh[u]], [t2[u]])
                            k.tt(k.pool, mT[:, oc, t0:t0 + n], t1[u][:, 0:n], t2[u][:, 0:n], ALU.add, [t1[u], t2[u]], [mT])
                WO = k.sb(st, [128, 16, D], BF16, "WO", dma=True)
                k.dma(k.pool, [(WO[:, 2 * i:2 * i + 2, :], wv("w_out")[:, 2 * i:2 * i + 2, :]) for i in range(8)], WO,
                      reads=[self.dbuf["w_out"]], writes=[WO])
                gtm = [k.sb(st, [128, D], F32, "gtm", dma=True) for _ in range(2)]
                for j, bb in enumerate((NB, b)):
                    k.dma(k.sp, [(gtm[j][:], MODS[bb, :, 2 * D:3 * D])], gtm[j], reads=[self.dbuf["MODS"]], writes=[gtm[j]])
                xin = [k.sb(st, [128, D], F32, "xin", dma=True) for _ in range(2)]
                xo = [k.sb(st, [128, D], F32, "xo", dma=True) for _ in range(2)]
                tm = [k.sb(st, [128, 512], F32, "tm") for _ in range(2)]
                pso = [k.ps(st, [128, 512]) for _ in range(4)]
                n_ = 0
                for i in range(TOK // 128):
                    r0 = c0 + i * 128
                    xi_, xo_ = xin[i % 2], xo[i % 2]
                    g_ = gtm[0 if i < CTX // 128 else 1]
                    k.dma(k.sp, [(xi_[:], X[r0:r0 + 128, :])], xi_, reads=[self.dbuf["X"]], writes=[xi_])
                    for cc in range(4):
                        p = pso[n_ % 4]
                        tmv = tm[n_ % 2]
                        n_ += 1
                        cs = slice(cc * 512, (cc + 1) * 512)
                        for kc in range(16):
                            k.mm(p[:], mT[:, kc, i * 128:(i + 1) * 128], WO[:, kc, cs], kc == 0, kc == 15, [mT, WO], [p])
                        k.tt(k.dve, tmv[:], p[:], g_[:, cs], ALU.mult, [p, g_], [tmv])
                        k.tt(k.pool, xo_[:, cs], tmv[:], xi_[:, cs], ALU.add, [tmv, xi_], [xo_])
                    k.dma(k.sp, [(X[r0:r0 + 128, :], xo_[:])], xo_, reads=[xo_], writes=[self.dbuf["X"]], accumulate=True)

    def phase_peer(self, l):
        k = self.k
        X, MODS = self.dram["X"], self.dram["MODS"]
        UTAB, VTAB = self.dram["U16"], self.dram["V16"]
        with self.k.scope() as st:
            cds = k.dfree.pop(0)
            st.callback(lambda d_=cds: k.dfree.append(d_))
            for (src, dst, nm_, dn) in ((self.dram["peer_u"][l], UTAB, "peer_u", "U16"), (self.dram["peer_v"][l], VTAB, "peer_v", "V16")):
                k.dma(k.pool, [(dst[r_:r_ + 1024, :], src[r_:r_ + 1024, :]) for r_ in range(0, NEXP, 1024)], cds,
                      reads=[self.dbuf[nm_]], writes=[self.dbuf[dn]], accumulate=True)
            WQ = k.sb(st, [128, 16, D], BF16, "WQ", dma=True)
            k.dma(k.pool, [(WQ[:, 2 * i:2 * i + 2, :], self.dram["peer_w_q"][l].rearrange("(kc p) n -> p kc n", p=128)[:, 2 * i:2 * i + 2, :])
                           for i in range(8)], WQ, reads=[self.dbuf["peer_w_q"]], writes=[WQ])
            KT = k.sb(st, [128, 16, 128], BF16, "KT", dma=True)
            k.dma(k.pool, [(KT[:], self.dram["peer_kt"][l])], KT, reads=[self.dbuf["peer_kt"]], writes=[KT])
            io16 = k.sb(st, [128, 16], F32, "io16", dma=True)
            k.dma(k.sp, [(io16[:], self.dram["cst2"][:, 0:16])], io16, reads=[self.dbuf["cst2"]], writes=[io16])
            AF_ = k.sb(st, [128, D], F32, "AF", dma=True)
            SF_ = k.sb(st, [128, D], F32, "SF", dma=True)
            GF_ = k.sb(st, [128, D], F32, "GF", dma=True)
            xin = k.sb(st, [128, D], F32, "xin", dma=True)
            tn = k.sb(st, [128, D], F32, "tn", dma=True)
            xo = tn
            tmp = k.sb(st, [128, D], F32, "tmp")
            junk = k.sb(st, [128, D], BF16, "junk")
            tnb = k.sb(st, [128, D], BF16, "tnb")
            tnT = k.sb(st, [128, 16, 128], BF16, "tnT")
            qb = k.sb(st, [128, D], BF16, "qb")
            qT = k.sb(st, [128, 16, 128], BF16, "qT")
            Ssb = k.sb(st, [128, 16, 128], F32, "Ssb")
            ss = k.sb(st, [128, 1], F32, "ss")
            rs = k.sb(st, [128, 1], F32, "rs")
            NU = 4
            UB = [k.sb(st, [128, D], BF16, "UB", dma=True) for _ in range(NU)]
            VB = [k.sb(st, [128, D], BF16, "VB", dma=True) for _ in range(NU)]
            DJ = [k.sb(st, [128, 128], BF16, "DJ") for _ in range(2)]
            v8 = k.sb(st, [128, 2, 16], F32, "v8")
            i8 = k.sb(st, [128, 2, 16], U32, "i8")
            i8f = k.sb(st, [128, 2, 16], F32, "i8f")
            srep = k.sb(st, [128, 128], F32, "srep")
            cand = k.sb(st, [128, 256], F32, "cand")
            cand2 = k.sb(st, [128, 256], F32, "cand2")
            sc = k.sb(st, [128, 16], F32, "sc")
            ic = k.sb(st, [128, 16], U32, "ic")
            icf = k.sb(st, [128, 16], F32, "icf")
            hi_i = k.sb(st, [128, 16], I32, "hi_i")
            hif = k.sb(st, [128, 16], F32, "hif")
            lof = k.sb(st, [128, 16], F32, "lof")
            oh = k.sb(st, [128, 16, 16], F32, "oh")
            e1 = k.sb(st, [128, 16], F32, "e1")
            e2 = k.sb(st, [128, 16], F32, "e2")
            EXPf = k.sb(st, [128, 128], F32, "EXPf")
            EXPi = k.sb(st, [128, 128], I32, "EXPi")
            G = k.sb(st, [128, 128], F32, "G")
            nm = k.sb(st, [128, 1], F32, "nm")
            sm = k.sb(st, [128, 1], F32, "sm")
            araw = k.sb(st, [128, 128], F32, "araw")
            coef = k.sb(st, [128, 128], F32, "coef")
            pt = [k.ps(st, [128, 512]) for _ in range(3)]
            pacc = [k.ps(st, [128, 512]) for _ in range(4)]
            n_pt = 0
            cur_bb = None
            n_u = 0
            n_v = 0
            tiles = self.dbg.get("peer_tiles", list(range(NT // 128)))
            for ti in tiles:
                b, i = ti // (TOK // 128), ti % (TOK // 128)
                bb = NB if i < CTX // 128 else b
                if bb != cur_bb:
                    cur_bb = bb
                    k.dma(k.sp, [(AF_[:], MODS[bb, :, 4 * D:5 * D])], AF_, reads=[self.dbuf["MODS"]], writes=[AF_])
                    k.dma(k.sp, [(SF_[:], MODS[bb, :, 3 * D:4 * D])], SF_, reads=[self.dbuf["MODS"]], writes=[SF_])
                    k.dma(k.sp, [(GF_[:], MODS[bb, :, 5 * D:6 * D])], GF_, reads=[self.dbuf["MODS"]], writes=[GF_])
                r0 = ti * 128
                k.dma(k.sp, [(xin[:], X[r0:r0 + 128, :])], xin, reads=[self.dbuf["X"]], writes=[xin])
                self.norm_mod_tile((junk, ss, rs, tmp), xin, AF_, SF_, tn)
                k.cp(k.act, tnb[:], tn[:], [tn], [tnb])
                for q4 in range(4):
                    p = pt[n_pt % 3]
                    n_pt += 1
                    for jj in range(4):
                        kc = q4 * 4 + jj
                        k.mm(p[:, jj * 128:(jj + 1) * 128], tnb[:, kc * 128:(kc + 1) * 128], self.ident_b[:], True, True,
                             [tnb, self.ident_b], [p])
                    k.cp(k.act if q4 % 2 else k.dve, tnT[:, q4 * 4:(q4 + 1) * 4, :], p[:].rearrange("p (a b) -> p a b", a=4), [p], [tnT])
                for cc in range(4):
                    p = pt[n_pt % 3]
                    n_pt += 1
                    for kc in range(16):
                        k.mm(p[:], tnT[:, kc, :], WQ[:, kc, cc * 512:(cc + 1) * 512], kc == 0, kc == 15, [tnT, WQ], [p])
                    k.cp(k.act if cc % 2 else k.dve, qb[:, cc * 512:(cc + 1) * 512], p[:], [p], [qb])
                for q4 in range(4):
                    p = pt[n_pt % 3]
                    n_pt += 1
                    for jj in range(4):
                        kc = q4 * 4 + jj
                        k.mm(p[:, jj * 128:(jj + 1) * 128], qb[:, kc * 128:(kc + 1) * 128], self.ident_b[:], True, True,
                             [qb, self.ident_b], [p])
                    k.cp(k.act if q4 % 2 else k.dve, qT[:, q4 * 4:(q4 + 1) * 4, :], p[:].rearrange("p (a b) -> p a b", a=4), [p], [qT])
                for q4 in range(4):
                    p = pt[n_pt % 3]
                    n_pt += 1
                    for jj in range(4):
                        hh = q4 * 4 + jj
                        k.mm(p[:, jj * 128:(jj + 1) * 128], qT[:, hh, :], KT[:, hh, :], True, True, [qT, KT], [p])
                    k.cp(k.act, Ssb[:, q4 * 4:(q4 + 1) * 4, :], p[:].rearrange("p (a b) -> p a b", a=4), [p], [Ssb])
                for h in range(8):
                    for half in range(2):
                        sv = Ssb[:, 2 * h + half, :]
                        k.op(k.dve, lambda e, sv=sv, half=half: e.max(out=v8[:, half, 0:8], in_=sv), [Ssb], [v8])
                        k.op(k.dve, lambda e, sv=sv, half=half: e.max_index(out=i8[:, half, 0:8], in_max=v8[:, half, 0:8], in_values=sv),
                             [Ssb, v8], [i8])
                        k.op(k.dve, lambda e, sv=sv, half=half: e.match_replace(out=srep[:], in_to_replace=v8[:, half, 0:8],
                                                                              in_values=sv, imm_value=-1e30), [Ssb, v8], [srep])
                        k.op(k.dve, lambda e, half=half: e.max(out=v8[:, half, 8:16], in_=srep[:]), [srep], [v8])
                        k.op(k.dve, lambda e, half=half: e.max_index(out=i8[:, half, 8:16], in_max=v8[:, half, 8:16], in_values=srep[:]),
                             [srep, v8], [i8])
                    k.cp(k.dve, i8f[:], i8[:], [i8], [i8f])
                    k.tt(k.dve, cand[:].rearrange("p (a b) -> p a b", a=16), v8[:, 0, :].unsqueeze(2).broadcast_to([128, 16, 16]),
                         v8[:, 1, :].unsqueeze(1).broadcast_to([128, 16, 16]), ALU.add, [v8], [cand])
                    k.op(k.dve, lambda e: e.max(out=sc[:, 0:8], in_=cand[:]), [cand], [sc])
                    k.op(k.dve, lambda e: e.max_index(out=ic[:, 0:8], in_max=sc[:, 0:8], in_values=cand[:]), [cand, sc], [ic])
                    k.op(k.dve, lambda e: e.match_replace(out=cand2[:], in_to_replace=sc[:, 0:8], in_values=cand[:], imm_value=-1e30),
                         [cand, sc], [cand2])
                    k.op(k.dve, lambda e: e.max(out=sc[:, 8:16], in_=cand2[:]), [cand2], [sc])
                    k.op(k.dve, lambda e: e.max_index(out=ic[:, 8:16], in_max=sc[:, 8:16], in_values=cand2[:]), [cand2, sc], [ic])
                    k.cp(k.dve, icf[:], ic[:], [ic], [icf])
                    k.ts(k.dve, hi_i[:], icf[:], 1.0 / 16, -15.0 / 32, ALU.mult, ALU.add, [icf], [hi_i])
                    k.cp(k.dve, hif[:], hi_i[:], [hi_i], [hif])
                    k.stt(lof[:], hif[:], -16.0, icf[:], ALU.mult, ALU.add, [hif, icf], [lof])
                    for (sel, idxf, eo) in ((hif, i8f[:, 0, :], e1), (lof, i8f[:, 1, :], e2)):
                        k.tt(k.dve, oh[:], sel[:].unsqueeze(2).broadcast_to([128, 16, 16]),
                             io16[:].unsqueeze(1).broadcast_to([128, 16, 16]), ALU.is_equal, [sel, io16], [oh])
                        k.tt(k.dve, oh[:], oh[:], idxf.unsqueeze(1).broadcast_to([128, 16, 16]), ALU.mult, [oh, i8f], [oh])
                        k.op(k.dve, lambda e, eo=eo: e.tensor_reduce(out=eo[:], in_=oh[:], axis=AX.X, op=ALU.add), [oh], [eo])
                    k.stt(EXPf[:, h * 16:(h + 1) * 16], e1[:], 128.0, e2[:], ALU.mult, ALU.add, [e1, e2], [EXPf])
                    k.ts(k.dve, nm[:], sc[:, 0:1], -1.0, None, ALU.mult, None, [sc], [nm])
                    k.op(k.act, lambda e, h=h: e.activation(out=G[:, h * 16:(h + 1) * 16], in_=sc[:], func=AF.Exp, bias=nm[:, 0:1],
                                                            accum_out=sm[:]), [sc, nm], [G, sm])
                    k.op(k.dve, lambda e: e.reciprocal(out=sm[:], in_=sm[:]), [sm], [sm])
                    k.ts(k.dve, G[:, h * 16:(h + 1) * 16], G[:, h * 16:(h + 1) * 16], sm[:, 0:1], None, ALU.mult, None, [G, sm], [G])
                k.cp(k.dve, EXPi[:], EXPf[:], [EXPf], [EXPi])
                for j in range(128):
                    ub = UB[n_u % NU]
                    n_u += 1
                    self.idma(ub, UTAB, EXPi, j, "U16")
                    k.op(k.dve, lambda e, ub=ub, j=j: e.scalar_tensor_tensor(
                        out=junk[:], in0=ub[:], scalar=1.0, in1=tn[:], op0=ALU.mult, op1=ALU.mult,
                        accum_out=araw[:, j:j + 1]), [ub, tn], [junk, araw])
                k.actf(coef[:], araw[:], AF.Gelu_apprx_tanh, [araw], [coef])
                k.tt(k.dve, coef[:], coef[:], G[:], ALU.mult, [coef, G], [coef])
                for j in range(128):
                    vb = VB[n_v % NU]
                    dj = DJ[n_v % 2]
                    n_v += 1
                    self.idma(vb, VTAB, EXPi, j, "V16")
                    k.ts(k.dve, dj[:], self.ident_b[:], coef[:, j:j + 1], None, ALU.mult, None, [self.ident_b, coef], [dj])
                    for cc in range(4):
                        k.mm(pacc[cc][:], dj[:], vb[:, cc * 512:(cc + 1) * 512], j == 0, j == 127, [dj, vb], [pacc[cc]])
                for cc in range(4):
                    cs = slice(cc * 512, (cc + 1) * 512)
                    k.tt(k.dve, tmp[:, cs], pacc[cc][:], GF_[:, cs], ALU.mult, [pacc[cc], GF_], [tmp])
                    k.tt(k.pool, xo[:, cs], tmp[:, cs], xin[:, cs], ALU.add, [tmp, xin], [xo])
                k.dma(k.sp, [(X[r0:r0 + 128, :], xo[:])], xo, reads=[xo], writes=[self.dbuf["X"]], accumulate=True)

    def idma(self, dst, table, idx_tile, j, tname):
        k = self.k
        Q = k.pool
        ds = dst.dsem
        k._wait(Q, k._need([idx_tile, self.dbuf[tname]], [dst]))
        Q.eng.indirect_dma_start(out=dst[:], out_offset=None, in_=table,
                                 in_offset=bass.IndirectOffsetOnAxis(ap=idx_tile[:, j:j + 1], axis=0)).then_inc(ds.sem, 16)
        ds.count += 16
        k.ninst += 1
        k._mark([idx_tile, self.dbuf[tname]], [dst], ds.key, ds.sem, ds.count)

    def phase_final(self):
        k = self.k
        X, OUT = self.dram["X"], self.dram["out"]
        with self.k.scope() as st:
            gb = k.sb(st, [128, D], F32, "gb", dma=True)
            k.dma(k.sp, [(gb[:], self.dram["final_norm_g"][0].partition_broadcast(128))], gb,
                  reads=[self.dbuf["final_norm_g"]], writes=[gb])
            xin = [k.sb(st, [128, D], F32, "xin", dma=True) for _ in range(2)]
            xo = [k.sb(st, [128, D], F32, "xo", dma=True) for _ in range(2)]
            junk = k.sb(st, [128, D], BF16, "junk")
            ss = [k.sb(st, [128, 1], F32, "ss") for _ in range(2)]
            rs = [k.sb(st, [128, 1], F32, "rs") for _ in range(2)]
            n_ = 0
            for b in range(NB):
                for i in range(SEQ // 128):
                    u = n_ % 2
                    n_ += 1
                    r0 = b * TOK + CTX + i * 128
                    k.dma(k.sp, [(xin[u][:], X[r0:r0 + 128, :])], xin[u], reads=[self.dbuf["X"]], writes=[xin[u]])
                    k.op(k.act, lambda e, u=u: e.activation(out=junk[:], in_=xin[u][:], func=AF.Square, accum_out=ss[u][:]),
                         [xin[u]], [junk, ss[u]])
                    k.ts(k.dve, rs[u][:], ss[u][:], 1.0 / D, EPS, ALU.mult, ALU.add, [ss[u]], [rs[u]])
                    k.actf(rs[u][:], rs[u][:], AF.Sqrt, [rs[u]], [rs[u]])
                    k.op(k.dve, lambda e, u=u: e.reciprocal(out=rs[u][:], in_=rs[u][:]), [rs[u]], [rs[u]])
                    k.stt(xo[u][:], xin[u][:], rs[u][:, 0:1], gb[:], ALU.mult, ALU.mult, [xin[u], rs[u], gb], [xo[u]])
                    k.dma(k.sp, [(OUT[b, i * 128:(i + 1) * 128, :], xo[u][:])], xo[u], reads=[xo[u]],
                          writes=[self.dbuf["out"]], accumulate=True)

def host_consts():
    c = np.zeros((4, 128, 128), np.float32)
    c[0] = np.eye(128, dtype=np.float32)
    i = np.arange(128)
    same = (i[:, None] // 32) == (i[None, :] // 32)
    c[1] = (same & (i[:, None] <= i[None, :])).astype(np.float32)
    c[2] = (same & (i[:, None] >= i[None, :])).astype(np.float32)
    return c


def input_specs(DEPTH):
    return [
        ("cst", [4, 128, 128], F32), ("cst2", [128, 513], F32), ("cst3", [128, 4], F32),
        ("x", [NB, SEQ, D], F32), ("ctx", [NB, CTX, D], F32), ("cvec", [NB + 1, D], F32),
        ("w_ada", [DEPTH, D, 6 * D], F32), ("b_ada", [DEPTH, 6 * D], F32),
        ("norm_mix_g", [DEPTH, D], F32), ("norm_ffn_g", [DEPTH, D], F32), ("w_in", [DEPTH, D, PW], F32),
        ("s5sl", [DEPTH, 2, 128, 3, 32], F32), ("s5bl", [DEPTH, 2, 128, 3, 512], F32),
        ("s5bt", [DEPTH, 2, 128, 2, 8, 128], F32), ("s5ct", [DEPTH, 2, 128, 2, 32, 32], F32),
        ("gam_l", [128, 4, 16], F32), ("ghg_l", [DEPTH, 128, 8], F32), ("s5d_l", [DEPTH, 128, 8], F32),
        ("w_branch_s5", [DEPTH, 1024, D], F32), ("w_branch_hg", [DEPTH, 1024, D], F32), ("w_out", [DEPTH, D, D], F32),
        ("peer_w_q", [DEPTH, D, D], F32), ("peer_kt", [DEPTH, 128, 16, 128], F32),
        ("peer_u", [DEPTH, NEXP, D], F32), ("peer_v", [DEPTH, NEXP, D], F32), ("final_norm_g", [1, D], F32), ("bglu_l", [DEPTH, 128, 8], F32), ("s5_w_glu", [DEPTH, 1024, 1024], F32),
    ]


SCRATCH_SPECS = [
    ("X", [NT, D], F32), ("MODS", [NB + 1, 128, 6 * D], F32), ("UT", [1024, NT], F32), ("QFT", [4096, NT], F32),
    ("VOG", [NT, 1024], F32), ("GT", [4096, NT], F32), ("Y5T", [1024, NT], BF16), ("YHT", [1024, NT], BF16),
    ("U16", [NEXP, D], BF16), ("V16", [NEXP, D], BF16),
]


def build(dbg=None):
    P = Prog(dbg)
    dbg = P.dbg
    feed = dbg.get("feed", ())
    only = dbg.get("only", None)
    need_in = dbg.get("inputs", None)
    for (n, shp, dt) in input_specs(dbg.get("depth_in", DEPTH)):
        if need_in is None or n in need_in:
            P.din(n, shp, dt)
    if only is None or "final" in only:
        P.dout("out", [NB, SEQ, D], F32)
    for (n, shp, dt) in SCRATCH_SPECS:
        if n in feed:
            P.din(n, shp, dt)
        else:
            P.dscr(n, shp, dt)
    run = lambda ph: only is None or ph in only
    with P.es:
        st = P.es
        P.consts(st)
        if run("init"):
            P.phase_init_x()
        if run("hg"):
            P.phase_hg_lb(st)
        nl = dbg.get("layers", DEPTH)
        for l in range(nl):
            if run("mods"):
                P.phase_mods(l)
            if run("win"):
                P.phase_win(l)
            if run("s5"):
                P.phase_s5(l)
            if run("hg"):
                P.phase_hgrn(l)
            if run("merge"):
                P.phase_merge(l)
            if run("peer"):
                P.phase_peer(l)
        if run("final"):
            P.phase_final()
        for nme in dbg.get("copyout", ()):
            P.k.barrier()
            src = P.dram[nme]
            t = P.nc.dram_tensor("co_" + nme, list(src.shape), src.dtype, kind="ExternalOutput").ap()
            ds = P.k.dfree[0]
            nr = src.shape[0]
            step = (nr + 7) // 8
            P.k.dma(P.k.sp, [(t[r:min(r + step, nr)], src[r:min(r + step, nr)]) for r in range(0, nr, step)], ds,
                    reads=[P.dbuf[nme]], writes=[Buf("co")], accumulate=True)
        P.k.barrier()
    return P


def s5_layouts(inputs):
    a_re, a_im, ldt = inputs["s5_a_re"], inputs["s5_a_im"], inputs["s5_log_dt"]
    L = a_re.shape[0]
    def sl(a):
        return a.reshape(L, 2, 32, 2, 64).transpose(0, 1, 3, 4, 2).reshape(L, 2, 128, 32)
    ldt_sl = np.broadcast_to(ldt.reshape(L, 2, 32, 2).transpose(0, 1, 3, 2)[:, :, :, None, :], (L, 2, 2, 64, 32)).reshape(L, 2, 128, 32)
    s5sl = np.stack([sl(a_re), sl(a_im), ldt_sl], axis=3)
    def bl(a):
        t = a.reshape(L, 2, 8, 8, 64).transpose(0, 1, 3, 2, 4)
        return np.broadcast_to(t[:, :, :, None, :, :], (L, 2, 8, 16, 8, 64)).reshape(L, 2, 128, 512)
    ldt_g = np.broadcast_to(ldt[:, :, :, None], a_re.shape)
    s5bl = np.stack([bl(a_re), bl(a_im), bl(ldt_g)], axis=3)
    def btl(Bm):
        t = Bm.reshape(L, 2, 8, 8, 64, 16).transpose(0, 1, 3, 5, 2, 4)
        o = np.zeros((L, 2, 8, 16, 8, 2, 64), np.float32)
        for gl in range(8):
            o[:, :, gl, :, :, gl % 2, :] = t[:, :, gl]
        return o.reshape(L, 2, 128, 8, 128)
    s5bt = np.stack([btl(inputs["s5_b_re"]), btl(inputs["s5_b_im"])], axis=3)
    def ctl(Cm):
        t = Cm.reshape(L, 2, 32, 2, 16, 64)
        o = np.zeros((L, 2, 2, 64, 32, 2, 16), np.float32)
        for s_ in range(2):
            o[:, :, s_, :, :, s_, :] = t[:, :, :, s_].transpose(0, 1, 4, 2, 3)
        return o.reshape(L, 2, 128, 32, 32)
    s5ct = np.stack([ctl(inputs["s5_c_re"]), ctl(inputs["s5_c_im"])], axis=3)
    chl = lambda v: np.ascontiguousarray(v.reshape(L, 8, 128).transpose(0, 2, 1))
    return dict(s5sl=np.ascontiguousarray(s5sl), s5bl=np.ascontiguousarray(s5bl), s5bt=np.ascontiguousarray(s5bt),
                s5ct=np.ascontiguousarray(s5ct), s5d_l=chl(inputs["s5_d"]), bglu_l=chl(inputs["s5_b_glu"]))


def shared_inputs(inputs):
    d = {}
    d["cst"] = host_consts()
    c3 = np.zeros((128, 4), np.float32)
    c3[96:, 0] = 1.0
    d["cst3"] = c3
    d["cst2"] = np.ascontiguousarray(np.broadcast_to(np.arange(513, dtype=np.float32)[None, :], (128, 513)))
    for kname in ("w_ada", "b_ada", "norm_mix_g", "norm_ffn_g", "w_in", "s5_w_glu", "w_branch_s5", "w_branch_hg", "w_out",
                  "peer_w_q", "peer_u", "peer_v"):
        d[kname] = np.ascontiguousarray(inputs[kname])
    d["final_norm_g"] = np.ascontiguousarray(inputs["final_norm_g"][None, :])
    kt = np.stack([inputs["peer_k1"], inputs["peer_k2"]], axis=2)
    d["peer_kt"] = np.ascontiguousarray(kt.transpose(0, 4, 1, 2, 3).reshape(DEPTH, 128, 16, 128))
    d.update(s5_layouts(inputs))
    gam = inputs["hg_lb_gamma"]
    d["gam_l"] = np.ascontiguousarray(gam.reshape(DEPTH, 2, 8, 128).transpose(3, 0, 1, 2).reshape(128, DEPTH, 16))
    d["ghg_l"] = np.ascontiguousarray(inputs["hg_norm_g"].reshape(DEPTH, 8, 128).transpose(0, 2, 1))
    return d


def core_inputs(inputs, core, shared=None):
    b0 = core * NB
    d = dict(shared if shared is not None else shared_inputs(inputs))
    d["x"] = np.ascontiguousarray(inputs["x"][b0:b0 + NB])
    d["ctx"] = np.ascontiguousarray(inputs["ctx"][b0:b0 + NB])
    d["cvec"] = np.ascontiguousarray(np.concatenate([inputs["c"][b0:b0 + NB], inputs["c_ctx"][None, :]], axis=0))
    return d


_PROG_CACHE = {}


def kernel(**inputs):
    inputs = {k_: np.asarray(v) for k_, v in inputs.items()}
    if "prog" not in _PROG_CACHE:
        _PROG_CACHE["prog"] = build()
    P = _PROG_CACHE["prog"]
    shared = shared_inputs(inputs)
    names = [n for (n, _s, _t) in input_specs(DEPTH)]
    in_maps = []
    for c in range(N_CORES):
        d = core_inputs(inputs, c, shared)
        in_maps.append({n: d[n] for n in names})
    res = run_bass_kernel_spmd(P.nc, in_maps, core_ids=list(range(N_CORES)))
    out = np.concatenate([np.asarray(res.results[c]["out"]) for c in range(N_CORES)], axis=0)
    return out.astype(np.float32, copy=False)
```
